# Optimizing a Trainium2 kernel written in Bass

```python
import jax, jax.numpy as jnp
from jax import lax
import numpy as np

D_MODEL = 1024
BATCH = 16
SEQ = 2048
DEPTH = 1

NORM_EPS = 1e-6
HG_HEADS = 4
HG_KDIM = 128
HG_VDIM = 128
HG_CHUNK = 64
HG_WIDTH = HG_HEADS * HG_KDIM
ATT_GROUPS = ((128, 1), (512, 4), (2048, 16))
ATT_N_GROUPS = 3
ATT_HEADS_PER_GROUP = 4
ATT_HEAD_DIM = 64
ATT_BLOCK = 128
ATT_WIDTH = ATT_N_GROUPS * ATT_HEADS_PER_GROUP * ATT_HEAD_DIM
ATT_OUT_WIDTH = ATT_HEADS_PER_GROUP * ATT_HEAD_DIM
PEER_HEADS = 8
PEER_NKEYS = 128
PEER_N_EXPERTS = PEER_NKEYS * PEER_NKEYS
PEER_QDIM = 128
PEER_HALF = PEER_QDIM // 2
PEER_TOPK = 16
PEER_TOKEN_BLOCK = 128
IN_COLS = 4 * HG_WIDTH + 3 * ATT_WIDTH + 2 * D_MODEL
IN_SPLITS = (HG_WIDTH, 2 * HG_WIDTH, 3 * HG_WIDTH, 4 * HG_WIDTH,
             4 * HG_WIDTH + ATT_WIDTH, 4 * HG_WIDTH + 2 * ATT_WIDTH,
             4 * HG_WIDTH + 3 * ATT_WIDTH, 4 * HG_WIDTH + 3 * ATT_WIDTH + D_MODEL)

kernel_name = "hybrid_hgrn2_dilated_attn_peer"


def rms_norm(x, g):
    xf = x.astype(jnp.float32)
    y = xf * lax.rsqrt(jnp.mean(xf * xf, axis=-1, keepdims=True) + NORM_EPS)
    return (y * g.astype(jnp.float32)).astype(x.dtype)


def hgrn2_mixer(q_pre, f_pre, i_pre, og_pre, lb, norm_g):
    B, S, _ = q_pre.shape
    f32 = jnp.float32
    lb = lb.astype(f32)
    f = lb + (1.0 - lb) * jax.nn.sigmoid(f_pre.astype(f32))
    log_f = jnp.log(f)
    k = 1.0 - f
    q = jax.nn.sigmoid(q_pre.astype(f32))
    v = i_pre.astype(f32)
    nc = S // HG_CHUNK

    def heads(t):
        return t.reshape(B, nc, HG_CHUNK, HG_HEADS, -1).transpose(0, 3, 1, 2, 4)

    q, k, v, log_f = heads(q), heads(k), heads(v), heads(log_f)
    G = jnp.cumsum(log_f, axis=3)
    G_last = G[:, :, :, -1:, :]
    q_dec = q * jnp.exp(G)
    k_intra = k * jnp.exp(-G)
    k_state = k * jnp.exp(G_last - G)
    causal = jnp.tril(jnp.ones((HG_CHUNK, HG_CHUNK), dtype=bool))
    A = jnp.where(causal, jnp.einsum('bhnck,bhnsk->bhncs', q_dec, k_intra), 0.0)
    o_intra = jnp.einsum('bhncs,bhnsv->bhncv', A, v)

    def step(state, inp):
        q_c, ks_c, v_c, decay_c = inp
        o_c = jnp.einsum('bhck,bhkv->bhcv', q_c, state)
        new_state = decay_c[..., None] * state + jnp.einsum('bhck,bhcv->bhkv', ks_c, v_c)
        return new_state, o_c

    state0 = jnp.zeros((B, HG_HEADS, HG_KDIM, HG_VDIM), f32)
    xs = (jnp.moveaxis(q_dec, 2, 0), jnp.moveaxis(k_state, 2, 0), jnp.moveaxis(v, 2, 0),
          jnp.moveaxis(jnp.exp(G_last[:, :, :, 0, :]), 2, 0))
    _, o_inter = lax.scan(step, state0, xs)
    o = o_intra + jnp.moveaxis(o_inter, 0, 2)
    o = o.transpose(0, 2, 3, 1, 4).reshape(B, S, HG_HEADS, HG_VDIM)
    o = o * lax.rsqrt(jnp.mean(o * o, axis=-1, keepdims=True) + NORM_EPS) * norm_g.astype(f32)
    o = o.reshape(B, S, HG_WIDTH) * jax.nn.silu(og_pre.astype(f32))
    return o.astype(q_pre.dtype)


def dilated_window_attention(q, k, v, window, dilation):
    B, S, H, E = q.shape
    L = S // dilation
    n_blk = -(-L // ATT_BLOCK)
    Lp = n_blk * ATT_BLOCK

    def to_residue(t):
        t = t.reshape(B, L, dilation, H, E).transpose(0, 2, 3, 1, 4)
        return jnp.pad(t, ((0, 0), (0, 0), (0, 0), (0, Lp - L), (0, 0)))

    def with_prev(t):
        tb = t.reshape(B, dilation, H, n_blk, ATT_BLOCK, E)
        prev = jnp.pad(tb[:, :, :, :-1], ((0, 0), (0, 0), (0, 0), (1, 0), (0, 0), (0, 0)))
        return jnp.concatenate([prev, tb], axis=4)

    qb = to_residue(q).reshape(B, dilation, H, n_blk, ATT_BLOCK, E)
    kb = with_prev(to_residue(k))
    vb = with_prev(to_residue(v))
    scores = jnp.einsum('bdhnqe,bdhnke->bdhnqk', qb, kb) * (E ** -0.5)
    n_back = window // dilation
    blk = jnp.arange(n_blk)[:, None, None]
    q_pos = blk * ATT_BLOCK + jnp.arange(ATT_BLOCK)[None, :, None]
    k_pos = (blk - 1) * ATT_BLOCK + jnp.arange(2 * ATT_BLOCK)[None, None, :]
    dist = q_pos - k_pos
    valid = (dist >= 0) & (dist <= n_back) & (k_pos >= 0)
    scores = jnp.where(valid, scores, -jnp.inf)
    m = jnp.max(scores, axis=-1, keepdims=True)
    p = jnp.exp(scores - m)
    l = jnp.sum(p, axis=-1, keepdims=True)
    o = jnp.einsum('bdhnqk,bdhnke->bdhnqe', p, vb) / l

    def from_residue(t):
        X = t.shape[-1]
        t = t.reshape(B, dilation, H, Lp, X)[:, :, :, :L]
        return t.transpose(0, 3, 1, 2, 4).reshape(B, S, H, X)

    return from_residue(o), from_residue(m), from_residue(l)


def dilated_attention_mixer(q_pre, k_pre, v_pre, q_norm_g, k_norm_g):
    B, S, _ = q_pre.shape
    f32 = jnp.float32
    shp = (B, S, ATT_N_GROUPS, ATT_HEADS_PER_GROUP, ATT_HEAD_DIM)

    def head_rms(t, g):
        return t * lax.rsqrt(jnp.mean(t * t, axis=-1, keepdims=True) + NORM_EPS) * g.astype(f32)

    q = head_rms(q_pre.astype(f32).reshape(shp), q_norm_g)
    k = head_rms(k_pre.astype(f32).reshape(shp), k_norm_g)
    v = v_pre.astype(f32).reshape(shp)
    outs = [dilated_window_attention(q[:, :, g], k[:, :, g], v[:, :, g], w, d)
            for g, (w, d) in enumerate(ATT_GROUPS)]
    o_all = jnp.stack([o for o, _, _ in outs])
    m_all = jnp.stack([m for _, m, _ in outs])
    l_all = jnp.stack([l for _, _, l in outs])
    w_all = l_all * jnp.exp(m_all - jnp.max(m_all, axis=0, keepdims=True))
    o = jnp.sum(w_all * o_all, axis=0) / jnp.sum(w_all, axis=0)
    return o.reshape(B, S, ATT_OUT_WIDTH).astype(q_pre.dtype)


def peer_ffn(x, w_q, sub_keys, u, v):
    B, S, D = x.shape
    q = (x @ w_q).reshape(B, S, PEER_HEADS, 2, PEER_HALF)
    s = jnp.einsum('bshpc,hpnc->bshpn', q, sub_keys).astype(jnp.float32)
    s1, i1 = lax.top_k(s[..., 0, :], PEER_TOPK)
    s2, i2 = lax.top_k(s[..., 1, :], PEER_TOPK)
    cand_s = (s1[..., :, None] + s2[..., None, :]).reshape(B, S, PEER_HEADS, PEER_TOPK * PEER_TOPK)
    cand_i = (i1[..., :, None] * PEER_NKEYS + i2[..., None, :]).reshape(B, S, PEER_HEADS, PEER_TOPK * PEER_TOPK)
    top_s, pos = lax.top_k(cand_s, PEER_TOPK)
    idx = jnp.take_along_axis(cand_i, pos, axis=-1)
    gate = jax.nn.softmax(top_s, axis=-1)
    n_blk = (B * S) // PEER_TOKEN_BLOCK
    xt = x.reshape(n_blk, PEER_TOKEN_BLOCK, D)
    it = idx.reshape(n_blk, PEER_TOKEN_BLOCK, PEER_HEADS, PEER_TOPK)
    gt = gate.reshape(n_blk, PEER_TOKEN_BLOCK, PEER_HEADS, PEER_TOPK)

    def block(args):
        xb, ib, gb = args
        hid = jnp.einsum('thkd,td->thk', u[ib], xb)
        a = jax.nn.gelu(hid.astype(jnp.float32), approximate=False) * gb
        return jnp.einsum('thk,thkd->td', a.astype(x.dtype), v[ib])

    out = lax.map(block, (xt, it, gt))
    return out.reshape(B, S, D)


def setup_inputs(seed: int = 0) -> dict:
    key = jax.random.key(seed)
    ks = jax.random.split(key, 16)
    f32 = jnp.float32
    nrm = lambda k, shape, scale: jax.random.normal(k, shape, f32) * scale
    return {
        "x": nrm(ks[0], (BATCH, SEQ, D_MODEL), 1.0),
        "norm1_g": 1.0 + nrm(ks[1], (DEPTH, D_MODEL), 0.02),
        "w_in": nrm(ks[2], (DEPTH, D_MODEL, IN_COLS), D_MODEL ** -0.5),
        "hg_norm_g": 1.0 + nrm(ks[3], (DEPTH, HG_VDIM), 0.02),
        "hg_lb_logits": nrm(ks[4], (DEPTH + 1, HG_WIDTH), 0.02),
        "q_norm_g": 1.0 + nrm(ks[5], (DEPTH, ATT_HEAD_DIM), 0.02),
        "k_norm_g": 1.0 + nrm(ks[6], (DEPTH, ATT_HEAD_DIM), 0.02),
        "w_branch_a": nrm(ks[7], (DEPTH, HG_WIDTH, D_MODEL), HG_WIDTH ** -0.5),
        "w_branch_b": nrm(ks[8], (DEPTH, ATT_OUT_WIDTH, D_MODEL), ATT_OUT_WIDTH ** -0.5),
        "w_out": nrm(ks[9], (DEPTH, D_MODEL, D_MODEL), D_MODEL ** -0.5),
        "norm2_g": 1.0 + nrm(ks[10], (DEPTH, D_MODEL), 0.02),
        "peer_wq": nrm(ks[11], (DEPTH, D_MODEL, PEER_HEADS * PEER_QDIM), D_MODEL ** -0.5),
        "peer_subkeys": nrm(ks[12], (DEPTH, PEER_HEADS, 2, PEER_NKEYS, PEER_HALF), PEER_HALF ** -0.5),
        "peer_u": nrm(ks[13], (DEPTH, PEER_N_EXPERTS, D_MODEL), D_MODEL ** -0.5),
        "peer_v": nrm(ks[14], (DEPTH, PEER_N_EXPERTS, D_MODEL), PEER_HEADS ** -0.5),
    }


def reference(x, norm1_g, w_in, hg_norm_g, hg_lb_logits, q_norm_g, k_norm_g, w_branch_a,
              w_branch_b, w_out, norm2_g, peer_wq, peer_subkeys, peer_u, peer_v):
    lb_all = jnp.cumsum(jax.nn.softmax(hg_lb_logits.astype(jnp.float32), axis=0), axis=0)
    for layer in range(DEPTH):
        h = rms_norm(x, norm1_g[layer])
        proj = h @ w_in[layer]
        hq, hf, hi, hog, aq, ak, av, ga, gb = jnp.split(proj, IN_SPLITS, axis=-1)
        y_a = hgrn2_mixer(hq, hf, hi, hog, lb_all[layer], hg_norm_g[layer])
        y_b = dilated_attention_mixer(aq, ak, av, q_norm_g[layer], k_norm_g[layer])
        merged = (jax.nn.sigmoid(ga) * (y_a @ w_branch_a[layer])
                  + jax.nn.sigmoid(gb) * (y_b @ w_branch_b[layer]))
        x = x + merged @ w_out[layer]
        h2 = rms_norm(x, norm2_g[layer])
        x = x + peer_ffn(h2, peer_wq[layer], peer_subkeys[layer], peer_u[layer], peer_v[layer])
    return x
```

```python
import numpy as np
import ml_dtypes
import concourse.bass as bass
import concourse.mybir as mybir
from concourse.bass_utils import run_bass_kernel_spmd

F32 = mybir.dt.float32
BF16 = mybir.dt.bfloat16
AF = mybir.ActivationFunctionType
ALU = mybir.AluOpType
AX = mybir.AxisListType

NCORES = 8
D = 1024
S = 2048
NSEQ = 2
NT = S // 128
EPS = 1e-6
INC = 6400


class _Op:
    __slots__ = ("id", "eng", "fn", "deps", "dma", "needed", "sem", "val", "prewait", "waits")

    def __init__(self, id, eng, fn, dma):
        self.id = id
        self.eng = eng
        self.fn = fn
        self.deps = {}
        self.dma = dma
        self.needed = False
        self.sem = None
        self.val = 0
        self.prewait = None
        self.waits = []


class Sched:
    ENGS = ("pe", "act", "dve", "pool", "sp")

    def __init__(self):
        self.ops = []
        self.lastw = {}
        self.readers = {}
        self.last_eng_op = {}
        self.dma_since_barrier = []

    def add(self, eng, fn, reads=(), writes=(), dma=False):
        op = _Op(len(self.ops), eng, fn, dma)
        deps = op.deps
        for k in reads:
            w = self.lastw.get(k)
            if w is not None:
                deps[w] = True
        for k in writes:
            w = self.lastw.get(k)
            if w is not None:
                deps.setdefault(w, False)
            rd = self.readers.get(k)
            if rd:
                for r in rd.values():
                    deps.setdefault(r, False)
        for k in writes:
            self.lastw[k] = op.id
            self.readers[k] = {}
        for k in reads:
            rd = self.readers.setdefault(k, {})
            rd[("dma", op.id) if dma else eng] = op.id
        for d in list(deps):
            p = self.ops[d]
            if p.dma or dma:
                continue
            if p.eng == eng and (eng == "pe" or not deps[d]):
                del deps[d]
        self.ops.append(op)
        if dma:
            self.dma_since_barrier.append(op.id)
        elif fn is not None:
            self.last_eng_op[eng] = op.id
        return op

    def barrier(self):
        targets = list(self.last_eng_op.values()) + list(self.dma_since_barrier)
        for e in self.ENGS:
            op = _Op(len(self.ops), e, None, False)
            for t in targets:
                op.deps[t] = True
            self.ops.append(op)
        self.lastw = {}
        self.readers = {}
        self.dma_since_barrier = []

    def finalize(self, sems, dma_sems):
        for op in self.ops:
            for d in op.deps:
                self.ops[d].needed = True
        cnt = {e: 0 for e in self.ENGS}
        dcnt = {e: 0 for e in self.ENGS}
        for op in self.ops:
            if op.dma:
                j = dcnt[op.eng]
                dcnt[op.eng] += 1
                pool = dma_sems[op.eng]
                op.sem = pool[j % len(pool)]
                op.val = 16 * (j // len(pool) + 1)
                if op.val > 16:
                    op.prewait = (op.sem, op.val - 16)
            elif op.needed:
                cnt[op.eng] += 1
                op.sem = sems[op.eng]
                op.val = cnt[op.eng]
        know = {e: {} for e in self.ENGS}
        prod_know = {}
        for op in self.ops:
            K = know[op.eng]
            need = {}
            for d in sorted(op.deps, reverse=True):
                p = self.ops[d]
                key = id(p.sem)
                if key not in need or need[key][1] < p.val:
                    need[key] = (p.sem, p.val)
            if op.prewait is not None:
                key = id(op.prewait[0])
                if key not in need or need[key][1] < op.prewait[1]:
                    need[key] = op.prewait
            waits = []
            for key, (sm, v) in need.items():
                if K.get(key, 0) >= v:
                    continue
                waits.append((sm, v))
                K[key] = v
                pk = prod_know.get((key, v))
                if pk:
                    for k2, v2 in pk.items():
                        if K.get(k2, 0) < v2:
                            K[k2] = v2
            op.waits = waits
            if op.sem is not None and (op.dma or op.needed):
                snap = dict(K)
                if not op.dma:
                    snap[id(op.sem)] = op.val
                prod_know[(id(op.sem), op.val)] = snap

    def emit_engine(self, eng, E):
        for op in self.ops:
            if op.eng != eng:
                continue
            for (sm, v) in op.waits:
                E.wait_ge(sm, v)
            if op.fn is None:
                continue
            ins = op.fn(E)
            if op.dma:
                ins.then_inc(op.sem, 16)
            elif op.needed:
                ins.then_inc(op.sem, 1)


def build_program(debug=None, nseq=NSEQ):
    from contextlib import ExitStack

    nc = bass.Bass("TRN2", target_bir_lowering=False)
    sc = Sched()
    dbg_outs = {}

    def din(name, shape, dt=F32):
        return nc.dram_tensor(name, list(shape), dt, kind="ExternalInput").ap()

    x_d = din("x", [nseq, S, D])
    win_d = din("w_in", [D, INC])
    out_d = nc.dram_tensor("out", [nseq, S, D], F32, kind="ExternalOutput").ap()

    def dbg_out(name, shape, dt=F32):
        t = nc.dram_tensor("dbg_" + name, list(shape), dt, kind="ExternalOutput").ap()
        dbg_outs[name] = t
        return t

    es = ExitStack()

    def sb(name, shape, dt=F32):
        return es.enter_context(nc.sbuf_tensor("sb_" + name, list(shape), dt))

    def ps(name, shape, dt=F32):
        return es.enter_context(nc.psum_tensor("ps_" + name, list(shape), dt))

    def A(eng, fn, r=(), w=(), dma=False):
        return sc.add(eng, fn, r, w, dma)

    uniq = [0]

    class Scope:
        def __init__(self):
            self.es = ExitStack()

        def sb(self, name, shape, dt=F32, side=None):
            uniq[0] += 1
            return self.es.enter_context(nc.sbuf_tensor(f"sb_{name}_{uniq[0]}", list(shape), dt, side=side))

        def ps(self, name, shape, dt=F32):
            uniq[0] += 1
            return self.es.enter_context(nc.psum_tensor(f"ps_{name}_{uniq[0]}", list(shape), dt))

        def close(self):
            sc.barrier()
            self.es.close()

    cst = {}

    def load_const(name, shape, dt=F32):
        d = din(name, shape, dt)
        t = sb("c_" + name, shape, dt)
        if len(shape) == 2:
            A("sp", lambda e: e.dma_start(out=t[:], in_=d[:, :]), w=[name], dma=True)
        else:
            A("sp", lambda e: e.dma_start(out=t[:], in_=d[:, :, :]), w=[name], dma=True)
        cst[name] = t
        return t

    ident = load_const("ident", [128, 128], BF16)
    maskU_d = din("maskU", [128, 128])
    onesv = load_const("onesv", [128, 128], BF16)
    scanm_d = din("scanm", [128, 1024])
    lbl = load_const("lbl", [128, 8])
    hgg = load_const("hgg", [128, 1])
    load_const("blk64", [128, 128], BF16)
    load_const("ones64", [128, 64], BF16)
    load_const("maskUb", [128, 128], BF16)
    load_const("maskLb", [128, 128], BF16)
    shsel_d = din("shsel", [128, 64])
    load_const("epsc", [128, 1])
    load_const("gq", [128, 1])
    load_const("gk", [128, 1])
    lbc = sb("lbc", [128, 12])
    A("dve", lambda e: e.tensor_sub(out=lbc[:, 8:12], in0=lbl[:, 0:4], in1=lbl[:, 4:8]), r=["lbl"], w=["lbtmp"])
    A("act", lambda e: e.activation(out=lbc[:, 0:4], in_=lbc[:, 8:12], func=AF.Sigmoid), r=["lbtmp"], w=["lb0"])
    A("dve", lambda e: e.tensor_scalar(out=lbc[:, 4:8], in0=lbc[:, 0:4], scalar1=-1.0, scalar2=1.0,
                                       op0=ALU.mult, op1=ALU.add), r=["lb0"], w=["lb"])

    g1bc_d = din("g1bc", [128, D])
    g2bc_d = din("g2bc", [128, D])
    ctx = {}
    ostage = sb("ostage", [128, 512])

    def stage_norm(s, src):
        hT = ctx["hT"]
        sp_ = Scope()
        gbc = sp_.sb("gbc", [128, D])
        gsrc = g1bc_d if src == "x" else g2bc_d
        A("sp", lambda e: e.dma_start(out=gbc[:], in_=gsrc[:, :]), w=["gbc"], dma=True)
        xt = [sp_.sb(f"xt{i}", [128, D]) for i in range(2)]
        junk = sp_.sb("junk", [128, D], BF16)
        xn = [sp_.sb(f"xn{i}", [128, D], BF16) for i in range(2)]
        stat = sp_.sb("stat", [128, 4, 4])
        pT = [sp_.ps(f"pT{i}", [128, 8, 128], BF16) for i in range(2)]
        def stage_a(i):
            b = i % 2
            if src == "x":
                A("sp", lambda e: e.dma_start(out=xt[b][:], in_=x_d[s, i * 128:(i + 1) * 128, :]),
                  w=[("xt", b)], dma=True)
                xin = xt[b][:]
                xk = ("xt", b)
            else:
                xin = ctx["xacc"][:, i, :]
                xk = ("xacc", i)
            q4 = i % 4
            A("act", lambda e: e.activation(out=junk[:], in_=xin, func=AF.Square, accum_out=stat[:, q4, 0:1]),
              r=[xk], w=[("stat", q4, 0)])
            A("act", lambda e: e.activation(out=stat[:, q4, 1:2], in_=stat[:, q4, 0:1], func=AF.Sqrt,
                                            bias=EPS, scale=1.0 / D),
              r=[("stat", q4, 0)], w=[("stat", q4, 1)])
            A("dve", lambda e: e.reciprocal(out=stat[:, q4, 2:3], in_=stat[:, q4, 1:2]),
              r=[("stat", q4, 1)], w=[("stat", q4, 2)])
            A("dve", lambda e: e.scalar_tensor_tensor(
                out=xn[b][:], in0=xin, scalar=stat[:, q4, 2:3], in1=gbc[:], op0=ALU.mult, op1=ALU.mult),
              r=[xk, ("stat", q4, 2), "gbc"], w=[("xn", b)])
            for c in range(8):
                A("pe", lambda e, c=c: e.transpose(out=pT[b][:, c, :], in_=xn[b][:, c * 128:(c + 1) * 128],
                                                   identity=ident[:]),
                  r=[("xn", b), "ident"], w=[("pT", b)])

        def stage_b(i):
            b = i % 2
            A("act", lambda e: e.copy(out=hT[:, :, i * 128:(i + 1) * 128], in_=pT[b][:]),
              r=[("pT", b)], w=[("hT", i // 4)])

        stage_a(0)
        for i in range(NT):
            if i + 1 < NT:
                stage_a(i + 1)
            stage_b(i)
        sp_.close()

    wload_n = [0]

    def load_wblock(col0, ncols=512, src=None):
        src = win_d if src is None else src
        wbf = ctx["wbf"]
        b = wload_n[0] % 3
        wload_n[0] += 1
        A("pool", lambda e: e.dma_start(out=wbf[b][:, :, :ncols],
                                        in_=src[:, col0:col0 + ncols].rearrange("(c p) n -> p c n", p=128)),
          w=[("wbf", b)], dma=True)
        return b

    def proj_fm(pout, pkey, wb, j, t0, nt):
        hT = ctx["hT"]
        wbf = ctx["wbf"]
        for c in range(8):
            A("pe", lambda e, c=c: e.matmul(pout, lhsT=wbf[wb][:, c, j * 128:(j + 1) * 128],
                                            rhs=hT[:, c, t0:t0 + nt], start=(c == 0), stop=(c == 7)),
              r=[("wbf", wb)] + [("hT", k) for k in range(t0 // 512, (t0 + nt - 1) // 512 + 1)], w=[pkey])

    HS = 1024

    def stage_hgrn(s):
        wbf = ctx["wbf"]
        hT = ctx["hT"]
        yaT = ctx["yaT"]
        so = Scope()
        qd = so.sb("qd", [128, 4, S], BF16)
        ki = so.sb("ki", [128, 4, S], BF16)
        ks_tm = so.sb("ks_tm", [128, 4, NT, 128], BF16)
        v_tm = so.sb("v_tm", [128, NT, 512], BF16)
        sog = so.sb("sog", [128, 4, S], BF16)
        dec = so.sb("dec", [128, 4, NT])
        sa = Scope()
        t1 = sa.sb("t1", [128, S])
        t2 = sa.sb("t2", [128, S])
        Gt = sa.sb("G", [128, S])
        eG = sa.sb("eG", [128, S])
        kst = sa.sb("kst", [128, S], BF16)
        scanm = sa.sb("scanm", [128, 1024])
        A("sp", lambda e: e.dma_start(out=scanm[:], in_=scanm_d[:, :]), w=["scanm"], dma=True)
        pp = [sa.ps(f"pp{i}", [128, 2, 512]) for i in range(3)]
        pTk = sa.ps("pTk", [128, 16, 128], BF16)
        ppn = [0]

        def next_pp():
            i = ppn[0] % 3
            ppn[0] += 1
            return pp[i], ("pp", i)

        wb_f = load_wblock(512)
        wb_q = load_wblock(0)
        tq = sa.sb("tq", [128, S])
        HF = (0, 1)
        SL = [slice(hf * HS, (hf + 1) * HS) for hf in HF]
        for h in range(4):
            pf, pq_ = {}, {}
            for hf in HF:
                pf[hf] = next_pp()
                for q2 in range(2):
                    proj_fm(pf[hf][0][:, q2, :], pf[hf][1], wb_f, h, hf * HS + q2 * 512, 512)
            for hf in HF:
                p, pk = pf[hf]
                A("act", lambda e, p=p, sl=SL[hf]: e.activation(out=t1[:, sl], in_=p[:].rearrange("p a b -> p (a b)"),
                                                                func=AF.Sigmoid), r=[pk], w=[("t1", hf)])
            for hf in HF:
                pq_[hf] = next_pp()
                for q2 in range(2):
                    proj_fm(pq_[hf][0][:, q2, :], pq_[hf][1], wb_q, h, hf * HS + q2 * 512, 512)
                p, pk = pq_[hf]
                A("act", lambda e, p=p, sl=SL[hf]: e.activation(out=tq[:, sl], in_=p[:].rearrange("p a b -> p (a b)"),
                                                                func=AF.Sigmoid), r=[pk], w=[("tq", hf)])
            for hf in HF:
                A("dve", lambda e, sl=SL[hf], h=h: e.tensor_scalar(out=t2[:, sl], in0=t1[:, sl], scalar1=lbc[:, 4 + h:5 + h],
                                                                   scalar2=lbc[:, h:h + 1], op0=ALU.mult, op1=ALU.add),
                  r=[("t1", hf), "lb", "lb0"], w=[("t2", hf)])
            for hf in HF:
                A("act", lambda e, sl=SL[hf]: e.activation(out=t1[:, sl], in_=t2[:, sl], func=AF.Ln),
                  r=[("t2", hf)], w=[("t1", hf)])
            for hf in HF:
                A("dve", lambda e, sl=SL[hf]: e.tensor_tensor_scan(out=Gt[:, sl], data0=scanm[:], data1=t1[:, sl],
                                                                   initial=0.0, op0=ALU.mult, op1=ALU.add),
                  r=[("t1", hf), "scanm"], w=[("G", hf)])
                A("pool", lambda e, sl=SL[hf]: e.tensor_scalar(out=t2[:, sl], in0=t2[:, sl], scalar1=-1.0, scalar2=1.0,
                                                               op0=ALU.mult, op1=ALU.add),
                  r=[("t2", hf)], w=[("t2", hf)])
            for hf in HF:
                A("act", lambda e, sl=SL[hf]: e.activation(out=eG[:, sl], in_=Gt[:, sl], func=AF.Exp),
                  r=[("G", hf)], w=[("eG", hf)])
                A("act", lambda e, sl=SL[hf]: e.activation(out=t1[:, sl], in_=Gt[:, sl], func=AF.Exp, scale=-1.0),
                  r=[("G", hf)], w=[("t1", hf)])
            for hf in HF:
                A("dve", lambda e, sl=SL[hf]: e.tensor_mul(out=t2[:, sl], in0=t2[:, sl], in1=t1[:, sl]),
                  r=[("t2", hf), ("t1", hf)], w=[("t2", hf)])
                A("act", lambda e, sl=SL[hf], h=h, hf=hf: e.copy(
                    out=dec[:, h, hf * 8:(hf + 1) * 8],
                    in_=eG[:, sl].rearrange("p (n c) -> p n c", c=128)[:, :, 127]),
                  r=[("eG", hf)], w=[("dec", h, hf)])
            for hf in HF:
                A("dve", lambda e, sl=SL[hf], h=h: e.tensor_copy(out=ki[:, h, sl], in_=t2[:, sl]),
                  r=[("t2", hf)], w=[("ki", h, hf)])
                A("dve", lambda e, sl=SL[hf], h=h, hf=hf: e.tensor_tensor(
                    out=kst[:, sl].rearrange("p (n c) -> p n c", c=128),
                    in0=t2[:, sl].rearrange("p (n c) -> p n c", c=128),
                    in1=dec[:, h, hf * 8:(hf + 1) * 8].unsqueeze(2).to_broadcast([128, 8, 128]), op=ALU.mult),
                  r=[("t2", hf), ("dec", h, hf)], w=[("kst", hf)])
                A("dve", lambda e, sl=SL[hf], h=h: e.tensor_mul(out=qd[:, h, sl], in0=tq[:, sl], in1=eG[:, sl]),
                  r=[("tq", hf), ("eG", hf)], w=[("qd", h, hf)])
            for hf in HF:
                for cc in range(8):
                    n = hf * 8 + cc
                    A("pe", lambda e, n=n: e.transpose(out=pTk[:, n, :], in_=kst[:, n * 128:(n + 1) * 128],
                                                       identity=ident[:]),
                      r=[("kst", hf), "ident"], w=[("pTk", hf)])
                A("act", lambda e, h=h, hf=hf: e.copy(out=ks_tm[:, h, hf * 8:(hf + 1) * 8, :],
                                                      in_=pTk[:, hf * 8:(hf + 1) * 8, :]),
                  r=[("pTk", hf)], w=[("ks_tm", h, hf)])
        wb_o = load_wblock(1536)
        wb_v = load_wblock(1024)
        for h in range(4):
            for hf in range(2):
                sl = slice(hf * HS, (hf + 1) * HS)
                p, pk = next_pp()
                for q2 in range(2):
                    proj_fm(p[:, q2, :], pk, wb_o, h, hf * HS + q2 * 512, 512)
                A("act", lambda e, p=p, sl=sl, h=h: e.activation(out=sog[:, h, sl],
                                                                 in_=p[:].rearrange("p a b -> p (a b)"), func=AF.Silu),
                  r=[pk], w=[("sog", h, hf)])
        for i in range(0, NT, 2):
            p, pk = next_pp()
            for q2 in range(2):
                for c in range(8):
                    A("pe", lambda e, p=p, q2=q2, c=c, i=i: e.matmul(
                        p[:, q2, :], lhsT=hT[:, c, (i + q2) * 128:(i + q2 + 1) * 128], rhs=wbf[wb_v][:, c, :],
                        start=(c == 0), stop=(c == 7)),
                      r=[("wbf", wb_v), ("hT", (i + q2) // 4)], w=[pk])
            A("dve", lambda e, p=p, i=i: e.tensor_copy(out=v_tm[:, i:i + 2, :], in_=p[:]),
              r=[pk], w=[("v_tm", i), ("v_tm", i + 1)])
        sa.close()
        sb_ = Scope()
        maskU = sb_.sb("maskU", [128, 128])
        A("sp", lambda e: e.dma_start(out=maskU[:], in_=maskU_d[:, :]), w=["maskU"], dma=True)
        am = [[sb_.sb(f"am{h}_{i}", [128, 128], BF16) for i in range(2)] for h in range(4)]
        st = sb_.sb("st", [128, 4, 128])
        stb = sb_.sb("stb", [128, 4, 128], BF16)
        sq = [sb_.sb(f"sq{i}", [128, 512], BF16) for i in range(2)]
        sd = [sb_.sb(f"sd{i}", [128, 512]) for i in range(2)]
        yt = [sb_.sb(f"yt{i}", [128, 512]) for i in range(2)]
        pA = sb_.ps("pA", [128, 4, 128])
        pKV = sb_.ps("pKV", [128, 4, 128])
        pO = [sb_.ps(f"pO{h}", [128, 512]) for h in range(4)]
        pM = [sb_.ps(f"pM{i}", [128, 512]) for i in range(2)]
        nm = [0]
        for n in range(NT):
            cs = slice(n * 128, (n + 1) * 128)
            oc = slice((n % 4) * 128, (n % 4 + 1) * 128)
            for h in range(4):
                A("pe", lambda e, h=h, cs=cs: e.matmul(pA[:, h, :], lhsT=ki[:, h, cs], rhs=qd[:, h, cs],
                                                      start=True, stop=True),
                  r=[("ki", h, n // 8), ("qd", h, n // 8)], w=[("pA",)])
            for h in range(4):
                a = am[h][n % 2]
                ak = ("am", h, n % 2)
                A("dve", lambda e, h=h, a=a: e.tensor_tensor(out=a[:], in0=pA[:, h, :], in1=maskU[:], op=ALU.mult),
                  r=[("pA",), "maskU"], w=[ak])
            if n < NT - 1:
                for h in range(4):
                    A("pe", lambda e, h=h, n=n: e.matmul(pKV[:, h, :], lhsT=ks_tm[:, h, n, :],
                                                         rhs=v_tm[:, n, h * 128:(h + 1) * 128], start=True, stop=True),
                      r=[("ks_tm", h, n // 8), ("v_tm", n)], w=[("pKV",)])
            for h in range(4):
                a = am[h][n % 2]
                ak = ("am", h, n % 2)
                A("pe", lambda e, h=h, a=a, oc=oc, n=n: e.matmul(pO[h][:, oc], lhsT=v_tm[:, n, h * 128:(h + 1) * 128],
                                                               rhs=a[:], start=True, stop=(n == 0)),
                  r=[("v_tm", n), ak], w=[("pO", h)])
                if n > 0:
                    A("pe", lambda e, h=h, oc=oc, cs=cs: e.matmul(pO[h][:, oc], lhsT=stb[:, h, :], rhs=qd[:, h, cs],
                                                                 start=False, stop=True),
                      r=[("stb", h), ("qd", h, n // 8)], w=[("pO", h)])
            if n < NT - 1:
                for h in range(4):
                    if n == 0:
                        A("dve", lambda e, h=h: e.tensor_copy(out=st[:, h, :], in_=pKV[:, h, :]),
                          r=[("pKV",)], w=[("st", h)])
                    else:
                        A("dve", lambda e, h=h, n=n: e.scalar_tensor_tensor(
                            out=st[:, h, :], in0=st[:, h, :], scalar=dec[:, h, n:n + 1], in1=pKV[:, h, :],
                            op0=ALU.mult, op1=ALU.add),
                          r=[("pKV",), ("st", h), ("dec", h, n // 8)], w=[("st", h)])
                    A("act", lambda e, h=h: e.copy(out=stb[:, h, :], in_=st[:, h, :]),
                      r=[("st", h)], w=[("stb", h)])
            if n % 4 == 3:
                for h in range(4):
                    g = n // 4
                    i = nm[0] % 2
                    nm[0] += 1
                    gs = slice(g * 512, (g + 1) * 512)
                    A("act", lambda e, h=h, i=i: e.activation(out=sq[i][:], in_=pO[h][:], func=AF.Square),
                      r=[("pO", h)], w=[("sq", i)])
                    A("pe", lambda e, i=i: e.matmul(pM[i][:], lhsT=onesv[:], rhs=sq[i][:], start=True, stop=True),
                      r=[("sq", i), "onesv"], w=[("pM", i)])
                    A("act", lambda e, i=i: e.activation(out=sd[i][:], in_=pM[i][:], func=AF.Ln, bias=cst["epsc"][:, 0:1]),
                      r=[("pM", i), "epsc"], w=[("sd", i)])
                    A("act", lambda e, i=i: e.activation(out=sd[i][:], in_=sd[i][:], func=AF.Exp, scale=-0.5),
                      r=[("sd", i)], w=[("sd", i)])
                    A("dve", lambda e, h=h, i=i: e.scalar_tensor_tensor(
                        out=yt[i][:], in0=pO[h][:], scalar=hgg[:, 0:1], in1=sd[i][:], op0=ALU.mult, op1=ALU.mult),
                      r=[("pO", h), ("sd", i), "hgg"], w=[("yt", i)])
                    A("pool", lambda e, h=h, i=i, gs=gs: e.tensor_tensor(out=yaT[:, h, gs], in0=yt[i][:],
                                                                       in1=sog[:, h, gs], op=ALU.mult),
                      r=[("yt", i), ("sog", h, g // 2)], w=[("yaT", h, g)])
        sb_.close()
        so.close()

    ATT = [(128, 1), (512, 4), (2048, 16)]

    def stage_attn(s):
        wbf = ctx["wbf"]
        hT = ctx["hT"]
        ybT = ctx["ybT"]
        so = Scope()
        ND = so.sb("ND", [128, 4, S])
        def group(g, dl):
            nblk = (S // dl) // 128
            sg_ = Scope()
            QT = sg_.sb("QT", [64, 4, S], BF16)
            KT = sg_.sb("KT", [64, 4, S], BF16)
            Vg = sg_.sb("Vg", [128, 16, 4, 128], BF16)
            A("pool", lambda e: e.memset(Vg[:, :, :, 64:128], 1.0), w=[("Vg1",)])
            sa = Scope()
            sq = [sa.sb(f"asq{i}", [128, 512], BF16) for i in range(4)]
            sd = [sa.sb(f"asd{i}", [128, 512]) for i in range(4)]
            pp = [sa.ps(f"app{i}", [128, 512]) for i in range(4)]
            pM = [sa.ps(f"apM{i}", [128, 512]) for i in range(4)]
            cnt = [0]
            for which, (dst, col0, gain) in enumerate([(QT, 2048 + g * 256, "gq"), (KT, 2816 + g * 256, "gk")]):
                wb = load_wblock(col0, 256)
                for hh in range(4):
                    for tb in range(4):
                        i = cnt[0] % 4
                        p = pp[cnt[0] % 4]
                        pk = ("app", cnt[0] % 4)
                        cnt[0] += 1
                        ts_ = slice(tb * 512, (tb + 1) * 512)
                        for c in range(8):
                            A("pe", lambda e, c=c, p=p, wb=wb, hh=hh, ts_=ts_: e.matmul(
                                p[0:64, :], lhsT=wbf[wb][:, c, hh * 64:(hh + 1) * 64], rhs=hT[:, c, ts_],
                                start=(c == 0), stop=(c == 7)),
                              r=[("wbf", wb), ("hT", tb)], w=[pk])
                        A("act", lambda e, p=p, i=i: e.activation(out=sq[i][0:64, :], in_=p[0:64, :], func=AF.Square),
                          r=[pk], w=[("asq", i)])
                        A("pe", lambda e, i=i: e.matmul(pM[i][0:64, :], lhsT=cst["blk64"][0:64, 0:64], rhs=sq[i][0:64, :],
                                                        start=True, stop=True),
                          r=[("asq", i), "blk64"], w=[("apM", i)])
                        A("act", lambda e, i=i: e.activation(out=sd[i][0:64, :], in_=pM[i][0:64, :], func=AF.Ln, bias=cst["epsc"][0:64, 0:1]),
                          r=[("apM", i), "epsc"], w=[("asd", i)])
                        A("act", lambda e, i=i: e.activation(out=sd[i][0:64, :], in_=sd[i][0:64, :], func=AF.Exp, scale=-0.5),
                          r=[("asd", i)], w=[("asd", i)])
                        A("dve", lambda e, p=p, i=i, dst=dst, hh=hh, ts_=ts_, gain=gain: e.scalar_tensor_tensor(
                            out=dst[:, hh, ts_], in0=p[0:64, :], scalar=cst[gain][0:64, 0:1], in1=sd[i][0:64, :],
                            op0=ALU.mult, op1=ALU.mult),
                          r=[pk, ("asd", i), gain], w=[("QK", which, hh, tb)])
            wb = load_wblock(3584 + g * 256, 256)
            for r_ in range(dl):
                for n in range(nblk):
                    ti = r_ * nblk + n
                    t0 = r_ + dl * 128 * n
                    p = pp[cnt[0] % 4]
                    pk = ("app", cnt[0] % 4)
                    cnt[0] += 1
                    hs = slice(t0, t0 + 127 * dl + 1, dl)
                    for c in range(8):
                        A("pe", lambda e, p=p, c=c, hs=hs, wb=wb: e.matmul(
                            p[:, 0:256], lhsT=hT[:, c, hs], rhs=wbf[wb][:, c, 0:256],
                            start=(c == 0), stop=(c == 7)),
                          r=[("wbf", wb)] + [("hT", k) for k in range(4)], w=[pk])
                    A("dve", lambda e, p=p, ti=ti: e.tensor_copy(
                        out=Vg[:, ti, :, 0:64], in_=p[:, 0:256].rearrange("p (h e) -> p h e", e=64)),
                      r=[pk], w=[("Vg", ti)])
            sa.close()
            sb2 = Scope()
            ef = [sb2.sb(f"ef{i}", [128, 4, 128], BF16) for i in range(4)]
            em = [sb2.sb(f"em{i}", [128, 4, 128], BF16) for i in range(4)]
            pS = [sb2.ps(f"pS{i}", [128, 4, 128]) for i in range(4)]
            pO = [sb2.ps(f"pO{i}", [128, 4, 128]) for i in range(2)]
            kc = [0]
            allQK = [("QK", w_, jj, tb) for w_ in range(2) for jj in range(4) for tb in range(4)]

            def s_phase(r_, n):
                t0 = r_ + dl * 128 * n
                qs = slice(t0, t0 + 127 * dl + 1, dl)
                kbs = ([n - 1] if n > 0 else []) + [n]
                ems = []
                for kb in kbs:
                    i = kc[0] % 4
                    kc[0] += 1
                    k0 = r_ + dl * 128 * kb
                    ks = slice(k0, k0 + 127 * dl + 1, dl)
                    for hh in range(4):
                        A("pe", lambda e, i=i, hh=hh, ks=ks, qs=qs: e.matmul(
                            pS[i][:, hh, :], lhsT=KT[:, hh, ks], rhs=QT[:, hh, qs],
                            start=True, stop=True),
                          r=allQK, w=[("pS", i)])
                    A("act", lambda e, i=i: e.activation(out=ef[i][:], in_=pS[i][:], func=AF.Exp, scale=0.125),
                      r=[("pS", i)], w=[("ef", i)])
                    mname = "maskUb" if kb == n else "maskLb"
                    A("pool", lambda e, i=i, mname=mname: e.tensor_tensor(
                        out=em[i][:], in0=ef[i][:], in1=cst[mname][:].unsqueeze(1).to_broadcast([128, 4, 128]),
                        op=ALU.mult),
                      r=[("ef", i), mname], w=[("em", i)])
                    ems.append((i, r_ * nblk + kb))
                return qs, ems

            def pv_phase(it, qs, ems):
                o = it % 2
                for hh in range(4):
                    for idx, (i, ti) in enumerate(ems):
                        A("pe", lambda e, o=o, hh=hh, i=i, ti=ti, idx=idx, last=(idx == len(ems) - 1): e.matmul(
                            pO[o][:, hh, :], lhsT=Vg[:, ti, hh, :], rhs=em[i][:, hh, :],
                            start=(idx == 0), stop=last),
                          r=[("Vg", ti), ("Vg1",), ("em", i)], w=[("pO", o)])
                if g == 0:
                    A("act", lambda e, o=o, qs=qs: e.copy(out=ND[:, :, qs], in_=pO[o][:]),
                      r=[("pO", o)], w=["ND"])
                else:
                    A("dve", lambda e, o=o, qs=qs: e.tensor_tensor(out=ND[:, :, qs], in0=ND[:, :, qs],
                                                                  in1=pO[o][:], op=ALU.add),
                      r=[("pO", o), "ND"], w=["ND"])

            its = [(r_, n) for r_ in range(dl) for n in range(nblk)]
            prev_ = None
            for k_, (r_, n) in enumerate(its):
                cur_ = s_phase(r_, n)
                if prev_ is not None:
                    pv_phase(k_ - 1, *prev_)
                prev_ = cur_
            pv_phase(len(its) - 1, *prev_)
            sb2.close()
            sg_.close()

        for g, (win, dl) in enumerate(ATT):
            group(g, dl)
        sf = Scope()
        pB = [sf.ps(f"pB{i}", [128, 512]) for i in range(2)]
        shsel = sf.sb("shsel", [128, 64])
        A("sp", lambda e: e.dma_start(out=shsel[:], in_=shsel_d[:, :]), w=["shsel"], dma=True)
        k = 0
        for hh in range(4):
            A("act", lambda e, hh=hh: e.activation(out=ND[64:128, hh, :], in_=ND[64:128, hh, :], func=AF.Ln), r=["ND"], w=["ND"])
            A("act", lambda e, hh=hh: e.activation(out=ND[64:128, hh, :], in_=ND[64:128, hh, :], func=AF.Exp, scale=-1.0),
              r=["ND"], w=["ND"])
            for tb in range(4):
                i = k % 2
                k += 1
                ts_ = slice(tb * 512, (tb + 1) * 512)
                A("pe", lambda e, i=i, hh=hh, ts_=ts_: e.matmul(pB[i][0:64, :], lhsT=shsel[:], rhs=ND[:, hh, ts_],
                                                               start=True, stop=True),
                  r=["ND", "shsel"], w=[("pB", i)])
                A("dve", lambda e, i=i, hh=hh, ts_=ts_: e.tensor_tensor(out=ybT[:, hh, ts_], in0=ND[0:64, hh, ts_],
                                                                       in1=pB[i][0:64, :], op=ALU.mult),
                  r=["ND", ("pB", i)], w=[("ybT", hh, tb)])
        sf.close()
        so.close()

    wa_d = din("w_a", [512, D])
    wb_d = din("w_b", [256, D])
    wout_d = din("w_out", [D, D])

    def stage_merge(s, alloc_xacc):
        hT, yaT, ybT = ctx["hT"], ctx["yaT"], ctx["ybT"]
        sm = Scope()
        mT = sm.sb("mT", [128, 8, S], BF16)
        si = Scope()
        wa = si.sb("wa", [128, 4, D], BF16)
        wbb = si.sb("wbb", [64, 4, D], BF16)
        A("pool", lambda e: e.dma_start(out=wa[:], in_=wa_d.rearrange("(f p) n -> p f n", p=128)), w=["wa"], dma=True)
        A("pool", lambda e: e.dma_start(out=wbb[:], in_=wb_d.rearrange("(h e) n -> e h n", e=64)), w=["wbb"], dma=True)
        sg = [si.sb(f"sg{i}", [128, 512]) for i in range(4)]
        m12 = [si.sb(f"m12{i}", [128, 512]) for i in range(4)]
        pp = [si.ps(f"mpp{i}", [128, 512]) for i in range(4)]
        pab = [si.ps(f"mpab{i}", [128, 512]) for i in range(4)]
        k = 0
        for jb in range(2):
            wga = load_wblock(4352 + jb * 512)
            wgb = load_wblock(5376 + jb * 512)
            for jj in range(4):
                j = jb * 4 + jj
                js = slice(j * 128, (j + 1) * 128)
                for tb in range(4):
                    ts_ = slice(tb * 512, (tb + 1) * 512)
                    ia, ib = (2 * k) % 4, (2 * k + 1) % 4
                    k += 1
                    proj_fm(pp[ia][:], ("mpp", ia), wga, jj, tb * 512, 512)
                    A("act", lambda e, ia=ia: e.activation(out=sg[ia][:], in_=pp[ia][:], func=AF.Sigmoid),
                      r=[("mpp", ia)], w=[("sg", ia)])
                    for f in range(4):
                        A("pe", lambda e, ia=ia, f=f, js=js, ts_=ts_: e.matmul(
                            pab[ia][:], lhsT=wa[:, f, js], rhs=yaT[:, f, ts_], start=(f == 0), stop=(f == 3)),
                          r=["wa", "yaT"], w=[("mpab", ia)])
                    A("dve", lambda e, ia=ia: e.tensor_tensor(out=m12[ia][:], in0=pab[ia][:], in1=sg[ia][:], op=ALU.mult),
                      r=[("mpab", ia), ("sg", ia)], w=[("m12", ia)])
                    proj_fm(pp[ib][:], ("mpp", ib), wgb, jj, tb * 512, 512)
                    A("act", lambda e, ib=ib: e.activation(out=sg[ib][:], in_=pp[ib][:], func=AF.Sigmoid),
                      r=[("mpp", ib)], w=[("sg", ib)])
                    for hh in range(4):
                        A("pe", lambda e, ib=ib, hh=hh, js=js, ts_=ts_: e.matmul(
                            pab[ib][:], lhsT=wbb[:, hh, js], rhs=ybT[:, hh, ts_], start=(hh == 0), stop=(hh == 3)),
                          r=["wbb", "ybT"], w=[("mpab", ib)])
                    A("dve", lambda e, ib=ib: e.tensor_tensor(out=m12[ib][:], in0=pab[ib][:], in1=sg[ib][:], op=ALU.mult),
                      r=[("mpab", ib), ("sg", ib)], w=[("m12", ib)])
                    A("pool", lambda e, ia=ia, ib=ib, j=j, ts_=ts_: e.tensor_tensor(
                        out=mT[:, j, ts_], in0=m12[ia][:], in1=m12[ib][:], op=ALU.add),
                      r=[("m12", ia), ("m12", ib)], w=[("mT", j, tb)])
        si.close()
        alloc_xacc()
        xacc = ctx["xacc"]
        so = Scope()
        wbf = ctx["wbf"]
        wo = [load_wblock(mh * 512, 512, src=wout_d) for mh in range(2)]
        po = [so.ps(f"po{i}", [128, 512]) for i in range(4)]
        k = 0
        for i in range(NT):
            A("sp", lambda e, i=i: e.dma_start(out=xacc[:, i, :], in_=x_d[s, i * 128:(i + 1) * 128, :]),
              w=[("xacc", i)], dma=True)
            for mh in range(2):
                pi = k % 4
                k += 1
                ms_ = slice(mh * 512, (mh + 1) * 512)
                for n in range(8):
                    A("pe", lambda e, pi=pi, n=n, i=i, mh=mh: e.matmul(
                        po[pi][:], lhsT=mT[:, n, i * 128:(i + 1) * 128], rhs=wbf[wo[mh]][:, n, :],
                        start=(n == 0), stop=(n == 7)),
                      r=[("wbf", wo[mh])] + [("mT", n, i // 4)], w=[("po", pi)])
                A("dve", lambda e, pi=pi, i=i, ms_=ms_: e.tensor_tensor(out=xacc[:, i, ms_], in0=xacc[:, i, ms_],
                                                                       in1=po[pi][:], op=ALU.add),
                  r=[("po", pi), ("xacc", i)], w=[("xacc", i)])
        so.close()
        sm.close()

    wq_d = din("peer_wq", [D, D])
    KB_d = din("peer_kb", [128, 8, 256])
    KT12_d = din("peer_kt12", [128, 8, 128])
    uT_d = din("peer_uT", [D, 16384])
    v_d = din("peer_v", [16384, D])
    NEG_BIG = -1.0e30
    EGS = 512
    NEG = 16384 // EGS

    def stage_peer(s, dbg=None):
        h2T, xacc = ctx["hT"], ctx["xacc"]
        sp_ = Scope()
        qpT = sp_.sb("qpT", [128, 8, S], BF16)
        thr = sp_.sb("thr", [128, NT, 8])
        nb = sp_.sb("nb", [128, NT, 8])
        KT12 = sp_.sb("KT12", [128, 8, 128], BF16)
        A("pool", lambda e: e.dma_start(out=KT12[:], in_=KT12_d[:, :, :]), w=["KT12"], dma=True)
        sa = Scope()
        wq = sa.sb("wq", [128, 8, D], BF16)
        KB = sa.sb("KB", [128, 8, 256], BF16)
        A("pool", lambda e: e.dma_start(out=wq[:], in_=wq_d.rearrange("(c p) n -> p c n", p=128)), w=["wq"], dma=True)
        A("pool", lambda e: e.dma_start(out=KB[:], in_=KB_d[:, :, :]), w=["KB"], dma=True)
        s12 = [sa.sb(f"s12_{i}", [128, 8, 256]) for i in range(2)]
        wk = sa.sb("wk", [128, 8, 256])
        t16 = sa.sb("t16", [128, 8, 2, 16])
        cand = sa.sb("cand", [128, 8, 256])
        cand2 = sa.sb("cand2", [128, 8, 256])
        c16 = sa.sb("c16", [128, 8, 16])
        negm = sa.sb("negm", [128, 8])
        dlt = sa.sb("dlt", [128, 8, 16])
        zz = sa.sb("zz", [128, 8])
        pq = [sa.ps(f"pq{i}", [128, 512]) for i in range(2)]
        ps12 = [sa.ps(f"ps12{i}", [128, 2, 256]) for i in range(2)]
        k = 0
        for h in range(8):
            for tb in range(4):
                i = k % 2
                k += 1
                ts_ = slice(tb * 512, (tb + 1) * 512)
                for c in range(8):
                    A("pe", lambda e, i=i, c=c, h=h, ts_=ts_: e.matmul(
                        pq[i][:], lhsT=wq[:, c, h * 128:(h + 1) * 128], rhs=h2T[:, c, ts_],
                        start=(c == 0), stop=(c == 7)),
                      r=["wq", ("hT", tb)], w=[("pq", i)])
                if k % 2 == 0:
                    A("act", lambda e, i=i, h=h, ts_=ts_: e.copy(out=qpT[:, h, ts_], in_=pq[i][:]),
                      r=[("pq", i)], w=[("qpT", h, tb)])
                else:
                    A("dve", lambda e, i=i, h=h, ts_=ts_: e.tensor_copy(out=qpT[:, h, ts_], in_=pq[i][:]),
                      r=[("pq", i)], w=[("qpT", h, tb)])
        k = 0
        for i in range(NT):
            sb_ = s12[i % 2]
            sk = ("s12", i % 2)
            tk = slice(i * 128, (i + 1) * 128)
            for hp in range(4):
                pi = k % 2
                k += 1
                for hh in range(2):
                    h = hp * 2 + hh
                    A("pe", lambda e, pi=pi, hh=hh, h=h, tk=tk: e.matmul(
                        ps12[pi][:, hh, :], lhsT=qpT[:, h, tk], rhs=KB[:, h, :], start=True, stop=True),
                      r=[("qpT", h, i // 4), "KB"], w=[("ps12", pi)])
                A("act", lambda e, pi=pi, hp=hp, sb_=sb_: e.copy(out=sb_[:, hp * 2:hp * 2 + 2, :], in_=ps12[pi][:]),
                  r=[("ps12", pi)], w=[sk])
            hh_ = [(h, half, slice(half * 128, (half + 1) * 128)) for h in range(8) for half in range(2)]
            for h, half, hs_ in hh_:
                A("dve", lambda e, sb_=sb_, h=h, half=half, hs_=hs_: e.max(out=t16[:, h, half, 0:8], in_=sb_[:, h, hs_]),
                  r=[sk], w=[("t16a", h, half)])
            for h, half, hs_ in hh_:
                A("dve", lambda e, sb_=sb_, h=h, half=half, hs_=hs_: e.match_replace(
                    out=wk[:, h, hs_], in_to_replace=t16[:, h, half, 0:8], in_values=sb_[:, h, hs_],
                    imm_value=NEG_BIG),
                  r=[sk, ("t16a", h, half)], w=[("wk", h, half)])
            for h, half, hs_ in hh_:
                A("dve", lambda e, h=h, half=half, hs_=hs_: e.max(out=t16[:, h, half, 8:16], in_=wk[:, h, hs_]),
                  r=[("wk", h, half)], w=[("t16b", h, half)])
            for h in range(8):
                A("pool", lambda e, h=h: e.tensor_tensor(
                    out=cand[:, h, :].rearrange("p (a b) -> p a b", b=16),
                    in0=t16[:, h, 0, :].unsqueeze(2).to_broadcast([128, 16, 16]),
                    in1=t16[:, h, 1, :].unsqueeze(1).to_broadcast([128, 16, 16]), op=ALU.add),
                  r=[("t16a", h, 0), ("t16a", h, 1), ("t16b", h, 0), ("t16b", h, 1)], w=[("cand", h)])
            for h in range(8):
                A("dve", lambda e, h=h: e.max(out=c16[:, h, 0:8], in_=cand[:, h, :]), r=[("cand", h)], w=[("c16a", h)])
            for h in range(8):
                A("dve", lambda e, h=h: e.match_replace(out=cand2[:, h, :], in_to_replace=c16[:, h, 0:8],
                                                        in_values=cand[:, h, :], imm_value=NEG_BIG),
                  r=[("cand", h), ("c16a", h)], w=[("cand2", h)])
            for h in range(8):
                A("dve", lambda e, h=h: e.max(out=c16[:, h, 8:16], in_=cand2[:, h, :]), r=[("cand2", h)], w=[("c16b", h)])
            c16k = [("c16a", h) for h in range(8)] + [("c16b", h) for h in range(8)]
            A("dve", lambda e: e.tensor_scalar(out=negm[:], in0=c16[:, :, 0], scalar1=-1.0, scalar2=None, op0=ALU.mult),
              r=c16k, w=["negm"])
            A("dve", lambda e, i=i: e.tensor_scalar(out=thr[:, i, :], in0=c16[:, :, 15], scalar1=-1.0e-4, scalar2=None,
                                                    op0=ALU.add),
              r=c16k, w=[("thr", i)])
            A("dve", lambda e: e.tensor_tensor(out=dlt[:], in0=c16[:], in1=negm[:].unsqueeze(2).to_broadcast([128, 8, 16]),
                                               op=ALU.add),
              r=c16k + ["negm"], w=["dlt"])
            A("act", lambda e: e.activation(out=dlt[:], in_=dlt[:], func=AF.Exp), r=["dlt"], w=["dlt"])
            A("dve", lambda e: e.reduce_sum(out=zz[:], in_=dlt[:], axis=AX.X), r=["dlt"], w=["zz"])
            A("act", lambda e: e.activation(out=zz[:], in_=zz[:], func=AF.Ln), r=["zz"], w=["zz"])
            A("dve", lambda e, i=i: e.tensor_sub(out=nb[:, i, :], in0=negm[:], in1=zz[:]),
              r=["negm", "zz"], w=[("nb", i)])
        if dbg is not None:
            dthr, dnb, dq = dbg
            A("sp", lambda e: e.dma_start(out=dthr[:, :, :], in_=thr[:]), r=[("thr", i) for i in range(NT)], dma=True)
            A("sp", lambda e: e.dma_start(out=dnb[:, :, :], in_=nb[:]), r=[("nb", i) for i in range(NT)], dma=True)
            A("sp", lambda e: e.dma_start(out=dq[:, :, :], in_=qpT[:]),
              r=[("qpT", h, tb) for h in range(8) for tb in range(4)], dma=True)
        sa.close()
        sm = Scope()
        FKT = sm.sb("FKT", [128, 8, EGS], BF16)
        uT = [sm.sb(f"uT{i}", [128, 8, EGS], BF16) for i in range(2)]
        vv = [sm.sb(f"vv{i}", [128, 4, D], BF16) for i in range(2)]
        G = [sm.sb(f"G{i}", [128, 4, 512], BF16) for i in range(2)]
        E = [sm.sb(f"E{i}", [128, 512], BF16) for i in range(4)]
        EmE = [[sm.sb(f"EmE{j}_{k}", [128, 512], BF16) for k in range(4)] for j in range(2)]
        EmO = [sm.sb(f"EmO{k}", [128, 512], BF16) for k in range(4)]
        AT = [sm.sb(f"AT{i}", [128, 4, 128], BF16) for i in range(2)]
        pH = [sm.ps(f"pH{i}", [128, 512]) for i in range(1)]
        pS = [sm.ps(f"pS{i}", [128, 512]) for i in range(4)]
        pW = [sm.ps(f"pW{i}", [128, 4, 128]) for i in range(1)]
        pOut = [sm.ps(f"pOut{i}", [128, 512]) for i in range(2)]
        Gpre = sm.sb("Gpre", [128, 4, 512], BF16)
        A("pool", lambda e: e.tensor_copy(
            out=FKT[64:128, :, :].rearrange("p h (c i) -> p h c i", i=128),
            in_=KT12[64:128, :, :].unsqueeze(2).to_broadcast([64, 8, 4, 128])),
          r=["KT12"], w=[("FKTb",)])
        cn = {"H": 0, "S": 0, "E": 0, "O": 0}

        def load_group(eg):
            b = eg % 2
            A("pool", lambda e: e.dma_start(
                out=uT[b][:], in_=uT_d[:, eg * EGS:(eg + 1) * EGS].rearrange("(c p) n -> p c n", p=128)),
              w=[("uT", b)], dma=True)
            A("pool", lambda e: e.dma_start(
                out=vv[b][:], in_=v_d[eg * EGS:(eg + 1) * EGS, :].rearrange("(c p) n -> p c n", p=128)),
              w=[("vv", b)], dma=True)

        def fkt_group(eg):
            for hq in range(2):
                A("dve", lambda e, hq=hq: e.tensor_copy(
                    out=FKT[0:64, hq * 4:(hq + 1) * 4, :].rearrange("p h (c i) -> p h c i", i=128),
                    in_=KT12[0:64, hq * 4:(hq + 1) * 4, eg * 4:(eg + 1) * 4].unsqueeze(3).to_broadcast([64, 4, 4, 128])),
                  r=["KT12"], w=[("FKTt", h) for h in range(hq * 4, (hq + 1) * 4)])

        def h_mm(gs, cc, c):
            eg, sbk = gs // 4, gs % 4
            b = eg % 2
            A("pe", lambda e: e.matmul(
                pH[0][:], lhsT=uT[b][:, c, cc * 128:(cc + 1) * 128],
                rhs=h2T[:, c, sbk * 512:(sbk + 1) * 512], start=(c == 0), stop=(c == 7)),
              r=[("uT", b), ("hT", sbk)], w=[("pH", 0)])

        def h_copy(cc):
            if cc % 2 == 0:
                A("act", lambda e: e.copy(out=Gpre[:, cc, :], in_=pH[0][:]), r=[("pH", 0)], w=[("Gpre", cc)])
            else:
                A("dve", lambda e: e.tensor_copy(out=Gpre[:, cc, :], in_=pH[0][:]), r=[("pH", 0)], w=[("Gpre", cc)])

        def gelu_all(gs):
            A("act", lambda e: e.activation(out=G[gs % 2][:], in_=Gpre[:], func=AF.Gelu),
              r=[("Gpre", cc) for cc in range(4)], w=[("G", gs % 2)])

        def s_head(u, h):
            eg, i = u // NT, u % NT
            tk = slice(i * 128, (i + 1) * 128)
            p_ = cn["S"] % 4
            cn["S"] += 1
            x_ = cn["E"] % 4
            cn["E"] += 1
            es_ = u % 2
            A("pe", lambda e: e.matmul(pS[p_][:], lhsT=qpT[:, h, tk], rhs=FKT[:, h, :], start=True, stop=True),
              r=[("qpT", h, i // 4), ("FKTb",), ("FKTt", h)], w=[("pS", p_)])
            A("act", lambda e: e.activation(out=E[x_][:], in_=pS[p_][:], func=AF.Exp, bias=nb[:, i, h:h + 1]),
              r=[("pS", p_), ("nb", i)], w=[("E", x_)])
            if h % 2 == 0:
                dst, dk = EmE[es_][h // 2], ("EmE", es_, h // 2)
            else:
                dst, dk = EmO[h // 2], ("EmO", h // 2)
            A("dve", lambda e: e.scalar_tensor_tensor(
                out=dst[:], in0=pS[p_][:], scalar=thr[:, i, h:h + 1], in1=E[x_][:],
                op0=ALU.is_ge, op1=ALU.mult),
              r=[("pS", p_), ("E", x_), ("thr", i)], w=[dk])
            if h % 2 == 1:
                pk_ = ("EmE", es_, h // 2)
                A("pool", lambda e: e.tensor_tensor(out=EmE[es_][h // 2][:], in0=EmE[es_][h // 2][:], in1=dst[:],
                                                    op=ALU.add),
                  r=[pk_, dk], w=[pk_])

        def t_piece(u, slot):
            es_ = u % 2
            cc = slot // 2
            for pr in range((slot % 2) * 2, (slot % 2) * 2 + 2):
                A("pe", lambda e, pr=pr: e.matmul(
                    pW[0][:, cc, :], lhsT=EmE[es_][pr][:, cc * 128:(cc + 1) * 128], rhs=ident[:],
                    start=(pr == 0), stop=(pr == 3)),
                  r=[("EmE", es_, pr), "ident"], w=[("pW", 0)])

        def at_piece(u):
            eg, i = u // NT, u % NT
            gs, tt = eg * 4 + i // 4, i % 4
            w_ = u % 2
            A("dve", lambda e: e.tensor_tensor(
                out=AT[w_][:], in0=pW[0][:], in1=G[gs % 2][:, :, tt * 128:(tt + 1) * 128], op=ALU.mult),
              r=[("pW", 0), ("G", gs % 2)], w=[("AT", w_)])

        def o_mm(u, k):
            eg, i = u // NT, u % NT
            b = eg % 2
            w_ = u % 2
            mh, cc = k // 4, k % 4
            ms_ = slice(mh * 512, (mh + 1) * 512)
            A("pe", lambda e: e.matmul(
                pOut[mh][:], lhsT=AT[w_][:, cc, :], rhs=vv[b][:, cc, ms_], start=(cc == 0), stop=(cc == 3)),
              r=[("AT", w_), ("vv", b)], w=[("pOut", mh)])

        tmpO = sm.sb("tmpO", [128, 512])

        def add_piece(u, mh):
            eg, i = u // NT, u % NT
            ms_ = slice(mh * 512, (mh + 1) * 512)
            if mh == 1:
                A("act", lambda e: e.copy(out=tmpO[:], in_=pOut[1][:]), r=[("pOut", 1)], w=["tmpO"])
                if eg < NEG - 1:
                    A("pool", lambda e: e.tensor_tensor(out=xacc[:, i, ms_], in0=xacc[:, i, ms_], in1=tmpO[:], op=ALU.add),
                      r=["tmpO", ("xacc", i)], w=[("xacc", i)])
                else:
                    A("pool", lambda e: e.tensor_tensor(out=ostage[:], in0=xacc[:, i, ms_], in1=tmpO[:], op=ALU.add),
                      r=["tmpO", ("xacc", i)], w=["ostage"])
                    A("sp", lambda e: e.dma_start(out=out_d[s, i * 128:(i + 1) * 128, ms_], in_=ostage[:]),
                      r=["ostage"], dma=True)
                return
            if eg < NEG - 1:
                A("dve", lambda e: e.tensor_tensor(out=xacc[:, i, ms_], in0=xacc[:, i, ms_], in1=pOut[mh][:], op=ALU.add),
                  r=[("pOut", mh), ("xacc", i)], w=[("xacc", i)])
            else:
                A("dve", lambda e: e.tensor_tensor(out=ostage[:], in0=xacc[:, i, ms_], in1=pOut[mh][:], op=ALU.add),
                  r=[("pOut", mh), ("xacc", i)], w=["ostage"])
                A("sp", lambda e: e.dma_start(out=out_d[s, i * 128:(i + 1) * 128, ms_], in_=ostage[:]),
                  r=["ostage"], dma=True)

        NU = NEG * NT
        load_group(0)
        load_group(1)
        for cc in range(4):
            for c in range(8):
                h_mm(0, cc, c)
            h_copy(cc)
        gelu_all(0)
        for v in range(NU + 3):
            cur = v < NU
            tv = v - 1 if 0 <= v - 1 < NU else None
            oa = v - 2 if 0 <= v - 2 < NU else None
            ob = v - 3 if 0 <= v - 3 < NU else None
            hc = None
            if cur:
                eg, i = v // NT, v % NT
                if i == 0:
                    fkt_group(eg)
                if i == 4 and 1 <= eg < NEG - 1:
                    load_group(eg + 1)
                gs, tt = eg * 4 + i // 4, i % 4
                if gs + 1 < NEG * 4:
                    hc = (gs + 1, tt)
            hk = 0
            for h in range(8):
                if cur:
                    s_head(v, h)
                if h < 4 and ob is not None:
                    o_mm(ob, 4 + h)
                if h >= 4 and oa is not None:
                    o_mm(oa, h - 4)
                if hc is not None and h >= 1:
                    h_mm(hc[0], hc[1], hk)
                    hk += 1
                    if h == 7:
                        h_mm(hc[0], hc[1], hk)
                if tv is not None:
                    t_piece(tv, h)
                if ob is not None and h == 4:
                    add_piece(ob, 1)
            if hc is not None:
                h_copy(hc[1])
                if hc[1] == 3:
                    gelu_all(hc[0])
            if tv is not None:
                at_piece(tv)
            if oa is not None:
                add_piece(oa, 0)
        sm.close()
        sp_.close()

    def run_sequence(s, upto="all"):
        sq_ = Scope()
        ctx["hT"] = sq_.sb("hT", [128, 8, S], BF16, side="right")
        sy = Scope()
        ctx["wbf"] = [sy.sb(f"wbf{i}", [128, 8, 512], BF16) for i in range(3)]
        stage_norm(s, "x")
        ctx["yaT"] = sy.sb("yaT", [128, 4, S], BF16)
        stage_hgrn(s)
        if upto == "hgrn":
            return
        ctx["ybT"] = sy.sb("ybT", [64, 4, S], BF16)
        stage_attn(s)
        if upto == "attn":
            return
        def alloc_xacc():
            ctx["xacc"] = sq_.sb("xacc", [128, NT, D], F32, side="right")

        stage_merge(s, alloc_xacc)
        sy.close()
        if upto == "merge":
            return
        stage_norm(s, "xacc")
        if upto == "norm2":
            return
        if upto == "peerprep":
            dthr = dbg_out("thr", [128, NT, 8])
            dnb = dbg_out("nb", [128, NT, 8])
            dq = dbg_out("qpT", [128, 8, S], BF16)
            stage_peer(s, dbg=(dthr, dnb, dq))
        else:
            stage_peer(s)
        sq_.close()

    if debug == "hgrn":
        run_sequence(0, "hgrn")
        dbg_ya = dbg_out("yaT", [128, 4, S], BF16)
        A("sp", lambda e: e.dma_start(out=dbg_ya[:, :, :], in_=ctx["yaT"][:]), dma=True)
    elif debug == "attn":
        run_sequence(0, "attn")
        dbg_ya = dbg_out("yaT", [128, 4, S], BF16)
        A("sp", lambda e: e.dma_start(out=dbg_ya[:, :, :], in_=ctx["yaT"][:]), dma=True)
        dbg_yb = dbg_out("ybT", [64, 4, S], BF16)
        A("sp", lambda e: e.dma_start(out=dbg_yb[:, :, :], in_=ctx["ybT"][:]), dma=True)
    elif debug in ("peerprep", "seq0"):
        run_sequence(0, debug)
    elif debug is None:
        for s_ in range(nseq):
            run_sequence(s_)
    elif debug == "norm2":
        run_sequence(0, "norm2")
        dbg_x1 = dbg_out("x1", [128, NT, D])
        A("sp", lambda e: e.dma_start(out=dbg_x1[:, :, :], in_=ctx["xacc"][:]), dma=True)
        dbg_h2 = dbg_out("h2T", [128, 8, S], BF16)
        A("sp", lambda e: e.dma_start(out=dbg_h2[:, :, :], in_=ctx["hT"][:]), dma=True)

    sc.barrier()

    with ExitStack() as es2:
        sems = {e: es2.enter_context(nc.semaphore("sem_" + e)) for e in Sched.ENGS}
        dma_sems = {e: [es2.enter_context(nc.semaphore(f"dsem_{e}{i}")) for i in range(8)] for e in ("sp", "pool", "act")}
        sc.finalize(sems, dma_sems)
        with nc.Block() as block:
            @block.sync
            def _(E):
                sc.emit_engine("sp", E)

            @block.scalar
            def _(E):
                sc.emit_engine("act", E)

            @block.vector
            def _(E):
                sc.emit_engine("dve", E)

            @block.gpsimd
            def _(E):
                sc.emit_engine("pool", E)

            @block.tensor
            def _(E):
                sc.emit_engine("pe", E)
    try:
        es.close()
    except AssertionError:
        if debug is None:
            raise
    return nc, dbg_outs


def host_consts():
    bf = ml_dtypes.bfloat16
    c = {}
    c["ident"] = np.eye(128, dtype=np.float32).astype(bf)
    i = np.arange(128)
    c["maskU"] = (i[:, None] <= i[None, :]).astype(np.float32)
    c["onesv"] = np.full((128, 128), 1.0 / 128, np.float32).astype(bf)
    sm = np.ones((128, 1024), np.float32)
    sm[:, ::128] = 0.0
    c["scanm"] = sm
    b64 = np.zeros((128, 128), np.float32)
    b64[:64, :64] = 1.0 / 64
    b64[64:, 64:] = 1.0 / 64
    c["blk64"] = b64.astype(bf)
    c["ones64"] = np.ones((128, 64), np.float32).astype(bf)
    c["epsc"] = np.full((128, 1), EPS, np.float32)
    sh = np.zeros((128, 64), np.float32)
    sh[64 + np.arange(64), np.arange(64)] = 1.0
    c["shsel"] = sh
    c["maskUb"] = (i[:, None] <= i[None, :]).astype(np.float32).astype(bf)
    c["maskLb"] = (i[:, None] >= i[None, :]).astype(np.float32).astype(bf)
    return c


def make_in_maps(inputs, nseq=NSEQ, ncores=NCORES):
    x = np.ascontiguousarray(inputs["x"], dtype=np.float32)
    consts = host_consts()
    w_in = np.ascontiguousarray(inputs["w_in"][0], dtype=np.float32)
    g1bc = np.ascontiguousarray(np.broadcast_to(inputs["norm1_g"][0][None, :], (128, D)), dtype=np.float32)
    g2bc = np.ascontiguousarray(np.broadcast_to(inputs["norm2_g"][0][None, :], (128, D)), dtype=np.float32)
    w_a = np.ascontiguousarray(inputs["w_branch_a"][0], dtype=np.float32)
    w_b = np.ascontiguousarray(inputs["w_branch_b"][0], dtype=np.float32)
    w_out = np.ascontiguousarray(inputs["w_out"][0], dtype=np.float32)
    lg = np.asarray(inputs["hg_lb_logits"], dtype=np.float32)
    lbl = np.ascontiguousarray(np.concatenate([lg[0].reshape(4, 128).T, lg[1].reshape(4, 128).T], axis=1))
    hgg = np.ascontiguousarray(inputs["hg_norm_g"][0].reshape(128, 1), dtype=np.float32)
    gq = np.ascontiguousarray(np.tile(inputs["q_norm_g"][0], 2).reshape(128, 1), dtype=np.float32)
    gk = np.ascontiguousarray(np.tile(inputs["k_norm_g"][0], 2).reshape(128, 1), dtype=np.float32)
    wq = np.ascontiguousarray(inputs["peer_wq"][0], dtype=np.float32)
    sk = np.asarray(inputs["peer_subkeys"][0], dtype=np.float32)
    kt12 = np.ascontiguousarray(np.concatenate([sk[:, 0].transpose(2, 0, 1), sk[:, 1].transpose(2, 0, 1)], axis=0))
    kb = np.zeros((128, 8, 256), np.float32)
    kb[0:64, :, 0:128] = kt12[0:64]
    kb[64:128, :, 128:256] = kt12[64:128]
    uT = np.ascontiguousarray(inputs["peer_u"][0].T, dtype=np.float32)
    pv = np.ascontiguousarray(inputs["peer_v"][0], dtype=np.float32)
    maps = []
    for c in range(ncores):
        m = {"x": x[c * nseq:(c + 1) * nseq], "w_in": w_in, "g1bc": g1bc, "g2bc": g2bc, "lbl": lbl, "hgg": hgg,
             "gq": gq, "gk": gk, "w_a": w_a, "w_b": w_b, "w_out": w_out,
             "peer_wq": wq, "peer_kb": kb, "peer_kt12": kt12, "peer_uT": uT, "peer_v": pv}
        m.update(consts)
        maps.append(m)
    return maps


def kernel(**inputs):
    nc, _ = build_program()
    in_maps = make_in_maps(inputs)
    res = run_bass_kernel_spmd(nc, in_maps, core_ids=list(range(NCORES)))
    return np.concatenate([r["out"] for r in res.results], axis=0)
```

```python
import numpy as np
import ml_dtypes
import concourse.bass as bass
import concourse.mybir as mybir
from concourse.bass_utils import run_bass_kernel_spmd

F32 = mybir.dt.float32
BF16 = mybir.dt.bfloat16
AF = mybir.ActivationFunctionType
ALU = mybir.AluOpType
AX = mybir.AxisListType

NCORES = 8
D = 1024
S = 2048
NSEQ = 2
NT = S // 128
EPS = 1e-6
INC = 6400


class _Op:
    __slots__ = ("id", "eng", "fn", "deps", "dma", "needed", "sem", "val", "prewait", "waits")

    def __init__(self, id, eng, fn, dma):
        self.id = id
        self.eng = eng
        self.fn = fn
        self.deps = {}
        self.dma = dma
        self.needed = False
        self.sem = None
        self.val = 0
        self.prewait = None
        self.waits = []


class Sched:
    ENGS = ("pe", "act", "dve", "pool", "sp")

    def __init__(self):
        self.ops = []
        self.lastw = {}
        self.readers = {}
        self.last_eng_op = {}
        self.dma_since_barrier = []

    def add(self, eng, fn, reads=(), writes=(), dma=False):
        op = _Op(len(self.ops), eng, fn, dma)
        deps = op.deps
        for k in reads:
            w = self.lastw.get(k)
            if w is not None:
                deps[w] = True
        for k in writes:
            w = self.lastw.get(k)
            if w is not None:
                deps.setdefault(w, False)
            rd = self.readers.get(k)
            if rd:
                for r in rd.values():
                    deps.setdefault(r, False)
        for k in writes:
            self.lastw[k] = op.id
            self.readers[k] = {}
        for k in reads:
            rd = self.readers.setdefault(k, {})
            rd[("dma", op.id) if dma else eng] = op.id
        for d in list(deps):
            p = self.ops[d]
            if p.dma or dma:
                continue
            if p.eng == eng and (eng == "pe" or not deps[d]):
                del deps[d]
        self.ops.append(op)
        if dma:
            self.dma_since_barrier.append(op.id)
        elif fn is not None:
            self.last_eng_op[eng] = op.id
        return op

    def barrier(self):
        targets = list(self.last_eng_op.values()) + list(self.dma_since_barrier)
        for e in self.ENGS:
            op = _Op(len(self.ops), e, None, False)
            for t in targets:
                op.deps[t] = True
            self.ops.append(op)
        self.lastw = {}
        self.readers = {}
        self.dma_since_barrier = []

    def finalize(self, sems, dma_sems):
        for op in self.ops:
            for d in op.deps:
                self.ops[d].needed = True
        cnt = {e: 0 for e in self.ENGS}
        dcnt = {e: 0 for e in self.ENGS}
        for op in self.ops:
            if op.dma:
                j = dcnt[op.eng]
                dcnt[op.eng] += 1
                pool = dma_sems[op.eng]
                op.sem = pool[j % len(pool)]
                op.val = 16 * (j // len(pool) + 1)
                if op.val > 16:
                    op.prewait = (op.sem, op.val - 16)
            elif op.needed:
                cnt[op.eng] += 1
                op.sem = sems[op.eng]
                op.val = cnt[op.eng]
        know = {e: {} for e in self.ENGS}
        prod_know = {}
        for op in self.ops:
            K = know[op.eng]
            need = {}
            for d in sorted(op.deps, reverse=True):
                p = self.ops[d]
                key = id(p.sem)
                if key not in need or need[key][1] < p.val:
                    need[key] = (p.sem, p.val)
            if op.prewait is not None:
                key = id(op.prewait[0])
                if key not in need or need[key][1] < op.prewait[1]:
                    need[key] = op.prewait
            waits = []
            for key, (sm, v) in need.items():
                if K.get(key, 0) >= v:
                    continue
                waits.append((sm, v))
                K[key] = v
                pk = prod_know.get((key, v))
                if pk:
                    for k2, v2 in pk.items():
                        if K.get(k2, 0) < v2:
                            K[k2] = v2
            op.waits = waits
            if op.sem is not None and (op.dma or op.needed):
                snap = dict(K)
                if not op.dma:
                    snap[id(op.sem)] = op.val
                prod_know[(id(op.sem), op.val)] = snap

    def emit_engine(self, eng, E):
        for op in self.ops:
            if op.eng != eng:
                continue
            for (sm, v) in op.waits:
                E.wait_ge(sm, v)
            if op.fn is None:
                continue
            ins = op.fn(E)
            if op.dma:
                ins.then_inc(op.sem, 16)
            elif op.needed:
                ins.then_inc(op.sem, 1)


def build_program(debug=None, nseq=NSEQ):
    from contextlib import ExitStack

    nc = bass.Bass("TRN2", target_bir_lowering=False)
    sc = Sched()
    dbg_outs = {}

    def din(name, shape, dt=F32):
        return nc.dram_tensor(name, list(shape), dt, kind="ExternalInput").ap()

    x_d = din("x", [nseq, S, D])
    win_d = din("w_in", [D, INC])
    out_d = nc.dram_tensor("out", [nseq, S, D], F32, kind="ExternalOutput").ap()

    def dbg_out(name, shape, dt=F32):
        t = nc.dram_tensor("dbg_" + name, list(shape), dt, kind="ExternalOutput").ap()
        dbg_outs[name] = t
        return t

    es = ExitStack()

    def sb(name, shape, dt=F32):
        return es.enter_context(nc.sbuf_tensor("sb_" + name, list(shape), dt))

    def ps(name, shape, dt=F32):
        return es.enter_context(nc.psum_tensor("ps_" + name, list(shape), dt))

    def A(eng, fn, r=(), w=(), dma=False):
        return sc.add(eng, fn, r, w, dma)

    uniq = [0]

    class Scope:
        def __init__(self):
            self.es = ExitStack()

        def sb(self, name, shape, dt=F32, side=None):
            uniq[0] += 1
            return self.es.enter_context(nc.sbuf_tensor(f"sb_{name}_{uniq[0]}", list(shape), dt, side=side))

        def ps(self, name, shape, dt=F32):
            uniq[0] += 1
            return self.es.enter_context(nc.psum_tensor(f"ps_{name}_{uniq[0]}", list(shape), dt))

        def close(self):
            sc.barrier()
            self.es.close()

    cst = {}

    def load_const(name, shape, dt=F32):
        d = din(name, shape, dt)
        t = sb("c_" + name, shape, dt)
        if len(shape) == 2:
            A("sp", lambda e: e.dma_start(out=t[:], in_=d[:, :]), w=[name], dma=True)
        else:
            A("sp", lambda e: e.dma_start(out=t[:], in_=d[:, :, :]), w=[name], dma=True)
        cst[name] = t
        return t

    ident = load_const("ident", [128, 128], BF16)
    maskU_d = din("maskU", [128, 128])
    onesv = load_const("onesv", [128, 128], BF16)
    scanm_d = din("scanm", [128, 1024])
    lbl = load_const("lbl", [128, 8])
    hgg = load_const("hgg", [128, 1])
    load_const("blk64", [128, 128], BF16)
    load_const("ones64", [128, 64], BF16)
    load_const("maskUb", [128, 128], BF16)
    load_const("maskLb", [128, 128], BF16)
    shsel_d = din("shsel", [128, 64])
    load_const("epsc", [128, 1])
    load_const("gq", [128, 1])
    load_const("gk", [128, 1])
    lbc = sb("lbc", [128, 12])
    A("dve", lambda e: e.tensor_sub(out=lbc[:, 8:12], in0=lbl[:, 0:4], in1=lbl[:, 4:8]), r=["lbl"], w=["lbtmp"])
    A("act", lambda e: e.activation(out=lbc[:, 0:4], in_=lbc[:, 8:12], func=AF.Sigmoid), r=["lbtmp"], w=["lb0"])
    A("dve", lambda e: e.tensor_scalar(out=lbc[:, 4:8], in0=lbc[:, 0:4], scalar1=-1.0, scalar2=1.0,
                                       op0=ALU.mult, op1=ALU.add), r=["lb0"], w=["lb"])

    g1bc_d = din("g1bc", [128, D])
    g2bc_d = din("g2bc", [128, D])
    ctx = {}
    ostage = sb("ostage", [128, 512])

    def stage_norm(s, src):
        hT = ctx["hT"]
        sp_ = Scope()
        gbc = sp_.sb("gbc", [128, D])
        gsrc = g1bc_d if src == "x" else g2bc_d
        A("sp", lambda e: e.dma_start(out=gbc[:], in_=gsrc[:, :]), w=["gbc"], dma=True)
        xt = [sp_.sb(f"xt{i}", [128, D]) for i in range(2)]
        junk = sp_.sb("junk", [128, D], BF16)
        xn = [sp_.sb(f"xn{i}", [128, D], BF16) for i in range(2)]
        stat = sp_.sb("stat", [128, 4, 4])
        pT = [sp_.ps(f"pT{i}", [128, 8, 128], BF16) for i in range(2)]
        def stage_a(i):
            b = i % 2
            if src == "x":
                A("sp", lambda e: e.dma_start(out=xt[b][:], in_=x_d[s, i * 128:(i + 1) * 128, :]),
                  w=[("xt", b)], dma=True)
                xin = xt[b][:]
                xk = ("xt", b)
            else:
                xin = ctx["xacc"][:, i, :]
                xk = ("xacc", i)
            q4 = i % 4
            A("act", lambda e: e.activation(out=junk[:], in_=xin, func=AF.Square, accum_out=stat[:, q4, 0:1]),
              r=[xk], w=[("stat", q4, 0)])
            A("act", lambda e: e.activation(out=stat[:, q4, 1:2], in_=stat[:, q4, 0:1], func=AF.Sqrt,
                                            bias=EPS, scale=1.0 / D),
              r=[("stat", q4, 0)], w=[("stat", q4, 1)])
            A("dve", lambda e: e.reciprocal(out=stat[:, q4, 2:3], in_=stat[:, q4, 1:2]),
              r=[("stat", q4, 1)], w=[("stat", q4, 2)])
            A("dve", lambda e: e.scalar_tensor_tensor(
                out=xn[b][:], in0=xin, scalar=stat[:, q4, 2:3], in1=gbc[:], op0=ALU.mult, op1=ALU.mult),
              r=[xk, ("stat", q4, 2), "gbc"], w=[("xn", b)])
            for c in range(8):
                A("pe", lambda e, c=c: e.transpose(out=pT[b][:, c, :], in_=xn[b][:, c * 128:(c + 1) * 128],
                                                   identity=ident[:]),
                  r=[("xn", b), "ident"], w=[("pT", b)])

        def stage_b(i):
            b = i % 2
            A("act", lambda e: e.copy(out=hT[:, :, i * 128:(i + 1) * 128], in_=pT[b][:]),
              r=[("pT", b)], w=[("hT", i // 4)])

        stage_a(0)
        for i in range(NT):
            if i + 1 < NT:
                stage_a(i + 1)
            stage_b(i)
        sp_.close()

    wload_n = [0]

    def load_wblock(col0, ncols=512, src=None):
        src = win_d if src is None else src
        wbf = ctx["wbf"]
        b = wload_n[0] % 3
        wload_n[0] += 1
        A("pool", lambda e: e.dma_start(out=wbf[b][:, :, :ncols],
                                        in_=src[:, col0:col0 + ncols].rearrange("(c p) n -> p c n", p=128)),
          w=[("wbf", b)], dma=True)
        return b

    def proj_fm(pout, pkey, wb, j, t0, nt):
        hT = ctx["hT"]
        wbf = ctx["wbf"]
        for c in range(8):
            A("pe", lambda e, c=c: e.matmul(pout, lhsT=wbf[wb][:, c, j * 128:(j + 1) * 128],
                                            rhs=hT[:, c, t0:t0 + nt], start=(c == 0), stop=(c == 7)),
              r=[("wbf", wb)] + [("hT", k) for k in range(t0 // 512, (t0 + nt - 1) // 512 + 1)], w=[pkey])

    HS = 1024

    def stage_hgrn(s):
        wbf = ctx["wbf"]
        hT = ctx["hT"]
        yaT = ctx["yaT"]
        so = Scope()
        qd = so.sb("qd", [128, 4, S], BF16)
        ki = so.sb("ki", [128, 4, S], BF16)
        ks_tm = so.sb("ks_tm", [128, 4, NT, 128], BF16)
        v_tm = so.sb("v_tm", [128, NT, 512], BF16)
        sog = so.sb("sog", [128, 4, S], BF16)
        dec = so.sb("dec", [128, 4, NT])
        sa = Scope()
        t1 = sa.sb("t1", [128, S])
        t2 = sa.sb("t2", [128, S])
        Gt = sa.sb("G", [128, S])
        eG = sa.sb("eG", [128, S])
        kst = sa.sb("kst", [128, S], BF16)
        scanm = sa.sb("scanm", [128, 1024])
        A("sp", lambda e: e.dma_start(out=scanm[:], in_=scanm_d[:, :]), w=["scanm"], dma=True)
        pp = [sa.ps(f"pp{i}", [128, 2, 512]) for i in range(3)]
        pTk = sa.ps("pTk", [128, 16, 128], BF16)
        ppn = [0]

        def next_pp():
            i = ppn[0] % 3
            ppn[0] += 1
            return pp[i], ("pp", i)

        wb_f = load_wblock(512)
        wb_q = load_wblock(0)
        tq = sa.sb("tq", [128, S])
        HF = (0, 1)
        SL = [slice(hf * HS, (hf + 1) * HS) for hf in HF]
        for h in range(4):
            pf, pq_ = {}, {}
            for hf in HF:
                pf[hf] = next_pp()
                for q2 in range(2):
                    proj_fm(pf[hf][0][:, q2, :], pf[hf][1], wb_f, h, hf * HS + q2 * 512, 512)
            for hf in HF:
                p, pk = pf[hf]
                A("act", lambda e, p=p, sl=SL[hf]: e.activation(out=t1[:, sl], in_=p[:].rearrange("p a b -> p (a b)"),
                                                                func=AF.Sigmoid), r=[pk], w=[("t1", hf)])
            for hf in HF:
                pq_[hf] = next_pp()
                for q2 in range(2):
                    proj_fm(pq_[hf][0][:, q2, :], pq_[hf][1], wb_q, h, hf * HS + q2 * 512, 512)
                p, pk = pq_[hf]
                A("act", lambda e, p=p, sl=SL[hf]: e.activation(out=tq[:, sl], in_=p[:].rearrange("p a b -> p (a b)"),
                                                                func=AF.Sigmoid), r=[pk], w=[("tq", hf)])
            for hf in HF:
                A("dve", lambda e, sl=SL[hf], h=h: e.tensor_scalar(out=t2[:, sl], in0=t1[:, sl], scalar1=lbc[:, 4 + h:5 + h],
                                                                   scalar2=lbc[:, h:h + 1], op0=ALU.mult, op1=ALU.add),
                  r=[("t1", hf), "lb", "lb0"], w=[("t2", hf)])
            for hf in HF:
                A("act", lambda e, sl=SL[hf]: e.activation(out=t1[:, sl], in_=t2[:, sl], func=AF.Ln),
                  r=[("t2", hf)], w=[("t1", hf)])
            for hf in HF:
                A("dve", lambda e, sl=SL[hf]: e.tensor_tensor_scan(out=Gt[:, sl], data0=scanm[:], data1=t1[:, sl],
                                                                   initial=0.0, op0=ALU.mult, op1=ALU.add),
                  r=[("t1", hf), "scanm"], w=[("G", hf)])
                A("pool", lambda e, sl=SL[hf]: e.tensor_scalar(out=t2[:, sl], in0=t2[:, sl], scalar1=-1.0, scalar2=1.0,
                                                               op0=ALU.mult, op1=ALU.add),
                  r=[("t2", hf)], w=[("t2", hf)])
            for hf in HF:
                A("act", lambda e, sl=SL[hf]: e.activation(out=eG[:, sl], in_=Gt[:, sl], func=AF.Exp),
                  r=[("G", hf)], w=[("eG", hf)])
                A("act", lambda e, sl=SL[hf]: e.activation(out=t1[:, sl], in_=Gt[:, sl], func=AF.Exp, scale=-1.0),
                  r=[("G", hf)], w=[("t1", hf)])
            for hf in HF:
                A("dve", lambda e, sl=SL[hf]: e.tensor_mul(out=t2[:, sl], in0=t2[:, sl], in1=t1[:, sl]),
                  r=[("t2", hf), ("t1", hf)], w=[("t2", hf)])
                A("act", lambda e, sl=SL[hf], h=h, hf=hf: e.copy(
                    out=dec[:, h, hf * 8:(hf + 1) * 8],
                    in_=eG[:, sl].rearrange("p (n c) -> p n c", c=128)[:, :, 127]),
                  r=[("eG", hf)], w=[("dec", h, hf)])
            for hf in HF:
                A("dve", lambda e, sl=SL[hf], h=h: e.tensor_copy(out=ki[:, h, sl], in_=t2[:, sl]),
                  r=[("t2", hf)], w=[("ki", h, hf)])
                A("dve", lambda e, sl=SL[hf], h=h, hf=hf: e.tensor_tensor(
                    out=kst[:, sl].rearrange("p (n c) -> p n c", c=128),
                    in0=t2[:, sl].rearrange("p (n c) -> p n c", c=128),
                    in1=dec[:, h, hf * 8:(hf + 1) * 8].unsqueeze(2).to_broadcast([128, 8, 128]), op=ALU.mult),
                  r=[("t2", hf), ("dec", h, hf)], w=[("kst", hf)])
                A("dve", lambda e, sl=SL[hf], h=h: e.tensor_mul(out=qd[:, h, sl], in0=tq[:, sl], in1=eG[:, sl]),
                  r=[("tq", hf), ("eG", hf)], w=[("qd", h, hf)])
            for hf in HF:
                for cc in range(8):
                    n = hf * 8 + cc
                    A("pe", lambda e, n=n: e.transpose(out=pTk[:, n, :], in_=kst[:, n * 128:(n + 1) * 128],
                                                       identity=ident[:]),
                      r=[("kst", hf), "ident"], w=[("pTk", hf)])
                A("act", lambda e, h=h, hf=hf: e.copy(out=ks_tm[:, h, hf * 8:(hf + 1) * 8, :],
                                                      in_=pTk[:, hf * 8:(hf + 1) * 8, :]),
                  r=[("pTk", hf)], w=[("ks_tm", h, hf)])
        wb_o = load_wblock(1536)
        wb_v = load_wblock(1024)
        for h in range(4):
            for hf in range(2):
                sl = slice(hf * HS, (hf + 1) * HS)
                p, pk = next_pp()
                for q2 in range(2):
                    proj_fm(p[:, q2, :], pk, wb_o, h, hf * HS + q2 * 512, 512)
                A("act", lambda e, p=p, sl=sl, h=h: e.activation(out=sog[:, h, sl],
                                                                 in_=p[:].rearrange("p a b -> p (a b)"), func=AF.Silu),
                  r=[pk], w=[("sog", h, hf)])
        for i in range(0, NT, 2):
            p, pk = next_pp()
            for q2 in range(2):
                for c in range(8):
                    A("pe", lambda e, p=p, q2=q2, c=c, i=i: e.matmul(
                        p[:, q2, :], lhsT=hT[:, c, (i + q2) * 128:(i + q2 + 1) * 128], rhs=wbf[wb_v][:, c, :],
                        start=(c == 0), stop=(c == 7)),
                      r=[("wbf", wb_v), ("hT", (i + q2) // 4)], w=[pk])
            A("dve", lambda e, p=p, i=i: e.tensor_copy(out=v_tm[:, i:i + 2, :], in_=p[:]),
              r=[pk], w=[("v_tm", i), ("v_tm", i + 1)])
        sa.close()
        sb_ = Scope()
        maskU = sb_.sb("maskU", [128, 128])
        A("sp", lambda e: e.dma_start(out=maskU[:], in_=maskU_d[:, :]), w=["maskU"], dma=True)
        am = [[sb_.sb(f"am{h}_{i}", [128, 128], BF16) for i in range(2)] for h in range(4)]
        st = sb_.sb("st", [128, 4, 128])
        stb = sb_.sb("stb", [128, 4, 128], BF16)
        sq = [sb_.sb(f"sq{i}", [128, 512], BF16) for i in range(2)]
        sd = [sb_.sb(f"sd{i}", [128, 512]) for i in range(2)]
        yt = [sb_.sb(f"yt{i}", [128, 512]) for i in range(2)]
        pA = sb_.ps("pA", [128, 4, 128])
        pKV = sb_.ps("pKV", [128, 4, 128])
        pO = [sb_.ps(f"pO{h}", [128, 512]) for h in range(4)]
        pM = [sb_.ps(f"pM{i}", [128, 512]) for i in range(2)]
        nm = [0]
        for n in range(NT):
            cs = slice(n * 128, (n + 1) * 128)
            oc = slice((n % 4) * 128, (n % 4 + 1) * 128)
            for h in range(4):
                A("pe", lambda e, h=h, cs=cs: e.matmul(pA[:, h, :], lhsT=ki[:, h, cs], rhs=qd[:, h, cs],
                                                      start=True, stop=True),
                  r=[("ki", h, n // 8), ("qd", h, n // 8)], w=[("pA",)])
            for h in range(4):
                a = am[h][n % 2]
                ak = ("am", h, n % 2)
                A("dve", lambda e, h=h, a=a: e.tensor_tensor(out=a[:], in0=pA[:, h, :], in1=maskU[:], op=ALU.mult),
                  r=[("pA",), "maskU"], w=[ak])
            if n < NT - 1:
                for h in range(4):
                    A("pe", lambda e, h=h, n=n: e.matmul(pKV[:, h, :], lhsT=ks_tm[:, h, n, :],
                                                         rhs=v_tm[:, n, h * 128:(h + 1) * 128], start=True, stop=True),
                      r=[("ks_tm", h, n // 8), ("v_tm", n)], w=[("pKV",)])
            for h in range(4):
                a = am[h][n % 2]
                ak = ("am", h, n % 2)
                A("pe", lambda e, h=h, a=a, oc=oc, n=n: e.matmul(pO[h][:, oc], lhsT=v_tm[:, n, h * 128:(h + 1) * 128],
                                                               rhs=a[:], start=True, stop=(n == 0)),
                  r=[("v_tm", n), ak], w=[("pO", h)])
                if n > 0:
                    A("pe", lambda e, h=h, oc=oc, cs=cs: e.matmul(pO[h][:, oc], lhsT=stb[:, h, :], rhs=qd[:, h, cs],
                                                                 start=False, stop=True),
                      r=[("stb", h), ("qd", h, n // 8)], w=[("pO", h)])
            if n < NT - 1:
                for h in range(4):
                    if n == 0:
                        A("dve", lambda e, h=h: e.tensor_copy(out=st[:, h, :], in_=pKV[:, h, :]),
                          r=[("pKV",)], w=[("st", h)])
                    else:
                        A("dve", lambda e, h=h, n=n: e.scalar_tensor_tensor(
                            out=st[:, h, :], in0=st[:, h, :], scalar=dec[:, h, n:n + 1], in1=pKV[:, h, :],
                            op0=ALU.mult, op1=ALU.add),
                          r=[("pKV",), ("st", h), ("dec", h, n // 8)], w=[("st", h)])
                    A("act", lambda e, h=h: e.copy(out=stb[:, h, :], in_=st[:, h, :]),
                      r=[("st", h)], w=[("stb", h)])
            if n % 4 == 3:
                for h in range(4):
                    g = n // 4
                    i = nm[0] % 2
                    nm[0] += 1
                    gs = slice(g * 512, (g + 1) * 512)
                    A("act", lambda e, h=h, i=i: e.activation(out=sq[i][:], in_=pO[h][:], func=AF.Square),
                      r=[("pO", h)], w=[("sq", i)])
                    A("pe", lambda e, i=i: e.matmul(pM[i][:], lhsT=onesv[:], rhs=sq[i][:], start=True, stop=True),
                      r=[("sq", i), "onesv"], w=[("pM", i)])
                    A("act", lambda e, i=i: e.activation(out=sd[i][:], in_=pM[i][:], func=AF.Ln, bias=cst["epsc"][:, 0:1]),
                      r=[("pM", i), "epsc"], w=[("sd", i)])
                    A("act", lambda e, i=i: e.activation(out=sd[i][:], in_=sd[i][:], func=AF.Exp, scale=-0.5),
                      r=[("sd", i)], w=[("sd", i)])
                    A("dve", lambda e, h=h, i=i: e.scalar_tensor_tensor(
                        out=yt[i][:], in0=pO[h][:], scalar=hgg[:, 0:1], in1=sd[i][:], op0=ALU.mult, op1=ALU.mult),
                      r=[("pO", h), ("sd", i), "hgg"], w=[("yt", i)])
                    A("pool", lambda e, h=h, i=i, gs=gs: e.tensor_tensor(out=yaT[:, h, gs], in0=yt[i][:],
                                                                       in1=sog[:, h, gs], op=ALU.mult),
                      r=[("yt", i), ("sog", h, g // 2)], w=[("yaT", h, g)])
        sb_.close()
        so.close()

    ATT = [(128, 1), (512, 4), (2048, 16)]

    def stage_attn(s):
        wbf = ctx["wbf"]
        hT = ctx["hT"]
        ybT = ctx["ybT"]
        so = Scope()
        ND = so.sb("ND", [128, 4, S])
        def group(g, dl):
            nblk = (S // dl) // 128
            sg_ = Scope()
            QT = sg_.sb("QT", [64, 4, S], BF16)
            KT = sg_.sb("KT", [64, 4, S], BF16)
            Vg = sg_.sb("Vg", [128, 16, 4, 128], BF16)
            A("pool", lambda e: e.memset(Vg[:, :, :, 64:128], 1.0), w=[("Vg1",)])
            sa = Scope()
            sq = [sa.sb(f"asq{i}", [128, 512], BF16) for i in range(4)]
            sd = [sa.sb(f"asd{i}", [128, 512]) for i in range(4)]
            pp = [sa.ps(f"app{i}", [128, 512]) for i in range(4)]
            pM = [sa.ps(f"apM{i}", [128, 512]) for i in range(4)]
            cnt = [0]
            for which, (dst, col0, gain) in enumerate([(QT, 2048 + g * 256, "gq"), (KT, 2816 + g * 256, "gk")]):
                wb = load_wblock(col0, 256)
                for hh in range(4):
                    for tb in range(4):
                        i = cnt[0] % 4
                        p = pp[cnt[0] % 4]
                        pk = ("app", cnt[0] % 4)
                        cnt[0] += 1
                        ts_ = slice(tb * 512, (tb + 1) * 512)
                        for c in range(8):
                            A("pe", lambda e, c=c, p=p, wb=wb, hh=hh, ts_=ts_: e.matmul(
                                p[0:64, :], lhsT=wbf[wb][:, c, hh * 64:(hh + 1) * 64], rhs=hT[:, c, ts_],
                                start=(c == 0), stop=(c == 7)),
                              r=[("wbf", wb), ("hT", tb)], w=[pk])
                        A("act", lambda e, p=p, i=i: e.activation(out=sq[i][0:64, :], in_=p[0:64, :], func=AF.Square),
                          r=[pk], w=[("asq", i)])
                        A("pe", lambda e, i=i: e.matmul(pM[i][0:64, :], lhsT=cst["blk64"][0:64, 0:64], rhs=sq[i][0:64, :],
                                                        start=True, stop=True),
                          r=[("asq", i), "blk64"], w=[("apM", i)])
                        A("act", lambda e, i=i: e.activation(out=sd[i][0:64, :], in_=pM[i][0:64, :], func=AF.Ln, bias=cst["epsc"][0:64, 0:1]),
                          r=[("apM", i), "epsc"], w=[("asd", i)])
                        A("act", lambda e, i=i: e.activation(out=sd[i][0:64, :], in_=sd[i][0:64, :], func=AF.Exp, scale=-0.5),
                          r=[("asd", i)], w=[("asd", i)])
                        A("dve", lambda e, p=p, i=i, dst=dst, hh=hh, ts_=ts_, gain=gain: e.scalar_tensor_tensor(
                            out=dst[:, hh, ts_], in0=p[0:64, :], scalar=cst[gain][0:64, 0:1], in1=sd[i][0:64, :],
                            op0=ALU.mult, op1=ALU.mult),
                          r=[pk, ("asd", i), gain], w=[("QK", which, hh, tb)])
            wb = load_wblock(3584 + g * 256, 256)
            for r_ in range(dl):
                for n in range(nblk):
                    ti = r_ * nblk + n
                    t0 = r_ + dl * 128 * n
                    p = pp[cnt[0] % 4]
                    pk = ("app", cnt[0] % 4)
                    cnt[0] += 1
                    hs = slice(t0, t0 + 127 * dl + 1, dl)
                    for c in range(8):
                        A("pe", lambda e, p=p, c=c, hs=hs, wb=wb: e.matmul(
                            p[:, 0:256], lhsT=hT[:, c, hs], rhs=wbf[wb][:, c, 0:256],
                            start=(c == 0), stop=(c == 7)),
                          r=[("wbf", wb)] + [("hT", k) for k in range(4)], w=[pk])
                    A("dve", lambda e, p=p, ti=ti: e.tensor_copy(
                        out=Vg[:, ti, :, 0:64], in_=p[:, 0:256].rearrange("p (h e) -> p h e", e=64)),
                      r=[pk], w=[("Vg", ti)])
            sa.close()
            sb2 = Scope()
            ef = [sb2.sb(f"ef{i}", [128, 4, 128], BF16) for i in range(4)]
            em = [sb2.sb(f"em{i}", [128, 4, 128], BF16) for i in range(4)]
            pS = [sb2.ps(f"pS{i}", [128, 4, 128]) for i in range(4)]
            pO = [sb2.ps(f"pO{i}", [128, 4, 128]) for i in range(2)]
            kc = [0]
            allQK = [("QK", w_, jj, tb) for w_ in range(2) for jj in range(4) for tb in range(4)]

            def s_phase(r_, n):
                t0 = r_ + dl * 128 * n
                qs = slice(t0, t0 + 127 * dl + 1, dl)
                kbs = ([n - 1] if n > 0 else []) + [n]
                ems = []
                for kb in kbs:
                    i = kc[0] % 4
                    kc[0] += 1
                    k0 = r_ + dl * 128 * kb
                    ks = slice(k0, k0 + 127 * dl + 1, dl)
                    for hh in range(4):
                        A("pe", lambda e, i=i, hh=hh, ks=ks, qs=qs: e.matmul(
                            pS[i][:, hh, :], lhsT=KT[:, hh, ks], rhs=QT[:, hh, qs],
                            start=True, stop=True),
                          r=allQK, w=[("pS", i)])
                    A("act", lambda e, i=i: e.activation(out=ef[i][:], in_=pS[i][:], func=AF.Exp, scale=0.125),
                      r=[("pS", i)], w=[("ef", i)])
                    mname = "maskUb" if kb == n else "maskLb"
                    A("pool", lambda e, i=i, mname=mname: e.tensor_tensor(
                        out=em[i][:], in0=ef[i][:], in1=cst[mname][:].unsqueeze(1).to_broadcast([128, 4, 128]),
                        op=ALU.mult),
                      r=[("ef", i), mname], w=[("em", i)])
                    ems.append((i, r_ * nblk + kb))
                return qs, ems

            def pv_phase(it, qs, ems):
                o = it % 2
                for hh in range(4):
                    for idx, (i, ti) in enumerate(ems):
                        A("pe", lambda e, o=o, hh=hh, i=i, ti=ti, idx=idx, last=(idx == len(ems) - 1): e.matmul(
                            pO[o][:, hh, :], lhsT=Vg[:, ti, hh, :], rhs=em[i][:, hh, :],
                            start=(idx == 0), stop=last),
                          r=[("Vg", ti), ("Vg1",), ("em", i)], w=[("pO", o)])
                if g == 0:
                    A("act", lambda e, o=o, qs=qs: e.copy(out=ND[:, :, qs], in_=pO[o][:]),
                      r=[("pO", o)], w=["ND"])
                else:
                    A("dve", lambda e, o=o, qs=qs: e.tensor_tensor(out=ND[:, :, qs], in0=ND[:, :, qs],
                                                                  in1=pO[o][:], op=ALU.add),
                      r=[("pO", o), "ND"], w=["ND"])

            its = [(r_, n) for r_ in range(dl) for n in range(nblk)]
            prev_ = None
            for k_, (r_, n) in enumerate(its):
                cur_ = s_phase(r_, n)
                if prev_ is not None:
                    pv_phase(k_ - 1, *prev_)
                prev_ = cur_
            pv_phase(len(its) - 1, *prev_)
            sb2.close()
            sg_.close()

        for g, (win, dl) in enumerate(ATT):
            group(g, dl)
        sf = Scope()
        pB = [sf.ps(f"pB{i}", [128, 512]) for i in range(2)]
        shsel = sf.sb("shsel", [128, 64])
        A("sp", lambda e: e.dma_start(out=shsel[:], in_=shsel_d[:, :]), w=["shsel"], dma=True)
        k = 0
        for hh in range(4):
            A("act", lambda e, hh=hh: e.activation(out=ND[64:128, hh, :], in_=ND[64:128, hh, :], func=AF.Ln), r=["ND"], w=["ND"])
            A("act", lambda e, hh=hh: e.activation(out=ND[64:128, hh, :], in_=ND[64:128, hh, :], func=AF.Exp, scale=-1.0),
              r=["ND"], w=["ND"])
            for tb in range(4):
                i = k % 2
                k += 1
                ts_ = slice(tb * 512, (tb + 1) * 512)
                A("pe", lambda e, i=i, hh=hh, ts_=ts_: e.matmul(pB[i][0:64, :], lhsT=shsel[:], rhs=ND[:, hh, ts_],
                                                               start=True, stop=True),
                  r=["ND", "shsel"], w=[("pB", i)])
                A("dve", lambda e, i=i, hh=hh, ts_=ts_: e.tensor_tensor(out=ybT[:, hh, ts_], in0=ND[0:64, hh, ts_],
                                                                       in1=pB[i][0:64, :], op=ALU.mult),
                  r=["ND", ("pB", i)], w=[("ybT", hh, tb)])
        sf.close()
        so.close()

    wa_d = din("w_a", [512, D])
    wb_d = din("w_b", [256, D])
    wout_d = din("w_out", [D, D])

    def stage_merge(s, alloc_xacc):
        hT, yaT, ybT = ctx["hT"], ctx["yaT"], ctx["ybT"]
        sm = Scope()
        mT = sm.sb("mT", [128, 8, S], BF16)
        si = Scope()
        wa = si.sb("wa", [128, 4, D], BF16)
        wbb = si.sb("wbb", [64, 4, D], BF16)
        A("pool", lambda e: e.dma_start(out=wa[:], in_=wa_d.rearrange("(f p) n -> p f n", p=128)), w=["wa"], dma=True)
        A("pool", lambda e: e.dma_start(out=wbb[:], in_=wb_d.rearrange("(h e) n -> e h n", e=64)), w=["wbb"], dma=True)
        sg = [si.sb(f"sg{i}", [128, 512]) for i in range(4)]
        m12 = [si.sb(f"m12{i}", [128, 512]) for i in range(4)]
        pp = [si.ps(f"mpp{i}", [128, 512]) for i in range(4)]
        pab = [si.ps(f"mpab{i}", [128, 512]) for i in range(4)]
        k = 0
        for jb in range(2):
            wga = load_wblock(4352 + jb * 512)
            wgb = load_wblock(5376 + jb * 512)
            for jj in range(4):
                j = jb * 4 + jj
                js = slice(j * 128, (j + 1) * 128)
                for tb in range(4):
                    ts_ = slice(tb * 512, (tb + 1) * 512)
                    ia, ib = (2 * k) % 4, (2 * k + 1) % 4
                    k += 1
                    proj_fm(pp[ia][:], ("mpp", ia), wga, jj, tb * 512, 512)
                    A("act", lambda e, ia=ia: e.activation(out=sg[ia][:], in_=pp[ia][:], func=AF.Sigmoid),
                      r=[("mpp", ia)], w=[("sg", ia)])
                    for f in range(4):
                        A("pe", lambda e, ia=ia, f=f, js=js, ts_=ts_: e.matmul(
                            pab[ia][:], lhsT=wa[:, f, js], rhs=yaT[:, f, ts_], start=(f == 0), stop=(f == 3)),
                          r=["wa", "yaT"], w=[("mpab", ia)])
                    A("dve", lambda e, ia=ia: e.tensor_tensor(out=m12[ia][:], in0=pab[ia][:], in1=sg[ia][:], op=ALU.mult),
                      r=[("mpab", ia), ("sg", ia)], w=[("m12", ia)])
                    proj_fm(pp[ib][:], ("mpp", ib), wgb, jj, tb * 512, 512)
                    A("act", lambda e, ib=ib: e.activation(out=sg[ib][:], in_=pp[ib][:], func=AF.Sigmoid),
                      r=[("mpp", ib)], w=[("sg", ib)])
                    for hh in range(4):
                        A("pe", lambda e, ib=ib, hh=hh, js=js, ts_=ts_: e.matmul(
                            pab[ib][:], lhsT=wbb[:, hh, js], rhs=ybT[:, hh, ts_], start=(hh == 0), stop=(hh == 3)),
                          r=["wbb", "ybT"], w=[("mpab", ib)])
                    A("dve", lambda e, ib=ib: e.tensor_tensor(out=m12[ib][:], in0=pab[ib][:], in1=sg[ib][:], op=ALU.mult),
                      r=[("mpab", ib), ("sg", ib)], w=[("m12", ib)])
                    A("pool", lambda e, ia=ia, ib=ib, j=j, ts_=ts_: e.tensor_tensor(
                        out=mT[:, j, ts_], in0=m12[ia][:], in1=m12[ib][:], op=ALU.add),
                      r=[("m12", ia), ("m12", ib)], w=[("mT", j, tb)])
        si.close()
        alloc_xacc()
        xacc = ctx["xacc"]
        so = Scope()
        wbf = ctx["wbf"]
        wo = [load_wblock(mh * 512, 512, src=wout_d) for mh in range(2)]
        po = [so.ps(f"po{i}", [128, 512]) for i in range(4)]
        k = 0
        for i in range(NT):
            A("sp", lambda e, i=i: e.dma_start(out=xacc[:, i, :], in_=x_d[s, i * 128:(i + 1) * 128, :]),
              w=[("xacc", i)], dma=True)
            for mh in range(2):
                pi = k % 4
                k += 1
                ms_ = slice(mh * 512, (mh + 1) * 512)
                for n in range(8):
                    A("pe", lambda e, pi=pi, n=n, i=i, mh=mh: e.matmul(
                        po[pi][:], lhsT=mT[:, n, i * 128:(i + 1) * 128], rhs=wbf[wo[mh]][:, n, :],
                        start=(n == 0), stop=(n == 7)),
                      r=[("wbf", wo[mh])] + [("mT", n, i // 4)], w=[("po", pi)])
                A("dve", lambda e, pi=pi, i=i, ms_=ms_: e.tensor_tensor(out=xacc[:, i, ms_], in0=xacc[:, i, ms_],
                                                                       in1=po[pi][:], op=ALU.add),
                  r=[("po", pi), ("xacc", i)], w=[("xacc", i)])
        so.close()
        sm.close()

    wq_d = din("peer_wq", [D, D])
    KB_d = din("peer_kb", [128, 8, 256])
    KT12_d = din("peer_kt12", [128, 8, 128])
    uT_d = din("peer_uT", [D, 16384])
    v_d = din("peer_v", [16384, D])
    NEG_BIG = -1.0e30
    EGS = 512
    NEG = 16384 // EGS

    def stage_peer(s, dbg=None):
        h2T, xacc = ctx["hT"], ctx["xacc"]
        sp_ = Scope()
        qpT = sp_.sb("qpT", [128, 8, S], BF16)
        thr = sp_.sb("thr", [128, NT, 8])
        nb = sp_.sb("nb", [128, NT, 8])
        KT12 = sp_.sb("KT12", [128, 8, 128], BF16)
        A("pool", lambda e: e.dma_start(out=KT12[:], in_=KT12_d[:, :, :]), w=["KT12"], dma=True)
        sa = Scope()
        wq = sa.sb("wq", [128, 8, D], BF16)
        KB = sa.sb("KB", [128, 8, 256], BF16)
        A("pool", lambda e: e.dma_start(out=wq[:], in_=wq_d.rearrange("(c p) n -> p c n", p=128)), w=["wq"], dma=True)
        A("pool", lambda e: e.dma_start(out=KB[:], in_=KB_d[:, :, :]), w=["KB"], dma=True)
        s12 = [sa.sb(f"s12_{i}", [128, 8, 256]) for i in range(2)]
        wk = sa.sb("wk", [128, 8, 256])
        t16 = sa.sb("t16", [128, 8, 2, 16])
        cand = sa.sb("cand", [128, 8, 256])
        cand2 = sa.sb("cand2", [128, 8, 256])
        c16 = sa.sb("c16", [128, 8, 16])
        negm = sa.sb("negm", [128, 8])
        dlt = sa.sb("dlt", [128, 8, 16])
        zz = sa.sb("zz", [128, 8])
        pq = [sa.ps(f"pq{i}", [128, 512]) for i in range(2)]
        ps12 = [sa.ps(f"ps12{i}", [128, 2, 256]) for i in range(2)]
        k = 0
        for h in range(8):
            for tb in range(4):
                i = k % 2
                k += 1
                ts_ = slice(tb * 512, (tb + 1) * 512)
                for c in range(8):
                    A("pe", lambda e, i=i, c=c, h=h, ts_=ts_: e.matmul(
                        pq[i][:], lhsT=wq[:, c, h * 128:(h + 1) * 128], rhs=h2T[:, c, ts_],
                        start=(c == 0), stop=(c == 7)),
                      r=["wq", ("hT", tb)], w=[("pq", i)])
                if k % 2 == 0:
                    A("act", lambda e, i=i, h=h, ts_=ts_: e.copy(out=qpT[:, h, ts_], in_=pq[i][:]),
                      r=[("pq", i)], w=[("qpT", h, tb)])
                else:
                    A("dve", lambda e, i=i, h=h, ts_=ts_: e.tensor_copy(out=qpT[:, h, ts_], in_=pq[i][:]),
                      r=[("pq", i)], w=[("qpT", h, tb)])
        k = 0
        for i in range(NT):
            sb_ = s12[i % 2]
            sk = ("s12", i % 2)
            tk = slice(i * 128, (i + 1) * 128)
            for hp in range(4):
                pi = k % 2
                k += 1
                for hh in range(2):
                    h = hp * 2 + hh
                    A("pe", lambda e, pi=pi, hh=hh, h=h, tk=tk: e.matmul(
                        ps12[pi][:, hh, :], lhsT=qpT[:, h, tk], rhs=KB[:, h, :], start=True, stop=True),
                      r=[("qpT", h, i // 4), "KB"], w=[("ps12", pi)])
                A("act", lambda e, pi=pi, hp=hp, sb_=sb_: e.copy(out=sb_[:, hp * 2:hp * 2 + 2, :], in_=ps12[pi][:]),
                  r=[("ps12", pi)], w=[sk])
            hh_ = [(h, half, slice(half * 128, (half + 1) * 128)) for h in range(8) for half in range(2)]
            for h, half, hs_ in hh_:
                A("dve", lambda e, sb_=sb_, h=h, half=half, hs_=hs_: e.max(out=t16[:, h, half, 0:8], in_=sb_[:, h, hs_]),
                  r=[sk], w=[("t16a", h, half)])
            for h, half, hs_ in hh_:
                A("dve", lambda e, sb_=sb_, h=h, half=half, hs_=hs_: e.match_replace(
                    out=wk[:, h, hs_], in_to_replace=t16[:, h, half, 0:8], in_values=sb_[:, h, hs_],
                    imm_value=NEG_BIG),
                  r=[sk, ("t16a", h, half)], w=[("wk", h, half)])
            for h, half, hs_ in hh_:
                A("dve", lambda e, h=h, half=half, hs_=hs_: e.max(out=t16[:, h, half, 8:16], in_=wk[:, h, hs_]),
                  r=[("wk", h, half)], w=[("t16b", h, half)])
            for h in range(8):
                A("pool", lambda e, h=h: e.tensor_tensor(
                    out=cand[:, h, :].rearrange("p (a b) -> p a b", b=16),
                    in0=t16[:, h, 0, :].unsqueeze(2).to_broadcast([128, 16, 16]),
                    in1=t16[:, h, 1, :].unsqueeze(1).to_broadcast([128, 16, 16]), op=ALU.add),
                  r=[("t16a", h, 0), ("t16a", h, 1), ("t16b", h, 0), ("t16b", h, 1)], w=[("cand", h)])
            for h in range(8):
                A("dve", lambda e, h=h: e.max(out=c16[:, h, 0:8], in_=cand[:, h, :]), r=[("cand", h)], w=[("c16a", h)])
            for h in range(8):
                A("dve", lambda e, h=h: e.match_replace(out=cand2[:, h, :], in_to_replace=c16[:, h, 0:8],
                                                        in_values=cand[:, h, :], imm_value=NEG_BIG),
                  r=[("cand", h), ("c16a", h)], w=[("cand2", h)])
            for h in range(8):
                A("dve", lambda e, h=h: e.max(out=c16[:, h, 8:16], in_=cand2[:, h, :]), r=[("cand2", h)], w=[("c16b", h)])
            c16k = [("c16a", h) for h in range(8)] + [("c16b", h) for h in range(8)]
            A("dve", lambda e: e.tensor_scalar(out=negm[:], in0=c16[:, :, 0], scalar1=-1.0, scalar2=None, op0=ALU.mult),
              r=c16k, w=["negm"])
            A("dve", lambda e, i=i: e.tensor_scalar(out=thr[:, i, :], in0=c16[:, :, 15], scalar1=-1.0e-4, scalar2=None,
                                                    op0=ALU.add),
              r=c16k, w=[("thr", i)])
            A("dve", lambda e: e.tensor_tensor(out=dlt[:], in0=c16[:], in1=negm[:].unsqueeze(2).to_broadcast([128, 8, 16]),
                                               op=ALU.add),
              r=c16k + ["negm"], w=["dlt"])
            A("act", lambda e: e.activation(out=dlt[:], in_=dlt[:], func=AF.Exp), r=["dlt"], w=["dlt"])
            A("dve", lambda e: e.reduce_sum(out=zz[:], in_=dlt[:], axis=AX.X), r=["dlt"], w=["zz"])
            A("act", lambda e: e.activation(out=zz[:], in_=zz[:], func=AF.Ln), r=["zz"], w=["zz"])
            A("dve", lambda e, i=i: e.tensor_sub(out=nb[:, i, :], in0=negm[:], in1=zz[:]),
              r=["negm", "zz"], w=[("nb", i)])
        if dbg is not None:
            dthr, dnb, dq = dbg
            A("sp", lambda e: e.dma_start(out=dthr[:, :, :], in_=thr[:]), r=[("thr", i) for i in range(NT)], dma=True)
            A("sp", lambda e: e.dma_start(out=dnb[:, :, :], in_=nb[:]), r=[("nb", i) for i in range(NT)], dma=True)
            A("sp", lambda e: e.dma_start(out=dq[:, :, :], in_=qpT[:]),
              r=[("qpT", h, tb) for h in range(8) for tb in range(4)], dma=True)
        sa.close()
        sm = Scope()
        FKT = sm.sb("FKT", [128, 8, EGS], BF16)
        uT = [sm.sb(f"uT{i}", [128, 8, EGS], BF16) for i in range(2)]
        vv = [sm.sb(f"vv{i}", [128, 4, D], BF16) for i in range(2)]
        G = [sm.sb(f"G{i}", [128, 4, 512], BF16) for i in range(2)]
        E = [sm.sb(f"E{i}", [128, 512], BF16) for i in range(4)]
        EmE = [[sm.sb(f"EmE{j}_{k}", [128, 512], BF16) for k in range(4)] for j in range(2)]
        EmO = [sm.sb(f"EmO{k}", [128, 512], BF16) for k in range(4)]
        AT = [sm.sb(f"AT{i}", [128, 4, 128], BF16) for i in range(2)]
        pH = [sm.ps(f"pH{i}", [128, 512]) for i in range(1)]
        pS = [sm.ps(f"pS{i}", [128, 512]) for i in range(4)]
        pW = [sm.ps(f"pW{i}", [128, 4, 128]) for i in range(1)]
        pOut = [sm.ps(f"pOut{i}", [128, 512]) for i in range(2)]
        Gpre = sm.sb("Gpre", [128, 4, 512], BF16)
        A("pool", lambda e: e.tensor_copy(
            out=FKT[64:128, :, :].rearrange("p h (c i) -> p h c i", i=128),
            in_=KT12[64:128, :, :].unsqueeze(2).to_broadcast([64, 8, 4, 128])),
          r=["KT12"], w=[("FKTb",)])
        cn = {"H": 0, "S": 0, "E": 0, "O": 0}

        def load_group(eg):
            b = eg % 2
            A("pool", lambda e: e.dma_start(
                out=uT[b][:], in_=uT_d[:, eg * EGS:(eg + 1) * EGS].rearrange("(c p) n -> p c n", p=128)),
              w=[("uT", b)], dma=True)
            A("pool", lambda e: e.dma_start(
                out=vv[b][:], in_=v_d[eg * EGS:(eg + 1) * EGS, :].rearrange("(c p) n -> p c n", p=128)),
              w=[("vv", b)], dma=True)

        def fkt_group(eg):
            for hq in range(2):
                A("dve", lambda e, hq=hq: e.tensor_copy(
                    out=FKT[0:64, hq * 4:(hq + 1) * 4, :].rearrange("p h (c i) -> p h c i", i=128),
                    in_=KT12[0:64, hq * 4:(hq + 1) * 4, eg * 4:(eg + 1) * 4].unsqueeze(3).to_broadcast([64, 4, 4, 128])),
                  r=["KT12"], w=[("FKTt", h) for h in range(hq * 4, (hq + 1) * 4)])

        def h_mm(gs, cc, c):
            eg, sbk = gs // 4, gs % 4
            b = eg % 2
            A("pe", lambda e: e.matmul(
                pH[0][:], lhsT=uT[b][:, c, cc * 128:(cc + 1) * 128],
                rhs=h2T[:, c, sbk * 512:(sbk + 1) * 512], start=(c == 0), stop=(c == 7)),
              r=[("uT", b), ("hT", sbk)], w=[("pH", 0)])

        def h_copy(cc):
            A("act", lambda e: e.copy(out=Gpre[:, cc, :], in_=pH[0][:]), r=[("pH", 0)], w=[("Gpre", cc)])

        def gelu_all(gs):
            A("act", lambda e: e.activation(out=G[gs % 2][:], in_=Gpre[:], func=AF.Gelu),
              r=[("Gpre", cc) for cc in range(4)], w=[("G", gs % 2)])

        def s_head(u, h):
            eg, i = u // NT, u % NT
            tk = slice(i * 128, (i + 1) * 128)
            p_ = cn["S"] % 4
            cn["S"] += 1
            x_ = cn["E"] % 4
            cn["E"] += 1
            es_ = u % 2
            A("pe", lambda e: e.matmul(pS[p_][:], lhsT=qpT[:, h, tk], rhs=FKT[:, h, :], start=True, stop=True),
              r=[("qpT", h, i // 4), ("FKTb",), ("FKTt", h)], w=[("pS", p_)])
            A("act", lambda e: e.activation(out=E[x_][:], in_=pS[p_][:], func=AF.Exp, bias=nb[:, i, h:h + 1]),
              r=[("pS", p_), ("nb", i)], w=[("E", x_)])
            if h % 2 == 0:
                dst, dk = EmE[es_][h // 2], ("EmE", es_, h // 2)
            else:
                dst, dk = EmO[h // 2], ("EmO", h // 2)
            A("dve", lambda e: e.scalar_tensor_tensor(
                out=dst[:], in0=pS[p_][:], scalar=thr[:, i, h:h + 1], in1=E[x_][:],
                op0=ALU.is_ge, op1=ALU.mult),
              r=[("pS", p_), ("E", x_), ("thr", i)], w=[dk])
            if h % 2 == 1:
                pk_ = ("EmE", es_, h // 2)
                A("pool", lambda e: e.tensor_tensor(out=EmE[es_][h // 2][:], in0=EmE[es_][h // 2][:], in1=dst[:],
                                                    op=ALU.add),
                  r=[pk_, dk], w=[pk_])

        def t_piece(u, slot):
            es_ = u % 2
            cc = slot // 2
            for pr in range((slot % 2) * 2, (slot % 2) * 2 + 2):
                A("pe", lambda e, pr=pr: e.matmul(
                    pW[0][:, cc, :], lhsT=EmE[es_][pr][:, cc * 128:(cc + 1) * 128], rhs=ident[:],
                    start=(pr == 0), stop=(pr == 3)),
                  r=[("EmE", es_, pr), "ident"], w=[("pW", 0)])

        def at_piece(u):
            eg, i = u // NT, u % NT
            gs, tt = eg * 4 + i // 4, i % 4
            w_ = u % 2
            A("dve", lambda e: e.tensor_tensor(
                out=AT[w_][:], in0=pW[0][:], in1=G[gs % 2][:, :, tt * 128:(tt + 1) * 128], op=ALU.mult),
              r=[("pW", 0), ("G", gs % 2)], w=[("AT", w_)])

        def o_mm(u, k):
            eg, i = u // NT, u % NT
            b = eg % 2
            w_ = u % 2
            mh, cc = k // 4, k % 4
            ms_ = slice(mh * 512, (mh + 1) * 512)
            A("pe", lambda e: e.matmul(
                pOut[mh][:], lhsT=AT[w_][:, cc, :], rhs=vv[b][:, cc, ms_], start=(cc == 0), stop=(cc == 3)),
              r=[("AT", w_), ("vv", b)], w=[("pOut", mh)])

        tmpO = sm.sb("tmpO", [128, 512])

        def add_piece(u, mh):
            eg, i = u // NT, u % NT
            ms_ = slice(mh * 512, (mh + 1) * 512)
            if mh == 1:
                A("act", lambda e: e.copy(out=tmpO[:], in_=pOut[1][:]), r=[("pOut", 1)], w=["tmpO"])
                if eg < NEG - 1:
                    A("pool", lambda e: e.tensor_tensor(out=xacc[:, i, ms_], in0=xacc[:, i, ms_], in1=tmpO[:], op=ALU.add),
                      r=["tmpO", ("xacc", i)], w=[("xacc", i)])
                else:
                    A("pool", lambda e: e.tensor_tensor(out=ostage[:], in0=xacc[:, i, ms_], in1=tmpO[:], op=ALU.add),
                      r=["tmpO", ("xacc", i)], w=["ostage"])
                    A("sp", lambda e: e.dma_start(out=out_d[s, i * 128:(i + 1) * 128, ms_], in_=ostage[:]),
                      r=["ostage"], dma=True)
                return
            if eg < NEG - 1:
                A("dve", lambda e: e.tensor_tensor(out=xacc[:, i, ms_], in0=xacc[:, i, ms_], in1=pOut[mh][:], op=ALU.add),
                  r=[("pOut", mh), ("xacc", i)], w=[("xacc", i)])
            else:
                A("dve", lambda e: e.tensor_tensor(out=ostage[:], in0=xacc[:, i, ms_], in1=pOut[mh][:], op=ALU.add),
                  r=[("pOut", mh), ("xacc", i)], w=["ostage"])
                A("sp", lambda e: e.dma_start(out=out_d[s, i * 128:(i + 1) * 128, ms_], in_=ostage[:]),
                  r=["ostage"], dma=True)

        NU = NEG * NT
        load_group(0)
        load_group(1)
        for cc in range(4):
            for c in range(8):
                h_mm(0, cc, c)
            h_copy(cc)
        gelu_all(0)
        for v in range(NU + 3):
            cur = v < NU
            tv = v - 1 if 0 <= v - 1 < NU else None
            oa = v - 2 if 0 <= v - 2 < NU else None
            ob = v - 3 if 0 <= v - 3 < NU else None
            hc = None
            if cur:
                eg, i = v // NT, v % NT
                if i == 0:
                    fkt_group(eg)
                if i == 4 and 1 <= eg < NEG - 1:
                    load_group(eg + 1)
                gs, tt = eg * 4 + i // 4, i % 4
                if gs + 1 < NEG * 4:
                    hc = (gs + 1, tt)
            hk = 0
            for h in range(8):
                if cur:
                    s_head(v, h)
                if h < 4 and ob is not None:
                    o_mm(ob, 4 + h)
                if h >= 4 and oa is not None:
                    o_mm(oa, h - 4)
                if hc is not None and h >= 3:
                    for _ in range(1 if h < 5 else 2):
                        h_mm(hc[0], hc[1], hk)
                        hk += 1
                if tv is not None:
                    t_piece(tv, h)
                if ob is not None and h == 4:
                    add_piece(ob, 1)
            if hc is not None:
                h_copy(hc[1])
                if hc[1] == 3:
                    gelu_all(hc[0])
            if tv is not None:
                at_piece(tv)
            if oa is not None:
                add_piece(oa, 0)
        sm.close()
        sp_.close()

    def run_sequence(s, upto="all"):
        sq_ = Scope()
        ctx["hT"] = sq_.sb("hT", [128, 8, S], BF16, side="right")
        sy = Scope()
        ctx["wbf"] = [sy.sb(f"wbf{i}", [128, 8, 512], BF16) for i in range(3)]
        stage_norm(s, "x")
        ctx["yaT"] = sy.sb("yaT", [128, 4, S], BF16)
        stage_hgrn(s)
        if upto == "hgrn":
            return
        ctx["ybT"] = sy.sb("ybT", [64, 4, S], BF16)
        stage_attn(s)
        if upto == "attn":
            return
        def alloc_xacc():
            ctx["xacc"] = sq_.sb("xacc", [128, NT, D], F32, side="right")

        stage_merge(s, alloc_xacc)
        sy.close()
        if upto == "merge":
            return
        stage_norm(s, "xacc")
        if upto == "norm2":
            return
        if upto == "peerprep":
            dthr = dbg_out("thr", [128, NT, 8])
            dnb = dbg_out("nb", [128, NT, 8])
            dq = dbg_out("qpT", [128, 8, S], BF16)
            stage_peer(s, dbg=(dthr, dnb, dq))
        else:
            stage_peer(s)
        sq_.close()

    if debug == "hgrn":
        run_sequence(0, "hgrn")
        dbg_ya = dbg_out("yaT", [128, 4, S], BF16)
        A("sp", lambda e: e.dma_start(out=dbg_ya[:, :, :], in_=ctx["yaT"][:]), dma=True)
    elif debug == "attn":
        run_sequence(0, "attn")
        dbg_ya = dbg_out("yaT", [128, 4, S], BF16)
        A("sp", lambda e: e.dma_start(out=dbg_ya[:, :, :], in_=ctx["yaT"][:]), dma=True)
        dbg_yb = dbg_out("ybT", [64, 4, S], BF16)
        A("sp", lambda e: e.dma_start(out=dbg_yb[:, :, :], in_=ctx["ybT"][:]), dma=True)
    elif debug in ("peerprep", "seq0"):
        run_sequence(0, debug)
    elif debug is None:
        for s_ in range(nseq):
            run_sequence(s_)
    elif debug == "norm2":
        run_sequence(0, "norm2")
        dbg_x1 = dbg_out("x1", [128, NT, D])
        A("sp", lambda e: e.dma_start(out=dbg_x1[:, :, :], in_=ctx["xacc"][:]), dma=True)
        dbg_h2 = dbg_out("h2T", [128, 8, S], BF16)
        A("sp", lambda e: e.dma_start(out=dbg_h2[:, :, :], in_=ctx["hT"][:]), dma=True)

    sc.barrier()

    with ExitStack() as es2:
        sems = {e: es2.enter_context(nc.semaphore("sem_" + e)) for e in Sched.ENGS}
        dma_sems = {e: [es2.enter_context(nc.semaphore(f"dsem_{e}{i}")) for i in range(8)] for e in ("sp", "pool", "act")}
        sc.finalize(sems, dma_sems)
        with nc.Block() as block:
            @block.sync
            def _(E):
                sc.emit_engine("sp", E)

            @block.scalar
            def _(E):
                sc.emit_engine("act", E)

            @block.vector
            def _(E):
                sc.emit_engine("dve", E)

            @block.gpsimd
            def _(E):
                sc.emit_engine("pool", E)

            @block.tensor
            def _(E):
                sc.emit_engine("pe", E)
    try:
        es.close()
    except AssertionError:
        if debug is None:
            raise
    return nc, dbg_outs


def host_consts():
    bf = ml_dtypes.bfloat16
    c = {}
    c["ident"] = np.eye(128, dtype=np.float32).astype(bf)
    i = np.arange(128)
    c["maskU"] = (i[:, None] <= i[None, :]).astype(np.float32)
    c["onesv"] = np.full((128, 128), 1.0 / 128, np.float32).astype(bf)
    sm = np.ones((128, 1024), np.float32)
    sm[:, ::128] = 0.0
    c["scanm"] = sm
    b64 = np.zeros((128, 128), np.float32)
    b64[:64, :64] = 1.0 / 64
    b64[64:, 64:] = 1.0 / 64
    c["blk64"] = b64.astype(bf)
    c["ones64"] = np.ones((128, 64), np.float32).astype(bf)
    c["epsc"] = np.full((128, 1), EPS, np.float32)
    sh = np.zeros((128, 64), np.float32)
    sh[64 + np.arange(64), np.arange(64)] = 1.0
    c["shsel"] = sh
    c["maskUb"] = (i[:, None] <= i[None, :]).astype(np.float32).astype(bf)
    c["maskLb"] = (i[:, None] >= i[None, :]).astype(np.float32).astype(bf)
    return c


def make_in_maps(inputs, nseq=NSEQ, ncores=NCORES):
    x = np.ascontiguousarray(inputs["x"], dtype=np.float32)
    consts = host_consts()
    w_in = np.ascontiguousarray(inputs["w_in"][0], dtype=np.float32)
    g1bc = np.ascontiguousarray(np.broadcast_to(inputs["norm1_g"][0][None, :], (128, D)), dtype=np.float32)
    g2bc = np.ascontiguousarray(np.broadcast_to(inputs["norm2_g"][0][None, :], (128, D)), dtype=np.float32)
    w_a = np.ascontiguousarray(inputs["w_branch_a"][0], dtype=np.float32)
    w_b = np.ascontiguousarray(inputs["w_branch_b"][0], dtype=np.float32)
    w_out = np.ascontiguousarray(inputs["w_out"][0], dtype=np.float32)
    lg = np.asarray(inputs["hg_lb_logits"], dtype=np.float32)
    lbl = np.ascontiguousarray(np.concatenate([lg[0].reshape(4, 128).T, lg[1].reshape(4, 128).T], axis=1))
    hgg = np.ascontiguousarray(inputs["hg_norm_g"][0].reshape(128, 1), dtype=np.float32)
    gq = np.ascontiguousarray(np.tile(inputs["q_norm_g"][0], 2).reshape(128, 1), dtype=np.float32)
    gk = np.ascontiguousarray(np.tile(inputs["k_norm_g"][0], 2).reshape(128, 1), dtype=np.float32)
    wq = np.ascontiguousarray(inputs["peer_wq"][0], dtype=np.float32)
    sk = np.asarray(inputs["peer_subkeys"][0], dtype=np.float32)
    kt12 = np.ascontiguousarray(np.concatenate([sk[:, 0].transpose(2, 0, 1), sk[:, 1].transpose(2, 0, 1)], axis=0))
    kb = np.zeros((128, 8, 256), np.float32)
    kb[0:64, :, 0:128] = kt12[0:64]
    kb[64:128, :, 128:256] = kt12[64:128]
    uT = np.ascontiguousarray(inputs["peer_u"][0].T, dtype=np.float32)
    pv = np.ascontiguousarray(inputs["peer_v"][0], dtype=np.float32)
    maps = []
    for c in range(ncores):
        m = {"x": x[c * nseq:(c + 1) * nseq], "w_in": w_in, "g1bc": g1bc, "g2bc": g2bc, "lbl": lbl, "hgg": hgg,
             "gq": gq, "gk": gk, "w_a": w_a, "w_b": w_b, "w_out": w_out,
             "peer_wq": wq, "peer_kb": kb, "peer_kt12": kt12, "peer_uT": uT, "peer_v": pv}
        m.update(consts)
        maps.append(m)
    return maps


def kernel(**inputs):
    nc, _ = build_program()
    in_maps = make_in_maps(inputs)
    res = run_bass_kernel_spmd(nc, in_maps, core_ids=list(range(NCORES)))
    return np.concatenate([r["out"] for r in res.results], axis=0)
```

```python
import numpy as np
import ml_dtypes
import concourse.bass as bass
import concourse.mybir as mybir
from concourse.bass_utils import run_bass_kernel_spmd

F32 = mybir.dt.float32
BF16 = mybir.dt.bfloat16
AF = mybir.ActivationFunctionType
ALU = mybir.AluOpType
AX = mybir.AxisListType

NCORES = 8
D = 1024
S = 2048
NSEQ = 2
NT = S // 128
EPS = 1e-6
INC = 6400


class _Op:
    __slots__ = ("id", "eng", "fn", "deps", "dma", "needed", "sem", "val", "prewait", "waits")

    def __init__(self, id, eng, fn, dma):
        self.id = id
        self.eng = eng
        self.fn = fn
        self.deps = {}
        self.dma = dma
        self.needed = False
        self.sem = None
        self.val = 0
        self.prewait = None
        self.waits = []


class Sched:
    ENGS = ("pe", "act", "dve", "pool", "sp")

    def __init__(self):
        self.ops = []
        self.lastw = {}
        self.readers = {}
        self.last_eng_op = {}
        self.dma_since_barrier = []

    def add(self, eng, fn, reads=(), writes=(), dma=False):
        op = _Op(len(self.ops), eng, fn, dma)
        deps = op.deps
        for k in reads:
            w = self.lastw.get(k)
            if w is not None:
                deps[w] = True
        for k in writes:
            w = self.lastw.get(k)
            if w is not None:
                deps.setdefault(w, False)
            rd = self.readers.get(k)
            if rd:
                for r in rd.values():
                    deps.setdefault(r, False)
        for k in writes:
            self.lastw[k] = op.id
            self.readers[k] = {}
        for k in reads:
            rd = self.readers.setdefault(k, {})
            rd[("dma", op.id) if dma else eng] = op.id
        for d in list(deps):
            p = self.ops[d]
            if p.dma or dma:
                continue
            if p.eng == eng and (eng == "pe" or not deps[d]):
                del deps[d]
        self.ops.append(op)
        if dma:
            self.dma_since_barrier.append(op.id)
        elif fn is not None:
            self.last_eng_op[eng] = op.id
        return op

    def barrier(self):
        targets = list(self.last_eng_op.values()) + list(self.dma_since_barrier)
        for e in self.ENGS:
            op = _Op(len(self.ops), e, None, False)
            for t in targets:
                op.deps[t] = True
            self.ops.append(op)
        self.lastw = {}
        self.readers = {}
        self.dma_since_barrier = []

    def finalize(self, sems, dma_sems):
        for op in self.ops:
            for d in op.deps:
                self.ops[d].needed = True
        cnt = {e: 0 for e in self.ENGS}
        dcnt = {e: 0 for e in self.ENGS}
        for op in self.ops:
            if op.dma:
                j = dcnt[op.eng]
                dcnt[op.eng] += 1
                pool = dma_sems[op.eng]
                op.sem = pool[j % len(pool)]
                op.val = 16 * (j // len(pool) + 1)
                if op.val > 16:
                    op.prewait = (op.sem, op.val - 16)
            elif op.needed:
                cnt[op.eng] += 1
                op.sem = sems[op.eng]
                op.val = cnt[op.eng]
        know = {e: {} for e in self.ENGS}
        prod_know = {}
        for op in self.ops:
            K = know[op.eng]
            need = {}
            for d in sorted(op.deps, reverse=True):
                p = self.ops[d]
                key = id(p.sem)
                if key not in need or need[key][1] < p.val:
                    need[key] = (p.sem, p.val)
            if op.prewait is not None:
                key = id(op.prewait[0])
                if key not in need or need[key][1] < op.prewait[1]:
                    need[key] = op.prewait
            waits = []
            for key, (sm, v) in need.items():
                if K.get(key, 0) >= v:
                    continue
                waits.append((sm, v))
                K[key] = v
                pk = prod_know.get((key, v))
                if pk:
                    for k2, v2 in pk.items():
                        if K.get(k2, 0) < v2:
                            K[k2] = v2
            op.waits = waits
            if op.sem is not None and (op.dma or op.needed):
                snap = dict(K)
                if not op.dma:
                    snap[id(op.sem)] = op.val
                prod_know[(id(op.sem), op.val)] = snap

    def emit_engine(self, eng, E):
        for op in self.ops:
            if op.eng != eng:
                continue
            for (sm, v) in op.waits:
                E.wait_ge(sm, v)
            if op.fn is None:
                continue
            ins = op.fn(E)
            if op.dma:
                ins.then_inc(op.sem, 16)
            elif op.needed:
                ins.then_inc(op.sem, 1)


def build_program(debug=None, nseq=NSEQ):
    from contextlib import ExitStack

    nc = bass.Bass("TRN2", target_bir_lowering=False)
    sc = Sched()
    dbg_outs = {}

    def din(name, shape, dt=F32):
        return nc.dram_tensor(name, list(shape), dt, kind="ExternalInput").ap()

    x_d = din("x", [nseq, S, D])
    win_d = din("w_in", [D, INC])
    out_d = nc.dram_tensor("out", [nseq, S, D], F32, kind="ExternalOutput").ap()

    def dbg_out(name, shape, dt=F32):
        t = nc.dram_tensor("dbg_" + name, list(shape), dt, kind="ExternalOutput").ap()
        dbg_outs[name] = t
        return t

    es = ExitStack()

    def sb(name, shape, dt=F32):
        return es.enter_context(nc.sbuf_tensor("sb_" + name, list(shape), dt))

    def ps(name, shape, dt=F32):
        return es.enter_context(nc.psum_tensor("ps_" + name, list(shape), dt))

    def A(eng, fn, r=(), w=(), dma=False):
        return sc.add(eng, fn, r, w, dma)

    uniq = [0]

    class Scope:
        def __init__(self):
            self.es = ExitStack()

        def sb(self, name, shape, dt=F32, side=None):
            uniq[0] += 1
            return self.es.enter_context(nc.sbuf_tensor(f"sb_{name}_{uniq[0]}", list(shape), dt, side=side))

        def ps(self, name, shape, dt=F32):
            uniq[0] += 1
            return self.es.enter_context(nc.psum_tensor(f"ps_{name}_{uniq[0]}", list(shape), dt))

        def close(self):
            sc.barrier()
            self.es.close()

    cst = {}

    def load_const(name, shape, dt=F32):
        d = din(name, shape, dt)
        t = sb("c_" + name, shape, dt)
        if len(shape) == 2:
            A("sp", lambda e: e.dma_start(out=t[:], in_=d[:, :]), w=[name], dma=True)
        else:
            A("sp", lambda e: e.dma_start(out=t[:], in_=d[:, :, :]), w=[name], dma=True)
        cst[name] = t
        return t

    ident = load_const("ident", [128, 128], BF16)
    maskU_d = din("maskU", [128, 128])
    onesv = load_const("onesv", [128, 128], BF16)
    scanm_d = din("scanm", [128, 1024])
    lbl = load_const("lbl", [128, 8])
    hgg = load_const("hgg", [128, 1])
    load_const("blk64", [128, 128], BF16)
    load_const("ones64", [128, 64], BF16)
    load_const("maskUb", [128, 128], BF16)
    load_const("maskLb", [128, 128], BF16)
    shsel_d = din("shsel", [128, 64])
    load_const("epsc", [128, 1])
    load_const("gq", [128, 1])
    load_const("gk", [128, 1])
    lbc = sb("lbc", [128, 12])
    A("dve", lambda e: e.tensor_sub(out=lbc[:, 8:12], in0=lbl[:, 0:4], in1=lbl[:, 4:8]), r=["lbl"], w=["lbtmp"])
    A("act", lambda e: e.activation(out=lbc[:, 0:4], in_=lbc[:, 8:12], func=AF.Sigmoid), r=["lbtmp"], w=["lb0"])
    A("dve", lambda e: e.tensor_scalar(out=lbc[:, 4:8], in0=lbc[:, 0:4], scalar1=-1.0, scalar2=1.0,
                                       op0=ALU.mult, op1=ALU.add), r=["lb0"], w=["lb"])

    g1bc_d = din("g1bc", [128, D])
    g2bc_d = din("g2bc", [128, D])
    ctx = {}
    ostage = sb("ostage", [128, 512])

    def stage_norm(s, src):
        hT = ctx["hT"]
        sp_ = Scope()
        gbc = sp_.sb("gbc", [128, D])
        gsrc = g1bc_d if src == "x" else g2bc_d
        A("sp", lambda e: e.dma_start(out=gbc[:], in_=gsrc[:, :]), w=["gbc"], dma=True)
        xt = [sp_.sb(f"xt{i}", [128, D]) for i in range(2)]
        junk = sp_.sb("junk", [128, D], BF16)
        xn = [sp_.sb(f"xn{i}", [128, D], BF16) for i in range(2)]
        stat = sp_.sb("stat", [128, 4, 4])
        pT = [sp_.ps(f"pT{i}", [128, 8, 128], BF16) for i in range(2)]
        def stage_a(i):
            b = i % 2
            if src == "x":
                A("sp", lambda e: e.dma_start(out=xt[b][:], in_=x_d[s, i * 128:(i + 1) * 128, :]),
                  w=[("xt", b)], dma=True)
                xin = xt[b][:]
                xk = ("xt", b)
            else:
                xin = ctx["xacc"][:, i, :]
                xk = ("xacc", i)
            q4 = i % 4
            A("act", lambda e: e.activation(out=junk[:], in_=xin, func=AF.Square, accum_out=stat[:, q4, 0:1]),
              r=[xk], w=[("stat", q4, 0)])
            A("act", lambda e: e.activation(out=stat[:, q4, 1:2], in_=stat[:, q4, 0:1], func=AF.Sqrt,
                                            bias=EPS, scale=1.0 / D),
              r=[("stat", q4, 0)], w=[("stat", q4, 1)])
            A("dve", lambda e: e.reciprocal(out=stat[:, q4, 2:3], in_=stat[:, q4, 1:2]),
              r=[("stat", q4, 1)], w=[("stat", q4, 2)])
            A("dve", lambda e: e.scalar_tensor_tensor(
                out=xn[b][:], in0=xin, scalar=stat[:, q4, 2:3], in1=gbc[:], op0=ALU.mult, op1=ALU.mult),
              r=[xk, ("stat", q4, 2), "gbc"], w=[("xn", b)])
            for c in range(8):
                A("pe", lambda e, c=c: e.transpose(out=pT[b][:, c, :], in_=xn[b][:, c * 128:(c + 1) * 128],
                                                   identity=ident[:]),
                  r=[("xn", b), "ident"], w=[("pT", b)])

        def stage_b(i):
            b = i % 2
            A("act", lambda e: e.copy(out=hT[:, :, i * 128:(i + 1) * 128], in_=pT[b][:]),
              r=[("pT", b)], w=[("hT", i // 4)])

        stage_a(0)
        for i in range(NT):
            if i + 1 < NT:
                stage_a(i + 1)
            stage_b(i)
        sp_.close()

    wload_n = [0]

    def load_wblock(col0, ncols=512, src=None):
        src = win_d if src is None else src
        wbf = ctx["wbf"]
        b = wload_n[0] % 3
        wload_n[0] += 1
        A("pool", lambda e: e.dma_start(out=wbf[b][:, :, :ncols],
                                        in_=src[:, col0:col0 + ncols].rearrange("(c p) n -> p c n", p=128)),
          w=[("wbf", b)], dma=True)
        return b

    def proj_fm(pout, pkey, wb, j, t0, nt):
        hT = ctx["hT"]
        wbf = ctx["wbf"]
        for c in range(8):
            A("pe", lambda e, c=c: e.matmul(pout, lhsT=wbf[wb][:, c, j * 128:(j + 1) * 128],
                                            rhs=hT[:, c, t0:t0 + nt], start=(c == 0), stop=(c == 7)),
              r=[("wbf", wb)] + [("hT", k) for k in range(t0 // 512, (t0 + nt - 1) // 512 + 1)], w=[pkey])

    HS = 1024

    def stage_hgrn(s):
        wbf = ctx["wbf"]
        hT = ctx["hT"]
        yaT = ctx["yaT"]
        so = Scope()
        qd = so.sb("qd", [128, 4, S], BF16)
        ki = so.sb("ki", [128, 4, S], BF16)
        ks_tm = so.sb("ks_tm", [128, 4, NT, 128], BF16)
        v_tm = so.sb("v_tm", [128, NT, 512], BF16)
        sog = so.sb("sog", [128, 4, S], BF16)
        dec = so.sb("dec", [128, 4, NT])
        sa = Scope()
        t1 = sa.sb("t1", [128, S])
        t2 = sa.sb("t2", [128, S])
        Gt = sa.sb("G", [128, S])
        eG = sa.sb("eG", [128, S])
        kst = sa.sb("kst", [128, S], BF16)
        scanm = sa.sb("scanm", [128, 1024])
        A("sp", lambda e: e.dma_start(out=scanm[:], in_=scanm_d[:, :]), w=["scanm"], dma=True)
        pp = [sa.ps(f"pp{i}", [128, 2, 512]) for i in range(3)]
        pTk = sa.ps("pTk", [128, 16, 128], BF16)
        ppn = [0]

        def next_pp():
            i = ppn[0] % 3
            ppn[0] += 1
            return pp[i], ("pp", i)

        wb_f = load_wblock(512)
        wb_q = load_wblock(0)
        tq = sa.sb("tq", [128, S])
        HF = (0, 1)
        SL = [slice(hf * HS, (hf + 1) * HS) for hf in HF]
        for h in range(4):
            pf, pq_ = {}, {}
            for hf in HF:
                pf[hf] = next_pp()
                for q2 in range(2):
                    proj_fm(pf[hf][0][:, q2, :], pf[hf][1], wb_f, h, hf * HS + q2 * 512, 512)
            for hf in HF:
                p, pk = pf[hf]
                A("act", lambda e, p=p, sl=SL[hf]: e.activation(out=t1[:, sl], in_=p[:].rearrange("p a b -> p (a b)"),
                                                                func=AF.Sigmoid), r=[pk], w=[("t1", hf)])
            for hf in HF:
                pq_[hf] = next_pp()
                for q2 in range(2):
                    proj_fm(pq_[hf][0][:, q2, :], pq_[hf][1], wb_q, h, hf * HS + q2 * 512, 512)
                p, pk = pq_[hf]
                A("act", lambda e, p=p, sl=SL[hf]: e.activation(out=tq[:, sl], in_=p[:].rearrange("p a b -> p (a b)"),
                                                                func=AF.Sigmoid), r=[pk], w=[("tq", hf)])
            for hf in HF:
                A("dve", lambda e, sl=SL[hf], h=h: e.tensor_scalar(out=t2[:, sl], in0=t1[:, sl], scalar1=lbc[:, 4 + h:5 + h],
                                                                   scalar2=lbc[:, h:h + 1], op0=ALU.mult, op1=ALU.add),
                  r=[("t1", hf), "lb", "lb0"], w=[("t2", hf)])
            for hf in HF:
                A("act", lambda e, sl=SL[hf]: e.activation(out=t1[:, sl], in_=t2[:, sl], func=AF.Ln),
                  r=[("t2", hf)], w=[("t1", hf)])
            for hf in HF:
                A("dve", lambda e, sl=SL[hf]: e.tensor_tensor_scan(out=Gt[:, sl], data0=scanm[:], data1=t1[:, sl],
                                                                   initial=0.0, op0=ALU.mult, op1=ALU.add),
                  r=[("t1", hf), "scanm"], w=[("G", hf)])
                A("pool", lambda e, sl=SL[hf]: e.tensor_scalar(out=t2[:, sl], in0=t2[:, sl], scalar1=-1.0, scalar2=1.0,
                                                               op0=ALU.mult, op1=ALU.add),
                  r=[("t2", hf)], w=[("t2", hf)])
            for hf in HF:
                A("act", lambda e, sl=SL[hf]: e.activation(out=eG[:, sl], in_=Gt[:, sl], func=AF.Exp),
                  r=[("G", hf)], w=[("eG", hf)])
                A("act", lambda e, sl=SL[hf]: e.activation(out=t1[:, sl], in_=Gt[:, sl], func=AF.Exp, scale=-1.0),
                  r=[("G", hf)], w=[("t1", hf)])
            for hf in HF:
                A("dve", lambda e, sl=SL[hf]: e.tensor_mul(out=t2[:, sl], in0=t2[:, sl], in1=t1[:, sl]),
                  r=[("t2", hf), ("t1", hf)], w=[("t2", hf)])
                A("act", lambda e, sl=SL[hf], h=h, hf=hf: e.copy(
                    out=dec[:, h, hf * 8:(hf + 1) * 8],
                    in_=eG[:, sl].rearrange("p (n c) -> p n c", c=128)[:, :, 127]),
                  r=[("eG", hf)], w=[("dec", h, hf)])
            for hf in HF:
                A("dve", lambda e, sl=SL[hf], h=h: e.tensor_copy(out=ki[:, h, sl], in_=t2[:, sl]),
                  r=[("t2", hf)], w=[("ki", h, hf)])
                A("dve", lambda e, sl=SL[hf], h=h, hf=hf: e.tensor_tensor(
                    out=kst[:, sl].rearrange("p (n c) -> p n c", c=128),
                    in0=t2[:, sl].rearrange("p (n c) -> p n c", c=128),
                    in1=dec[:, h, hf * 8:(hf + 1) * 8].unsqueeze(2).to_broadcast([128, 8, 128]), op=ALU.mult),
                  r=[("t2", hf), ("dec", h, hf)], w=[("kst", hf)])
                A("dve", lambda e, sl=SL[hf], h=h: e.tensor_mul(out=qd[:, h, sl], in0=tq[:, sl], in1=eG[:, sl]),
                  r=[("tq", hf), ("eG", hf)], w=[("qd", h, hf)])
            for hf in HF:
                for cc in range(8):
                    n = hf * 8 + cc
                    A("pe", lambda e, n=n: e.transpose(out=pTk[:, n, :], in_=kst[:, n * 128:(n + 1) * 128],
                                                       identity=ident[:]),
                      r=[("kst", hf), "ident"], w=[("pTk", hf)])
                A("act", lambda e, h=h, hf=hf: e.copy(out=ks_tm[:, h, hf * 8:(hf + 1) * 8, :],
                                                      in_=pTk[:, hf * 8:(hf + 1) * 8, :]),
                  r=[("pTk", hf)], w=[("ks_tm", h, hf)])
        wb_o = load_wblock(1536)
        wb_v = load_wblock(1024)
        for h in range(4):
            for hf in range(2):
                sl = slice(hf * HS, (hf + 1) * HS)
                p, pk = next_pp()
                for q2 in range(2):
                    proj_fm(p[:, q2, :], pk, wb_o, h, hf * HS + q2 * 512, 512)
                A("act", lambda e, p=p, sl=sl, h=h: e.activation(out=sog[:, h, sl],
                                                                 in_=p[:].rearrange("p a b -> p (a b)"), func=AF.Silu),
                  r=[pk], w=[("sog", h, hf)])
        for i in range(0, NT, 2):
            p, pk = next_pp()
            for q2 in range(2):
                for c in range(8):
                    A("pe", lambda e, p=p, q2=q2, c=c, i=i: e.matmul(
                        p[:, q2, :], lhsT=hT[:, c, (i + q2) * 128:(i + q2 + 1) * 128], rhs=wbf[wb_v][:, c, :],
                        start=(c == 0), stop=(c == 7)),
                      r=[("wbf", wb_v), ("hT", (i + q2) // 4)], w=[pk])
            A("dve", lambda e, p=p, i=i: e.tensor_copy(out=v_tm[:, i:i + 2, :], in_=p[:]),
              r=[pk], w=[("v_tm", i), ("v_tm", i + 1)])
        sa.close()
        sb_ = Scope()
        maskU = sb_.sb("maskU", [128, 128])
        A("sp", lambda e: e.dma_start(out=maskU[:], in_=maskU_d[:, :]), w=["maskU"], dma=True)
        am = [[sb_.sb(f"am{h}_{i}", [128, 128], BF16) for i in range(2)] for h in range(4)]
        st = sb_.sb("st", [128, 4, 128])
        stb = sb_.sb("stb", [128, 4, 128], BF16)
        sq = [sb_.sb(f"sq{i}", [128, 512], BF16) for i in range(2)]
        sd = [sb_.sb(f"sd{i}", [128, 512]) for i in range(2)]
        yt = [sb_.sb(f"yt{i}", [128, 512]) for i in range(2)]
        pA = sb_.ps("pA", [128, 4, 128])
        pKV = sb_.ps("pKV", [128, 4, 128])
        pO = [sb_.ps(f"pO{h}", [128, 512]) for h in range(4)]
        pM = [sb_.ps(f"pM{i}", [128, 512]) for i in range(2)]
        nm = [0]
        for n in range(NT):
            cs = slice(n * 128, (n + 1) * 128)
            oc = slice((n % 4) * 128, (n % 4 + 1) * 128)
            for h in range(4):
                A("pe", lambda e, h=h, cs=cs: e.matmul(pA[:, h, :], lhsT=ki[:, h, cs], rhs=qd[:, h, cs],
                                                      start=True, stop=True),
                  r=[("ki", h, n // 8), ("qd", h, n // 8)], w=[("pA",)])
            for h in range(4):
                a = am[h][n % 2]
                ak = ("am", h, n % 2)
                A("dve", lambda e, h=h, a=a: e.tensor_tensor(out=a[:], in0=pA[:, h, :], in1=maskU[:], op=ALU.mult),
                  r=[("pA",), "maskU"], w=[ak])
            if n < NT - 1:
                for h in range(4):
                    A("pe", lambda e, h=h, n=n: e.matmul(pKV[:, h, :], lhsT=ks_tm[:, h, n, :],
                                                         rhs=v_tm[:, n, h * 128:(h + 1) * 128], start=True, stop=True),
                      r=[("ks_tm", h, n // 8), ("v_tm", n)], w=[("pKV",)])
            for h in range(4):
                a = am[h][n % 2]
                ak = ("am", h, n % 2)
                A("pe", lambda e, h=h, a=a, oc=oc, n=n: e.matmul(pO[h][:, oc], lhsT=v_tm[:, n, h * 128:(h + 1) * 128],
                                                               rhs=a[:], start=True, stop=(n == 0)),
                  r=[("v_tm", n), ak], w=[("pO", h)])
                if n > 0:
                    A("pe", lambda e, h=h, oc=oc, cs=cs: e.matmul(pO[h][:, oc], lhsT=stb[:, h, :], rhs=qd[:, h, cs],
                                                                 start=False, stop=True),
                      r=[("stb", h), ("qd", h, n // 8)], w=[("pO", h)])
            if n < NT - 1:
                for h in range(4):
                    if n == 0:
                        A("dve", lambda e, h=h: e.tensor_copy(out=st[:, h, :], in_=pKV[:, h, :]),
                          r=[("pKV",)], w=[("st", h)])
                    else:
                        A("dve", lambda e, h=h, n=n: e.scalar_tensor_tensor(
                            out=st[:, h, :], in0=st[:, h, :], scalar=dec[:, h, n:n + 1], in1=pKV[:, h, :],
                            op0=ALU.mult, op1=ALU.add),
                          r=[("pKV",), ("st", h), ("dec", h, n // 8)], w=[("st", h)])
                    A("act", lambda e, h=h: e.copy(out=stb[:, h, :], in_=st[:, h, :]),
                      r=[("st", h)], w=[("stb", h)])
            if n % 4 == 3:
                for h in range(4):
                    g = n // 4
                    i = nm[0] % 2
                    nm[0] += 1
                    gs = slice(g * 512, (g + 1) * 512)
                    A("act", lambda e, h=h, i=i: e.activation(out=sq[i][:], in_=pO[h][:], func=AF.Square),
                      r=[("pO", h)], w=[("sq", i)])
                    A("pe", lambda e, i=i: e.matmul(pM[i][:], lhsT=onesv[:], rhs=sq[i][:], start=True, stop=True),
                      r=[("sq", i), "onesv"], w=[("pM", i)])
                    A("act", lambda e, i=i: e.activation(out=sd[i][:], in_=pM[i][:], func=AF.Ln, bias=cst["epsc"][:, 0:1]),
                      r=[("pM", i), "epsc"], w=[("sd", i)])
                    A("act", lambda e, i=i: e.activation(out=sd[i][:], in_=sd[i][:], func=AF.Exp, scale=-0.5),
                      r=[("sd", i)], w=[("sd", i)])
                    A("dve", lambda e, h=h, i=i: e.scalar_tensor_tensor(
                        out=yt[i][:], in0=pO[h][:], scalar=hgg[:, 0:1], in1=sd[i][:], op0=ALU.mult, op1=ALU.mult),
                      r=[("pO", h), ("sd", i), "hgg"], w=[("yt", i)])
                    A("pool", lambda e, h=h, i=i, gs=gs: e.tensor_tensor(out=yaT[:, h, gs], in0=yt[i][:],
                                                                       in1=sog[:, h, gs], op=ALU.mult),
                      r=[("yt", i), ("sog", h, g // 2)], w=[("yaT", h, g)])
        sb_.close()
        so.close()

    ATT = [(128, 1), (512, 4), (2048, 16)]

    def stage_attn(s):
        wbf = ctx["wbf"]
        hT = ctx["hT"]
        ybT = ctx["ybT"]
        so = Scope()
        ND = so.sb("ND", [128, 4, S])
        def group(g, dl):
            nblk = (S // dl) // 128
            sg_ = Scope()
            QT = sg_.sb("QT", [64, 4, S], BF16)
            KT = sg_.sb("KT", [64, 4, S], BF16)
            Vg = sg_.sb("Vg", [128, 16, 4, 128], BF16)
            A("pool", lambda e: e.memset(Vg[:, :, :, 64:128], 1.0), w=[("Vg1",)])
            sa = Scope()
            sq = [sa.sb(f"asq{i}", [128, 512], BF16) for i in range(4)]
            sd = [sa.sb(f"asd{i}", [128, 512]) for i in range(4)]
            pp = [sa.ps(f"app{i}", [128, 512]) for i in range(4)]
            pM = [sa.ps(f"apM{i}", [128, 512]) for i in range(4)]
            cnt = [0]
            for which, (dst, col0, gain) in enumerate([(QT, 2048 + g * 256, "gq"), (KT, 2816 + g * 256, "gk")]):
                wb = load_wblock(col0, 256)
                for hh in range(4):
                    for tb in range(4):
                        i = cnt[0] % 4
                        p = pp[cnt[0] % 4]
                        pk = ("app", cnt[0] % 4)
                        cnt[0] += 1
                        ts_ = slice(tb * 512, (tb + 1) * 512)
                        for c in range(8):
                            A("pe", lambda e, c=c, p=p, wb=wb, hh=hh, ts_=ts_: e.matmul(
                                p[0:64, :], lhsT=wbf[wb][:, c, hh * 64:(hh + 1) * 64], rhs=hT[:, c, ts_],
                                start=(c == 0), stop=(c == 7)),
                              r=[("wbf", wb), ("hT", tb)], w=[pk])
                        A("act", lambda e, p=p, i=i: e.activation(out=sq[i][0:64, :], in_=p[0:64, :], func=AF.Square),
                          r=[pk], w=[("asq", i)])
                        A("pe", lambda e, i=i: e.matmul(pM[i][0:64, :], lhsT=cst["blk64"][0:64, 0:64], rhs=sq[i][0:64, :],
                                                        start=True, stop=True),
                          r=[("asq", i), "blk64"], w=[("apM", i)])
                        A("act", lambda e, i=i: e.activation(out=sd[i][0:64, :], in_=pM[i][0:64, :], func=AF.Ln, bias=cst["epsc"][0:64, 0:1]),
                          r=[("apM", i), "epsc"], w=[("asd", i)])
                        A("act", lambda e, i=i: e.activation(out=sd[i][0:64, :], in_=sd[i][0:64, :], func=AF.Exp, scale=-0.5),
                          r=[("asd", i)], w=[("asd", i)])
                        A("dve", lambda e, p=p, i=i, dst=dst, hh=hh, ts_=ts_, gain=gain: e.scalar_tensor_tensor(
                            out=dst[:, hh, ts_], in0=p[0:64, :], scalar=cst[gain][0:64, 0:1], in1=sd[i][0:64, :],
                            op0=ALU.mult, op1=ALU.mult),
                          r=[pk, ("asd", i), gain], w=[("QK", which, hh, tb)])
            wb = load_wblock(3584 + g * 256, 256)
            for r_ in range(dl):
                for n in range(nblk):
                    ti = r_ * nblk + n
                    t0 = r_ + dl * 128 * n
                    p = pp[cnt[0] % 4]
                    pk = ("app", cnt[0] % 4)
                    cnt[0] += 1
                    hs = slice(t0, t0 + 127 * dl + 1, dl)
                    for c in range(8):
                        A("pe", lambda e, p=p, c=c, hs=hs, wb=wb: e.matmul(
                            p[:, 0:256], lhsT=hT[:, c, hs], rhs=wbf[wb][:, c, 0:256],
                            start=(c == 0), stop=(c == 7)),
                          r=[("wbf", wb)] + [("hT", k) for k in range(4)], w=[pk])
                    A("dve", lambda e, p=p, ti=ti: e.tensor_copy(
                        out=Vg[:, ti, :, 0:64], in_=p[:, 0:256].rearrange("p (h e) -> p h e", e=64)),
                      r=[pk], w=[("Vg", ti)])
            sa.close()
            sb2 = Scope()
            ef = [sb2.sb(f"ef{i}", [128, 4, 128], BF16) for i in range(4)]
            em = [sb2.sb(f"em{i}", [128, 4, 128], BF16) for i in range(4)]
            pS = [sb2.ps(f"pS{i}", [128, 4, 128]) for i in range(4)]
            pO = [sb2.ps(f"pO{i}", [128, 4, 128]) for i in range(2)]
            kc = [0]
            allQK = [("QK", w_, jj, tb) for w_ in range(2) for jj in range(4) for tb in range(4)]

            def s_phase(r_, n):
                t0 = r_ + dl * 128 * n
                qs = slice(t0, t0 + 127 * dl + 1, dl)
                kbs = ([n - 1] if n > 0 else []) + [n]
                ems = []
                for kb in kbs:
                    i = kc[0] % 4
                    kc[0] += 1
                    k0 = r_ + dl * 128 * kb
                    ks = slice(k0, k0 + 127 * dl + 1, dl)
                    for hh in range(4):
                        A("pe", lambda e, i=i, hh=hh, ks=ks, qs=qs: e.matmul(
                            pS[i][:, hh, :], lhsT=KT[:, hh, ks], rhs=QT[:, hh, qs],
                            start=True, stop=True),
                          r=allQK, w=[("pS", i)])
                    A("act", lambda e, i=i: e.activation(out=ef[i][:], in_=pS[i][:], func=AF.Exp, scale=0.125),
                      r=[("pS", i)], w=[("ef", i)])
                    mname = "maskUb" if kb == n else "maskLb"
                    A("pool", lambda e, i=i, mname=mname: e.tensor_tensor(
                        out=em[i][:], in0=ef[i][:], in1=cst[mname][:].unsqueeze(1).to_broadcast([128, 4, 128]),
                        op=ALU.mult),
                      r=[("ef", i), mname], w=[("em", i)])
                    ems.append((i, r_ * nblk + kb))
                return qs, ems

            def pv_phase(it, qs, ems):
                o = it % 2
                for hh in range(4):
                    for idx, (i, ti) in enumerate(ems):
                        A("pe", lambda e, o=o, hh=hh, i=i, ti=ti, idx=idx, last=(idx == len(ems) - 1): e.matmul(
                            pO[o][:, hh, :], lhsT=Vg[:, ti, hh, :], rhs=em[i][:, hh, :],
                            start=(idx == 0), stop=last),
                          r=[("Vg", ti), ("Vg1",), ("em", i)], w=[("pO", o)])
                if g == 0:
                    A("act", lambda e, o=o, qs=qs: e.copy(out=ND[:, :, qs], in_=pO[o][:]),
                      r=[("pO", o)], w=["ND"])
                else:
                    A("dve", lambda e, o=o, qs=qs: e.tensor_tensor(out=ND[:, :, qs], in0=ND[:, :, qs],
                                                                  in1=pO[o][:], op=ALU.add),
                      r=[("pO", o), "ND"], w=["ND"])

            its = [(r_, n) for r_ in range(dl) for n in range(nblk)]
            prev_ = None
            for k_, (r_, n) in enumerate(its):
                cur_ = s_phase(r_, n)
                if prev_ is not None:
                    pv_phase(k_ - 1, *prev_)
                prev_ = cur_
            pv_phase(len(its) - 1, *prev_)
            sb2.close()
            sg_.close()

        for g, (win, dl) in enumerate(ATT):
            group(g, dl)
        sf = Scope()
        pB = [sf.ps(f"pB{i}", [128, 512]) for i in range(2)]
        shsel = sf.sb("shsel", [128, 64])
        A("sp", lambda e: e.dma_start(out=shsel[:], in_=shsel_d[:, :]), w=["shsel"], dma=True)
        k = 0
        for hh in range(4):
            A("act", lambda e, hh=hh: e.activation(out=ND[64:128, hh, :], in_=ND[64:128, hh, :], func=AF.Ln), r=["ND"], w=["ND"])
            A("act", lambda e, hh=hh: e.activation(out=ND[64:128, hh, :], in_=ND[64:128, hh, :], func=AF.Exp, scale=-1.0),
              r=["ND"], w=["ND"])
            for tb in range(4):
                i = k % 2
                k += 1
                ts_ = slice(tb * 512, (tb + 1) * 512)
                A("pe", lambda e, i=i, hh=hh, ts_=ts_: e.matmul(pB[i][0:64, :], lhsT=shsel[:], rhs=ND[:, hh, ts_],
                                                               start=True, stop=True),
                  r=["ND", "shsel"], w=[("pB", i)])
                A("dve", lambda e, i=i, hh=hh, ts_=ts_: e.tensor_tensor(out=ybT[:, hh, ts_], in0=ND[0:64, hh, ts_],
                                                                       in1=pB[i][0:64, :], op=ALU.mult),
                  r=["ND", ("pB", i)], w=[("ybT", hh, tb)])
        sf.close()
        so.close()

    wa_d = din("w_a", [512, D])
    wb_d = din("w_b", [256, D])
    wout_d = din("w_out", [D, D])

    def stage_merge(s, alloc_xacc):
        hT, yaT, ybT = ctx["hT"], ctx["yaT"], ctx["ybT"]
        sm = Scope()
        mT = sm.sb("mT", [128, 8, S], BF16)
        si = Scope()
        wa = si.sb("wa", [128, 4, D], BF16)
        wbb = si.sb("wbb", [64, 4, D], BF16)
        A("pool", lambda e: e.dma_start(out=wa[:], in_=wa_d.rearrange("(f p) n -> p f n", p=128)), w=["wa"], dma=True)
        A("pool", lambda e: e.dma_start(out=wbb[:], in_=wb_d.rearrange("(h e) n -> e h n", e=64)), w=["wbb"], dma=True)
        sg = [si.sb(f"sg{i}", [128, 512]) for i in range(4)]
        m12 = [si.sb(f"m12{i}", [128, 512]) for i in range(4)]
        pp = [si.ps(f"mpp{i}", [128, 512]) for i in range(4)]
        pab = [si.ps(f"mpab{i}", [128, 512]) for i in range(4)]
        k = 0
        for jb in range(2):
            wga = load_wblock(4352 + jb * 512)
            wgb = load_wblock(5376 + jb * 512)
            for jj in range(4):
                j = jb * 4 + jj
                js = slice(j * 128, (j + 1) * 128)
                for tb in range(4):
                    ts_ = slice(tb * 512, (tb + 1) * 512)
                    ia, ib = (2 * k) % 4, (2 * k + 1) % 4
                    k += 1
                    proj_fm(pp[ia][:], ("mpp", ia), wga, jj, tb * 512, 512)
                    A("act", lambda e, ia=ia: e.activation(out=sg[ia][:], in_=pp[ia][:], func=AF.Sigmoid),
                      r=[("mpp", ia)], w=[("sg", ia)])
                    for f in range(4):
                        A("pe", lambda e, ia=ia, f=f, js=js, ts_=ts_: e.matmul(
                            pab[ia][:], lhsT=wa[:, f, js], rhs=yaT[:, f, ts_], start=(f == 0), stop=(f == 3)),
                          r=["wa", "yaT"], w=[("mpab", ia)])
                    A("dve", lambda e, ia=ia: e.tensor_tensor(out=m12[ia][:], in0=pab[ia][:], in1=sg[ia][:], op=ALU.mult),
                      r=[("mpab", ia), ("sg", ia)], w=[("m12", ia)])
                    proj_fm(pp[ib][:], ("mpp", ib), wgb, jj, tb * 512, 512)
                    A("act", lambda e, ib=ib: e.activation(out=sg[ib][:], in_=pp[ib][:], func=AF.Sigmoid),
                      r=[("mpp", ib)], w=[("sg", ib)])
                    for hh in range(4):
                        A("pe", lambda e, ib=ib, hh=hh, js=js, ts_=ts_: e.matmul(
                            pab[ib][:], lhsT=wbb[:, hh, js], rhs=ybT[:, hh, ts_], start=(hh == 0), stop=(hh == 3)),
                          r=["wbb", "ybT"], w=[("mpab", ib)])
                    A("dve", lambda e, ib=ib: e.tensor_tensor(out=m12[ib][:], in0=pab[ib][:], in1=sg[ib][:], op=ALU.mult),
                      r=[("mpab", ib), ("sg", ib)], w=[("m12", ib)])
                    A("pool", lambda e, ia=ia, ib=ib, j=j, ts_=ts_: e.tensor_tensor(
                        out=mT[:, j, ts_], in0=m12[ia][:], in1=m12[ib][:], op=ALU.add),
                      r=[("m12", ia), ("m12", ib)], w=[("mT", j, tb)])
        si.close()
        alloc_xacc()
        xacc = ctx["xacc"]
        so = Scope()
        wbf = ctx["wbf"]
        wo = [load_wblock(mh * 512, 512, src=wout_d) for mh in range(2)]
        po = [so.ps(f"po{i}", [128, 512]) for i in range(4)]
        k = 0
        for i in range(NT):
            A("sp", lambda e, i=i: e.dma_start(out=xacc[:, i, :], in_=x_d[s, i * 128:(i + 1) * 128, :]),
              w=[("xacc", i)], dma=True)
            for mh in range(2):
                pi = k % 4
                k += 1
                ms_ = slice(mh * 512, (mh + 1) * 512)
                for n in range(8):
                    A("pe", lambda e, pi=pi, n=n, i=i, mh=mh: e.matmul(
                        po[pi][:], lhsT=mT[:, n, i * 128:(i + 1) * 128], rhs=wbf[wo[mh]][:, n, :],
                        start=(n == 0), stop=(n == 7)),
                      r=[("wbf", wo[mh])] + [("mT", n, i // 4)], w=[("po", pi)])
                A("dve", lambda e, pi=pi, i=i, ms_=ms_: e.tensor_tensor(out=xacc[:, i, ms_], in0=xacc[:, i, ms_],
                                                                       in1=po[pi][:], op=ALU.add),
                  r=[("po", pi), ("xacc", i)], w=[("xacc", i)])
        so.close()
        sm.close()

    wq_d = din("peer_wq", [D, D])
    KB_d = din("peer_kb", [128, 8, 256])
    KT12_d = din("peer_kt12", [128, 8, 128])
    uT_d = din("peer_uT", [D, 16384])
    v_d = din("peer_v", [16384, D])
    NEG_BIG = -1.0e30
    EGS = 512
    NEG = 16384 // EGS

    def stage_peer(s, dbg=None):
        h2T, xacc = ctx["hT"], ctx["xacc"]
        sp_ = Scope()
        qpT = sp_.sb("qpT", [128, 8, S], BF16)
        thr = sp_.sb("thr", [128, NT, 8])
        nb = sp_.sb("nb", [128, NT, 8])
        KT12 = sp_.sb("KT12", [128, 8, 128], BF16)
        A("pool", lambda e: e.dma_start(out=KT12[:], in_=KT12_d[:, :, :]), w=["KT12"], dma=True)
        sa = Scope()
        wq = sa.sb("wq", [128, 8, D], BF16)
        KB = sa.sb("KB", [128, 8, 256], BF16)
        A("pool", lambda e: e.dma_start(out=wq[:], in_=wq_d.rearrange("(c p) n -> p c n", p=128)), w=["wq"], dma=True)
        A("pool", lambda e: e.dma_start(out=KB[:], in_=KB_d[:, :, :]), w=["KB"], dma=True)
        s12 = [sa.sb(f"s12_{i}", [128, 8, 256]) for i in range(2)]
        wk = sa.sb("wk", [128, 8, 256])
        t16 = sa.sb("t16", [128, 8, 2, 16])
        cand = sa.sb("cand", [128, 8, 256])
        cand2 = sa.sb("cand2", [128, 8, 256])
        c16 = sa.sb("c16", [128, 8, 16])
        negm = sa.sb("negm", [128, 8])
        dlt = sa.sb("dlt", [128, 8, 16])
        zz = sa.sb("zz", [128, 8])
        pq = [sa.ps(f"pq{i}", [128, 512]) for i in range(2)]
        ps12 = [sa.ps(f"ps12{i}", [128, 2, 256]) for i in range(2)]
        k = 0
        for h in range(8):
            for tb in range(4):
                i = k % 2
                k += 1
                ts_ = slice(tb * 512, (tb + 1) * 512)
                for c in range(8):
                    A("pe", lambda e, i=i, c=c, h=h, ts_=ts_: e.matmul(
                        pq[i][:], lhsT=wq[:, c, h * 128:(h + 1) * 128], rhs=h2T[:, c, ts_],
                        start=(c == 0), stop=(c == 7)),
                      r=["wq", ("hT", tb)], w=[("pq", i)])
                if k % 2 == 0:
                    A("act", lambda e, i=i, h=h, ts_=ts_: e.copy(out=qpT[:, h, ts_], in_=pq[i][:]),
                      r=[("pq", i)], w=[("qpT", h, tb)])
                else:
                    A("dve", lambda e, i=i, h=h, ts_=ts_: e.tensor_copy(out=qpT[:, h, ts_], in_=pq[i][:]),
                      r=[("pq", i)], w=[("qpT", h, tb)])
        k = 0
        for i in range(NT):
            sb_ = s12[i % 2]
            sk = ("s12", i % 2)
            tk = slice(i * 128, (i + 1) * 128)
            for hp in range(4):
                pi = k % 2
                k += 1
                for hh in range(2):
                    h = hp * 2 + hh
                    A("pe", lambda e, pi=pi, hh=hh, h=h, tk=tk: e.matmul(
                        ps12[pi][:, hh, :], lhsT=qpT[:, h, tk], rhs=KB[:, h, :], start=True, stop=True),
                      r=[("qpT", h, i // 4), "KB"], w=[("ps12", pi)])
                A("act", lambda e, pi=pi, hp=hp, sb_=sb_: e.copy(out=sb_[:, hp * 2:hp * 2 + 2, :], in_=ps12[pi][:]),
                  r=[("ps12", pi)], w=[sk])
            hh_ = [(h, half, slice(half * 128, (half + 1) * 128)) for h in range(8) for half in range(2)]
            for h, half, hs_ in hh_:
                A("dve", lambda e, sb_=sb_, h=h, half=half, hs_=hs_: e.max(out=t16[:, h, half, 0:8], in_=sb_[:, h, hs_]),
                  r=[sk], w=[("t16a", h, half)])
            for h, half, hs_ in hh_:
                A("dve", lambda e, sb_=sb_, h=h, half=half, hs_=hs_: e.match_replace(
                    out=wk[:, h, hs_], in_to_replace=t16[:, h, half, 0:8], in_values=sb_[:, h, hs_],
                    imm_value=NEG_BIG),
                  r=[sk, ("t16a", h, half)], w=[("wk", h, half)])
            for h, half, hs_ in hh_:
                A("dve", lambda e, h=h, half=half, hs_=hs_: e.max(out=t16[:, h, half, 8:16], in_=wk[:, h, hs_]),
                  r=[("wk", h, half)], w=[("t16b", h, half)])
            for h in range(8):
                A("pool", lambda e, h=h: e.tensor_tensor(
                    out=cand[:, h, :].rearrange("p (a b) -> p a b", b=16),
                    in0=t16[:, h, 0, :].unsqueeze(2).to_broadcast([128, 16, 16]),
                    in1=t16[:, h, 1, :].unsqueeze(1).to_broadcast([128, 16, 16]), op=ALU.add),
                  r=[("t16a", h, 0), ("t16a", h, 1), ("t16b", h, 0), ("t16b", h, 1)], w=[("cand", h)])
            for h in range(8):
                A("dve", lambda e, h=h: e.max(out=c16[:, h, 0:8], in_=cand[:, h, :]), r=[("cand", h)], w=[("c16a", h)])
            for h in range(8):
                A("dve", lambda e, h=h: e.match_replace(out=cand2[:, h, :], in_to_replace=c16[:, h, 0:8],
                                                        in_values=cand[:, h, :], imm_value=NEG_BIG),
                  r=[("cand", h), ("c16a", h)], w=[("cand2", h)])
            for h in range(8):
                A("dve", lambda e, h=h: e.max(out=c16[:, h, 8:16], in_=cand2[:, h, :]), r=[("cand2", h)], w=[("c16b", h)])
            c16k = [("c16a", h) for h in range(8)] + [("c16b", h) for h in range(8)]
            A("dve", lambda e: e.tensor_scalar(out=negm[:], in0=c16[:, :, 0], scalar1=-1.0, scalar2=None, op0=ALU.mult),
              r=c16k, w=["negm"])
            A("dve", lambda e, i=i: e.tensor_scalar(out=thr[:, i, :], in0=c16[:, :, 15], scalar1=-1.0e-4, scalar2=None,
                                                    op0=ALU.add),
              r=c16k, w=[("thr", i)])
            A("dve", lambda e: e.tensor_tensor(out=dlt[:], in0=c16[:], in1=negm[:].unsqueeze(2).to_broadcast([128, 8, 16]),
                                               op=ALU.add),
              r=c16k + ["negm"], w=["dlt"])
            A("act", lambda e: e.activation(out=dlt[:], in_=dlt[:], func=AF.Exp), r=["dlt"], w=["dlt"])
            A("dve", lambda e: e.reduce_sum(out=zz[:], in_=dlt[:], axis=AX.X), r=["dlt"], w=["zz"])
            A("act", lambda e: e.activation(out=zz[:], in_=zz[:], func=AF.Ln), r=["zz"], w=["zz"])
            A("dve", lambda e, i=i: e.tensor_sub(out=nb[:, i, :], in0=negm[:], in1=zz[:]),
              r=["negm", "zz"], w=[("nb", i)])
        if dbg is not None:
            dthr, dnb, dq = dbg
            A("sp", lambda e: e.dma_start(out=dthr[:, :, :], in_=thr[:]), r=[("thr", i) for i in range(NT)], dma=True)
            A("sp", lambda e: e.dma_start(out=dnb[:, :, :], in_=nb[:]), r=[("nb", i) for i in range(NT)], dma=True)
            A("sp", lambda e: e.dma_start(out=dq[:, :, :], in_=qpT[:]),
              r=[("qpT", h, tb) for h in range(8) for tb in range(4)], dma=True)
        sa.close()
        sm = Scope()
        FKT = sm.sb("FKT", [128, 8, EGS], BF16)
        uT = [sm.sb(f"uT{i}", [128, 8, EGS], BF16) for i in range(2)]
        vv = [sm.sb(f"vv{i}", [128, 4, D], BF16) for i in range(2)]
        G = [sm.sb(f"G{i}", [128, 4, 512], BF16) for i in range(2)]
        E = [sm.sb(f"E{i}", [128, 512], BF16) for i in range(4)]
        EmE = [[sm.sb(f"EmE{j}_{k}", [128, 512], BF16) for k in range(4)] for j in range(2)]
        EmO = [sm.sb(f"EmO{k}", [128, 512], BF16) for k in range(4)]
        AT = [sm.sb(f"AT{i}", [128, 4, 128], BF16) for i in range(2)]
        pH = [sm.ps(f"pH{i}", [128, 512]) for i in range(2)]
        pS = [sm.ps(f"pS{i}", [128, 512]) for i in range(3)]
        pW = [sm.ps(f"pW{i}", [128, 4, 128]) for i in range(1)]
        pOut = [sm.ps(f"pOut{i}", [128, 512]) for i in range(2)]
        Gpre = sm.sb("Gpre", [128, 4, 512], BF16)
        A("pool", lambda e: e.tensor_copy(
            out=FKT[64:128, :, :].rearrange("p h (c i) -> p h c i", i=128),
            in_=KT12[64:128, :, :].unsqueeze(2).to_broadcast([64, 8, 4, 128])),
          r=["KT12"], w=[("FKTb",)])
        cn = {"H": 0, "S": 0, "E": 0, "O": 0}

        def load_group(eg):
            b = eg % 2
            A("pool", lambda e: e.dma_start(
                out=uT[b][:], in_=uT_d[:, eg * EGS:(eg + 1) * EGS].rearrange("(c p) n -> p c n", p=128)),
              w=[("uT", b)], dma=True)
            A("pool", lambda e: e.dma_start(
                out=vv[b][:], in_=v_d[eg * EGS:(eg + 1) * EGS, :].rearrange("(c p) n -> p c n", p=128)),
              w=[("vv", b)], dma=True)

        def fkt_group(eg):
            for hq in range(2):
                A("dve", lambda e, hq=hq: e.tensor_copy(
                    out=FKT[0:64, hq * 4:(hq + 1) * 4, :].rearrange("p h (c i) -> p h c i", i=128),
                    in_=KT12[0:64, hq * 4:(hq + 1) * 4, eg * 4:(eg + 1) * 4].unsqueeze(3).to_broadcast([64, 4, 4, 128])),
                  r=["KT12"], w=[("FKTt", h) for h in range(hq * 4, (hq + 1) * 4)])

        def h_mm(gs, cc, c):
            eg, sbk = gs // 4, gs % 4
            b = eg % 2
            ph = cc % 2
            A("pe", lambda e: e.matmul(
                pH[ph][:], lhsT=uT[b][:, c, cc * 128:(cc + 1) * 128],
                rhs=h2T[:, c, sbk * 512:(sbk + 1) * 512], start=(c == 0), stop=(c == 7)),
              r=[("uT", b), ("hT", sbk)], w=[("pH", ph)])

        def h_copy(cc):
            ph = cc % 2
            A("act", lambda e: e.copy(out=Gpre[:, cc, :], in_=pH[ph][:]), r=[("pH", ph)], w=[("Gpre", cc)])

        def gelu_all(gs):
            A("act", lambda e: e.activation(out=G[gs % 2][:], in_=Gpre[:], func=AF.Gelu),
              r=[("Gpre", cc) for cc in range(4)], w=[("G", gs % 2)])

        def s_head(u, h):
            eg, i = u // NT, u % NT
            tk = slice(i * 128, (i + 1) * 128)
            p_ = cn["S"] % 3
            cn["S"] += 1
            x_ = cn["E"] % 4
            cn["E"] += 1
            es_ = u % 2
            A("pe", lambda e: e.matmul(pS[p_][:], lhsT=qpT[:, h, tk], rhs=FKT[:, h, :], start=True, stop=True),
              r=[("qpT", h, i // 4), ("FKTb",), ("FKTt", h)], w=[("pS", p_)])
            A("act", lambda e: e.activation(out=E[x_][:], in_=pS[p_][:], func=AF.Exp, bias=nb[:, i, h:h + 1]),
              r=[("pS", p_), ("nb", i)], w=[("E", x_)])
            if h % 2 == 0:
                dst, dk = EmE[es_][h // 2], ("EmE", es_, h // 2)
            else:
                dst, dk = EmO[h // 2], ("EmO", h // 2)
            A("dve", lambda e: e.scalar_tensor_tensor(
                out=dst[:], in0=pS[p_][:], scalar=thr[:, i, h:h + 1], in1=E[x_][:],
                op0=ALU.is_ge, op1=ALU.mult),
              r=[("pS", p_), ("E", x_), ("thr", i)], w=[dk])
            if h % 2 == 1:
                pk_ = ("EmE", es_, h // 2)
                A("pool", lambda e: e.tensor_tensor(out=EmE[es_][h // 2][:], in0=EmE[es_][h // 2][:], in1=dst[:],
                                                    op=ALU.add),
                  r=[pk_, dk], w=[pk_])

        def t_piece(u, slot):
            es_ = u % 2
            cc = slot // 2
            for pr in range((slot % 2) * 2, (slot % 2) * 2 + 2):
                A("pe", lambda e, pr=pr: e.matmul(
                    pW[0][:, cc, :], lhsT=EmE[es_][pr][:, cc * 128:(cc + 1) * 128], rhs=ident[:],
                    start=(pr == 0), stop=(pr == 3)),
                  r=[("EmE", es_, pr), "ident"], w=[("pW", 0)])

        def at_piece(u):
            eg, i = u // NT, u % NT
            gs, tt = eg * 4 + i // 4, i % 4
            w_ = u % 2
            A("dve", lambda e: e.tensor_tensor(
                out=AT[w_][:], in0=pW[0][:], in1=G[gs % 2][:, :, tt * 128:(tt + 1) * 128], op=ALU.mult),
              r=[("pW", 0), ("G", gs % 2)], w=[("AT", w_)])

        def o_mm(u, k):
            eg, i = u // NT, u % NT
            b = eg % 2
            w_ = u % 2
            mh, cc = k // 4, k % 4
            ms_ = slice(mh * 512, (mh + 1) * 512)
            A("pe", lambda e: e.matmul(
                pOut[mh][:], lhsT=AT[w_][:, cc, :], rhs=vv[b][:, cc, ms_], start=(cc == 0), stop=(cc == 3)),
              r=[("AT", w_), ("vv", b)], w=[("pOut", mh)])

        tmpO = sm.sb("tmpO", [128, 512])

        def add_piece(u, mh):
            eg, i = u // NT, u % NT
            ms_ = slice(mh * 512, (mh + 1) * 512)
            if mh == 1:
                A("act", lambda e: e.copy(out=tmpO[:], in_=pOut[1][:]), r=[("pOut", 1)], w=["tmpO"])
                if eg < NEG - 1:
                    A("pool", lambda e: e.tensor_tensor(out=xacc[:, i, ms_], in0=xacc[:, i, ms_], in1=tmpO[:], op=ALU.add),
                      r=["tmpO", ("xacc", i)], w=[("xacc", i)])
                else:
                    A("pool", lambda e: e.tensor_tensor(out=ostage[:], in0=xacc[:, i, ms_], in1=tmpO[:], op=ALU.add),
                      r=["tmpO", ("xacc", i)], w=["ostage"])
                    A("sp", lambda e: e.dma_start(out=out_d[s, i * 128:(i + 1) * 128, ms_], in_=ostage[:]),
                      r=["ostage"], dma=True)
                return
            if eg < NEG - 1:
                A("dve", lambda e: e.tensor_tensor(out=xacc[:, i, ms_], in0=xacc[:, i, ms_], in1=pOut[mh][:], op=ALU.add),
                  r=[("pOut", mh), ("xacc", i)], w=[("xacc", i)])
            else:
                A("dve", lambda e: e.tensor_tensor(out=ostage[:], in0=xacc[:, i, ms_], in1=pOut[mh][:], op=ALU.add),
                  r=[("pOut", mh), ("xacc", i)], w=["ostage"])
                A("sp", lambda e: e.dma_start(out=out_d[s, i * 128:(i + 1) * 128, ms_], in_=ostage[:]),
                  r=["ostage"], dma=True)

        NU = NEG * NT
        load_group(0)
        load_group(1)
        for cc in range(4):
            for c in range(8):
                h_mm(0, cc, c)
            h_copy(cc)
        gelu_all(0)
        for v in range(NU + 3):
            cur = v < NU
            tv = v - 1 if 0 <= v - 1 < NU else None
            oa = v - 2 if 0 <= v - 2 < NU else None
            ob = v - 3 if 0 <= v - 3 < NU else None
            hc = None
            if cur:
                eg, i = v // NT, v % NT
                if i == 0:
                    fkt_group(eg)
                if i == 4 and 1 <= eg < NEG - 1:
                    load_group(eg + 1)
                gs, tt = eg * 4 + i // 4, i % 4
                if gs + 1 < NEG * 4:
                    hc = (gs + 1, tt)
            hk = 0
            for h in range(8):
                if cur:
                    s_head(v, h)
                if h < 4 and ob is not None:
                    o_mm(ob, 4 + h)
                if h >= 4 and oa is not None:
                    o_mm(oa, h - 4)
                if hc is not None and h >= 1:
                    h_mm(hc[0], hc[1], hk)
                    hk += 1
                    if h == 7:
                        h_mm(hc[0], hc[1], hk)
                if tv is not None:
                    t_piece(tv, h)
                if ob is not None and h == 4:
                    add_piece(ob, 1)
            if hc is not None:
                h_copy(hc[1])
                if hc[1] == 3:
                    gelu_all(hc[0])
            if tv is not None:
                at_piece(tv)
            if oa is not None:
                add_piece(oa, 0)
        sm.close()
        sp_.close()

    def run_sequence(s, upto="all"):
        sq_ = Scope()
        ctx["hT"] = sq_.sb("hT", [128, 8, S], BF16, side="right")
        sy = Scope()
        ctx["wbf"] = [sy.sb(f"wbf{i}", [128, 8, 512], BF16) for i in range(3)]
        stage_norm(s, "x")
        ctx["yaT"] = sy.sb("yaT", [128, 4, S], BF16)
        stage_hgrn(s)
        if upto == "hgrn":
            return
        ctx["ybT"] = sy.sb("ybT", [64, 4, S], BF16)
        stage_attn(s)
        if upto == "attn":
            return
        def alloc_xacc():
            ctx["xacc"] = sq_.sb("xacc", [128, NT, D], F32, side="right")

        stage_merge(s, alloc_xacc)
        sy.close()
        if upto == "merge":
            return
        stage_norm(s, "xacc")
        if upto == "norm2":
            return
        if upto == "peerprep":
            dthr = dbg_out("thr", [128, NT, 8])
            dnb = dbg_out("nb", [128, NT, 8])
            dq = dbg_out("qpT", [128, 8, S], BF16)
            stage_peer(s, dbg=(dthr, dnb, dq))
        else:
            stage_peer(s)
        sq_.close()

    if debug == "hgrn":
        run_sequence(0, "hgrn")
        dbg_ya = dbg_out("yaT", [128, 4, S], BF16)
        A("sp", lambda e: e.dma_start(out=dbg_ya[:, :, :], in_=ctx["yaT"][:]), dma=True)
    elif debug == "attn":
        run_sequence(0, "attn")
        dbg_ya = dbg_out("yaT", [128, 4, S], BF16)
        A("sp", lambda e: e.dma_start(out=dbg_ya[:, :, :], in_=ctx["yaT"][:]), dma=True)
        dbg_yb = dbg_out("ybT", [64, 4, S], BF16)
        A("sp", lambda e: e.dma_start(out=dbg_yb[:, :, :], in_=ctx["ybT"][:]), dma=True)
    elif debug in ("peerprep", "seq0"):
        run_sequence(0, debug)
    elif debug is None:
        for s_ in range(nseq):
            run_sequence(s_)
    elif debug == "norm2":
        run_sequence(0, "norm2")
        dbg_x1 = dbg_out("x1", [128, NT, D])
        A("sp", lambda e: e.dma_start(out=dbg_x1[:, :, :], in_=ctx["xacc"][:]), dma=True)
        dbg_h2 = dbg_out("h2T", [128, 8, S], BF16)
        A("sp", lambda e: e.dma_start(out=dbg_h2[:, :, :], in_=ctx["hT"][:]), dma=True)

    sc.barrier()

    with ExitStack() as es2:
        sems = {e: es2.enter_context(nc.semaphore("sem_" + e)) for e in Sched.ENGS}
        dma_sems = {e: [es2.enter_context(nc.semaphore(f"dsem_{e}{i}")) for i in range(8)] for e in ("sp", "pool", "act")}
        sc.finalize(sems, dma_sems)
        with nc.Block() as block:
            @block.sync
            def _(E):
                sc.emit_engine("sp", E)

            @block.scalar
            def _(E):
                sc.emit_engine("act", E)

            @block.vector
            def _(E):
                sc.emit_engine("dve", E)

            @block.gpsimd
            def _(E):
                sc.emit_engine("pool", E)

            @block.tensor
            def _(E):
                sc.emit_engine("pe", E)
    try:
        es.close()
    except AssertionError:
        if debug is None:
            raise
    return nc, dbg_outs


def host_consts():
    bf = ml_dtypes.bfloat16
    c = {}
    c["ident"] = np.eye(128, dtype=np.float32).astype(bf)
    i = np.arange(128)
    c["maskU"] = (i[:, None] <= i[None, :]).astype(np.float32)
    c["onesv"] = np.full((128, 128), 1.0 / 128, np.float32).astype(bf)
    sm = np.ones((128, 1024), np.float32)
    sm[:, ::128] = 0.0
    c["scanm"] = sm
    b64 = np.zeros((128, 128), np.float32)
    b64[:64, :64] = 1.0 / 64
    b64[64:, 64:] = 1.0 / 64
    c["blk64"] = b64.astype(bf)
    c["ones64"] = np.ones((128, 64), np.float32).astype(bf)
    c["epsc"] = np.full((128, 1), EPS, np.float32)
    sh = np.zeros((128, 64), np.float32)
    sh[64 + np.arange(64), np.arange(64)] = 1.0
    c["shsel"] = sh
    c["maskUb"] = (i[:, None] <= i[None, :]).astype(np.float32).astype(bf)
    c["maskLb"] = (i[:, None] >= i[None, :]).astype(np.float32).astype(bf)
    return c


def make_in_maps(inputs, nseq=NSEQ, ncores=NCORES):
    x = np.ascontiguousarray(inputs["x"], dtype=np.float32)
    consts = host_consts()
    w_in = np.ascontiguousarray(inputs["w_in"][0], dtype=np.float32)
    g1bc = np.ascontiguousarray(np.broadcast_to(inputs["norm1_g"][0][None, :], (128, D)), dtype=np.float32)
    g2bc = np.ascontiguousarray(np.broadcast_to(inputs["norm2_g"][0][None, :], (128, D)), dtype=np.float32)
    w_a = np.ascontiguousarray(inputs["w_branch_a"][0], dtype=np.float32)
    w_b = np.ascontiguousarray(inputs["w_branch_b"][0], dtype=np.float32)
    w_out = np.ascontiguousarray(inputs["w_out"][0], dtype=np.float32)
    lg = np.asarray(inputs["hg_lb_logits"], dtype=np.float32)
    lbl = np.ascontiguousarray(np.concatenate([lg[0].reshape(4, 128).T, lg[1].reshape(4, 128).T], axis=1))
    hgg = np.ascontiguousarray(inputs["hg_norm_g"][0].reshape(128, 1), dtype=np.float32)
    gq = np.ascontiguousarray(np.tile(inputs["q_norm_g"][0], 2).reshape(128, 1), dtype=np.float32)
    gk = np.ascontiguousarray(np.tile(inputs["k_norm_g"][0], 2).reshape(128, 1), dtype=np.float32)
    wq = np.ascontiguousarray(inputs["peer_wq"][0], dtype=np.float32)
    sk = np.asarray(inputs["peer_subkeys"][0], dtype=np.float32)
    kt12 = np.ascontiguousarray(np.concatenate([sk[:, 0].transpose(2, 0, 1), sk[:, 1].transpose(2, 0, 1)], axis=0))
    kb = np.zeros((128, 8, 256), np.float32)
    kb[0:64, :, 0:128] = kt12[0:64]
    kb[64:128, :, 128:256] = kt12[64:128]
    uT = np.ascontiguousarray(inputs["peer_u"][0].T, dtype=np.float32)
    pv = np.ascontiguousarray(inputs["peer_v"][0], dtype=np.float32)
    maps = []
    for c in range(ncores):
        m = {"x": x[c * nseq:(c + 1) * nseq], "w_in": w_in, "g1bc": g1bc, "g2bc": g2bc, "lbl": lbl, "hgg": hgg,
             "gq": gq, "gk": gk, "w_a": w_a, "w_b": w_b, "w_out": w_out,
             "peer_wq": wq, "peer_kb": kb, "peer_kt12": kt12, "peer_uT": uT, "peer_v": pv}
        m.update(consts)
        maps.append(m)
    return maps


def kernel(**inputs):
    nc, _ = build_program()
    in_maps = make_in_maps(inputs)
    res = run_bass_kernel_spmd(nc, in_maps, core_ids=list(range(NCORES)))
    return np.concatenate([r["out"] for r in res.results], axis=0)
```

```python
import numpy as np
import ml_dtypes
import concourse.bass as bass
import concourse.mybir as mybir
from concourse.bass_utils import run_bass_kernel_spmd

F32 = mybir.dt.float32
BF16 = mybir.dt.bfloat16
AF = mybir.ActivationFunctionType
ALU = mybir.AluOpType
AX = mybir.AxisListType

NCORES = 8
D = 1024
S = 2048
NSEQ = 2
NT = S // 128
EPS = 1e-6
INC = 6400


class _Op:
    __slots__ = ("id", "eng", "fn", "deps", "dma", "needed", "sem", "val", "prewait", "waits")

    def __init__(self, id, eng, fn, dma):
        self.id = id
        self.eng = eng
        self.fn = fn
        self.deps = {}
        self.dma = dma
        self.needed = False
        self.sem = None
        self.val = 0
        self.prewait = None
        self.waits = []


class Sched:
    ENGS = ("pe", "act", "dve", "pool", "sp")

    def __init__(self):
        self.ops = []
        self.lastw = {}
        self.readers = {}
        self.last_eng_op = {}
        self.dma_since_barrier = []

    def add(self, eng, fn, reads=(), writes=(), dma=False):
        op = _Op(len(self.ops), eng, fn, dma)
        deps = op.deps
        for k in reads:
            w = self.lastw.get(k)
            if w is not None:
                deps[w] = True
        for k in writes:
            w = self.lastw.get(k)
            if w is not None:
                deps.setdefault(w, False)
            rd = self.readers.get(k)
            if rd:
                for r in rd.values():
                    deps.setdefault(r, False)
        for k in writes:
            self.lastw[k] = op.id
            self.readers[k] = {}
        for k in reads:
            rd = self.readers.setdefault(k, {})
            rd[("dma", op.id) if dma else eng] = op.id
        for d in list(deps):
            p = self.ops[d]
            if p.dma or dma:
                continue
            if p.eng == eng and (eng == "pe" or not deps[d]):
                del deps[d]
        self.ops.append(op)
        if dma:
            self.dma_since_barrier.append(op.id)
        elif fn is not None:
            self.last_eng_op[eng] = op.id
        return op

    def barrier(self):
        targets = list(self.last_eng_op.values()) + list(self.dma_since_barrier)
        for e in self.ENGS:
            op = _Op(len(self.ops), e, None, False)
            for t in targets:
                op.deps[t] = True
            self.ops.append(op)
        self.lastw = {}
        self.readers = {}
        self.dma_since_barrier = []

    def finalize(self, sems, dma_sems):
        for op in self.ops:
            for d in op.deps:
                self.ops[d].needed = True
        cnt = {e: 0 for e in self.ENGS}
        dcnt = {e: 0 for e in self.ENGS}
        for op in self.ops:
            if op.dma:
                j = dcnt[op.eng]
                dcnt[op.eng] += 1
                pool = dma_sems[op.eng]
                op.sem = pool[j % len(pool)]
                op.val = 16 * (j // len(pool) + 1)
                if op.val > 16:
                    op.prewait = (op.sem, op.val - 16)
            elif op.needed:
                cnt[op.eng] += 1
                op.sem = sems[op.eng]
                op.val = cnt[op.eng]
        know = {e: {} for e in self.ENGS}
        prod_know = {}
        for op in self.ops:
            K = know[op.eng]
            need = {}
            for d in sorted(op.deps, reverse=True):
                p = self.ops[d]
                key = id(p.sem)
                if key not in need or need[key][1] < p.val:
                    need[key] = (p.sem, p.val)
            if op.prewait is not None:
                key = id(op.prewait[0])
                if key not in need or need[key][1] < op.prewait[1]:
                    need[key] = op.prewait
            waits = []
            for key, (sm, v) in need.items():
                if K.get(key, 0) >= v:
                    continue
                waits.append((sm, v))
                K[key] = v
                pk = prod_know.get((key, v))
                if pk:
                    for k2, v2 in pk.items():
                        if K.get(k2, 0) < v2:
                            K[k2] = v2
            op.waits = waits
            if op.sem is not None and (op.dma or op.needed):
                snap = dict(K)
                if not op.dma:
                    snap[id(op.sem)] = op.val
                prod_know[(id(op.sem), op.val)] = snap

    def emit_engine(self, eng, E):
        for op in self.ops:
            if op.eng != eng:
                continue
            for (sm, v) in op.waits:
                E.wait_ge(sm, v)
            if op.fn is None:
                continue
            ins = op.fn(E)
            if op.dma:
                ins.then_inc(op.sem, 16)
            elif op.needed:
                ins.then_inc(op.sem, 1)


def build_program(debug=None, nseq=NSEQ):
    from contextlib import ExitStack

    nc = bass.Bass("TRN2", target_bir_lowering=False)
    sc = Sched()
    dbg_outs = {}

    def din(name, shape, dt=F32):
        return nc.dram_tensor(name, list(shape), dt, kind="ExternalInput").ap()

    x_d = din("x", [nseq, S, D])
    win_d = din("w_in", [D, INC])
    out_d = nc.dram_tensor("out", [nseq, S, D], F32, kind="ExternalOutput").ap()

    def dbg_out(name, shape, dt=F32):
        t = nc.dram_tensor("dbg_" + name, list(shape), dt, kind="ExternalOutput").ap()
        dbg_outs[name] = t
        return t

    es = ExitStack()

    def sb(name, shape, dt=F32):
        return es.enter_context(nc.sbuf_tensor("sb_" + name, list(shape), dt))

    def ps(name, shape, dt=F32):
        return es.enter_context(nc.psum_tensor("ps_" + name, list(shape), dt))

    def A(eng, fn, r=(), w=(), dma=False):
        return sc.add(eng, fn, r, w, dma)

    uniq = [0]

    class Scope:
        def __init__(self):
            self.es = ExitStack()

        def sb(self, name, shape, dt=F32, side=None):
            uniq[0] += 1
            return self.es.enter_context(nc.sbuf_tensor(f"sb_{name}_{uniq[0]}", list(shape), dt, side=side))

        def ps(self, name, shape, dt=F32):
            uniq[0] += 1
            return self.es.enter_context(nc.psum_tensor(f"ps_{name}_{uniq[0]}", list(shape), dt))

        def close(self):
            sc.barrier()
            self.es.close()

    cst = {}

    def load_const(name, shape, dt=F32):
        d = din(name, shape, dt)
        t = sb("c_" + name, shape, dt)
        if len(shape) == 2:
            A("sp", lambda e: e.dma_start(out=t[:], in_=d[:, :]), w=[name], dma=True)
        else:
            A("sp", lambda e: e.dma_start(out=t[:], in_=d[:, :, :]), w=[name], dma=True)
        cst[name] = t
        return t

    ident = load_const("ident", [128, 128], BF16)
    maskU_d = din("maskU", [128, 128])
    onesv = load_const("onesv", [128, 128], BF16)
    scanm_d = din("scanm", [128, 1024])
    lbl = load_const("lbl", [128, 8])
    hgg = load_const("hgg", [128, 1])
    load_const("blk64", [128, 128], BF16)
    load_const("ones64", [128, 64], BF16)
    load_const("maskUb", [128, 128], BF16)
    load_const("maskLb", [128, 128], BF16)
    shsel_d = din("shsel", [128, 64])
    load_const("epsc", [128, 1])
    load_const("gq", [128, 1])
    load_const("gk", [128, 1])
    lbc = sb("lbc", [128, 12])
    A("dve", lambda e: e.tensor_sub(out=lbc[:, 8:12], in0=lbl[:, 0:4], in1=lbl[:, 4:8]), r=["lbl"], w=["lbtmp"])
    A("act", lambda e: e.activation(out=lbc[:, 0:4], in_=lbc[:, 8:12], func=AF.Sigmoid), r=["lbtmp"], w=["lb0"])
    A("dve", lambda e: e.tensor_scalar(out=lbc[:, 4:8], in0=lbc[:, 0:4], scalar1=-1.0, scalar2=1.0,
                                       op0=ALU.mult, op1=ALU.add), r=["lb0"], w=["lb"])

    g1bc_d = din("g1bc", [128, D])
    g2bc_d = din("g2bc", [128, D])
    ctx = {}
    ostage = sb("ostage", [128, 512])

    def stage_norm(s, src):
        hT = ctx["hT"]
        sp_ = Scope()
        gbc = sp_.sb("gbc", [128, D])
        gsrc = g1bc_d if src == "x" else g2bc_d
        A("sp", lambda e: e.dma_start(out=gbc[:], in_=gsrc[:, :]), w=["gbc"], dma=True)
        xt = [sp_.sb(f"xt{i}", [128, D]) for i in range(2)]
        junk = sp_.sb("junk", [128, D], BF16)
        xn = [sp_.sb(f"xn{i}", [128, D], BF16) for i in range(2)]
        stat = sp_.sb("stat", [128, 4, 4])
        pT = [sp_.ps(f"pT{i}", [128, 8, 128], BF16) for i in range(2)]
        def stage_a(i):
            b = i % 2
            if src == "x":
                A("sp", lambda e: e.dma_start(out=xt[b][:], in_=x_d[s, i * 128:(i + 1) * 128, :]),
                  w=[("xt", b)], dma=True)
                xin = xt[b][:]
                xk = ("xt", b)
            else:
                xin = ctx["xacc"][:, i, :]
                xk = ("xacc", i)
            q4 = i % 4
            A("act", lambda e: e.activation(out=junk[:], in_=xin, func=AF.Square, accum_out=stat[:, q4, 0:1]),
              r=[xk], w=[("stat", q4, 0)])
            A("act", lambda e: e.activation(out=stat[:, q4, 1:2], in_=stat[:, q4, 0:1], func=AF.Sqrt,
                                            bias=EPS, scale=1.0 / D),
              r=[("stat", q4, 0)], w=[("stat", q4, 1)])
            A("dve", lambda e: e.reciprocal(out=stat[:, q4, 2:3], in_=stat[:, q4, 1:2]),
              r=[("stat", q4, 1)], w=[("stat", q4, 2)])
            A("dve", lambda e: e.scalar_tensor_tensor(
                out=xn[b][:], in0=xin, scalar=stat[:, q4, 2:3], in1=gbc[:], op0=ALU.mult, op1=ALU.mult),
              r=[xk, ("stat", q4, 2), "gbc"], w=[("xn", b)])
            for c in range(8):
                A("pe", lambda e, c=c: e.transpose(out=pT[b][:, c, :], in_=xn[b][:, c * 128:(c + 1) * 128],
                                                   identity=ident[:]),
                  r=[("xn", b), "ident"], w=[("pT", b)])

        def stage_b(i):
            b = i % 2
            A("act", lambda e: e.copy(out=hT[:, :, i * 128:(i + 1) * 128], in_=pT[b][:]),
              r=[("pT", b)], w=[("hT", i // 4)])

        stage_a(0)
        for i in range(NT):
            if i + 1 < NT:
                stage_a(i + 1)
            stage_b(i)
        sp_.close()

    wload_n = [0]

    def load_wblock(col0, ncols=512, src=None):
        src = win_d if src is None else src
        wbf = ctx["wbf"]
        b = wload_n[0] % 3
        wload_n[0] += 1
        A("pool", lambda e: e.dma_start(out=wbf[b][:, :, :ncols],
                                        in_=src[:, col0:col0 + ncols].rearrange("(c p) n -> p c n", p=128)),
          w=[("wbf", b)], dma=True)
        return b

    def proj_fm(pout, pkey, wb, j, t0, nt):
        hT = ctx["hT"]
        wbf = ctx["wbf"]
        for c in range(8):
            A("pe", lambda e, c=c: e.matmul(pout, lhsT=wbf[wb][:, c, j * 128:(j + 1) * 128],
                                            rhs=hT[:, c, t0:t0 + nt], start=(c == 0), stop=(c == 7)),
              r=[("wbf", wb)] + [("hT", k) for k in range(t0 // 512, (t0 + nt - 1) // 512 + 1)], w=[pkey])

    HS = 1024

    def stage_hgrn(s):
        wbf = ctx["wbf"]
        hT = ctx["hT"]
        yaT = ctx["yaT"]
        so = Scope()
        qd = so.sb("qd", [128, 4, S], BF16)
        ki = so.sb("ki", [128, 4, S], BF16)
        ks_tm = so.sb("ks_tm", [128, 4, NT, 128], BF16)
        v_tm = so.sb("v_tm", [128, NT, 512], BF16)
        sog = so.sb("sog", [128, 4, S], BF16)
        dec = so.sb("dec", [128, 4, NT])
        sa = Scope()
        t1 = sa.sb("t1", [128, S])
        t2 = sa.sb("t2", [128, S])
        Gt = sa.sb("G", [128, S])
        eG = sa.sb("eG", [128, S])
        kst = sa.sb("kst", [128, S], BF16)
        scanm = sa.sb("scanm", [128, 1024])
        A("sp", lambda e: e.dma_start(out=scanm[:], in_=scanm_d[:, :]), w=["scanm"], dma=True)
        pp = [sa.ps(f"pp{i}", [128, 2, 512]) for i in range(3)]
        pTk = sa.ps("pTk", [128, 16, 128], BF16)
        ppn = [0]

        def next_pp():
            i = ppn[0] % 3
            ppn[0] += 1
            return pp[i], ("pp", i)

        wb_f = load_wblock(512)
        wb_q = load_wblock(0)
        tq = sa.sb("tq", [128, S])
        HF = (0, 1)
        SL = [slice(hf * HS, (hf + 1) * HS) for hf in HF]
        for h in range(4):
            pf, pq_ = {}, {}
            for hf in HF:
                pf[hf] = next_pp()
                for q2 in range(2):
                    proj_fm(pf[hf][0][:, q2, :], pf[hf][1], wb_f, h, hf * HS + q2 * 512, 512)
            for hf in HF:
                p, pk = pf[hf]
                A("act", lambda e, p=p, sl=SL[hf]: e.activation(out=t1[:, sl], in_=p[:].rearrange("p a b -> p (a b)"),
                                                                func=AF.Sigmoid), r=[pk], w=[("t1", hf)])
            for hf in HF:
                pq_[hf] = next_pp()
                for q2 in range(2):
                    proj_fm(pq_[hf][0][:, q2, :], pq_[hf][1], wb_q, h, hf * HS + q2 * 512, 512)
                p, pk = pq_[hf]
                A("act", lambda e, p=p, sl=SL[hf]: e.activation(out=tq[:, sl], in_=p[:].rearrange("p a b -> p (a b)"),
                                                                func=AF.Sigmoid), r=[pk], w=[("tq", hf)])
            for hf in HF:
                A("dve", lambda e, sl=SL[hf], h=h: e.tensor_scalar(out=t2[:, sl], in0=t1[:, sl], scalar1=lbc[:, 4 + h:5 + h],
                                                                   scalar2=lbc[:, h:h + 1], op0=ALU.mult, op1=ALU.add),
                  r=[("t1", hf), "lb", "lb0"], w=[("t2", hf)])
            for hf in HF:
                A("act", lambda e, sl=SL[hf]: e.activation(out=t1[:, sl], in_=t2[:, sl], func=AF.Ln),
                  r=[("t2", hf)], w=[("t1", hf)])
            for hf in HF:
                A("dve", lambda e, sl=SL[hf]: e.tensor_tensor_scan(out=Gt[:, sl], data0=scanm[:], data1=t1[:, sl],
                                                                   initial=0.0, op0=ALU.mult, op1=ALU.add),
                  r=[("t1", hf), "scanm"], w=[("G", hf)])
                A("pool", lambda e, sl=SL[hf]: e.tensor_scalar(out=t2[:, sl], in0=t2[:, sl], scalar1=-1.0, scalar2=1.0,
                                                               op0=ALU.mult, op1=ALU.add),
                  r=[("t2", hf)], w=[("t2", hf)])
            for hf in HF:
                A("act", lambda e, sl=SL[hf]: e.activation(out=eG[:, sl], in_=Gt[:, sl], func=AF.Exp),
                  r=[("G", hf)], w=[("eG", hf)])
                A("act", lambda e, sl=SL[hf]: e.activation(out=t1[:, sl], in_=Gt[:, sl], func=AF.Exp, scale=-1.0),
                  r=[("G", hf)], w=[("t1", hf)])
            for hf in HF:
                A("dve", lambda e, sl=SL[hf]: e.tensor_mul(out=t2[:, sl], in0=t2[:, sl], in1=t1[:, sl]),
                  r=[("t2", hf), ("t1", hf)], w=[("t2", hf)])
                A("act", lambda e, sl=SL[hf], h=h, hf=hf: e.copy(
                    out=dec[:, h, hf * 8:(hf + 1) * 8],
                    in_=eG[:, sl].rearrange("p (n c) -> p n c", c=128)[:, :, 127]),
                  r=[("eG", hf)], w=[("dec", h, hf)])
            for hf in HF:
                A("dve", lambda e, sl=SL[hf], h=h: e.tensor_copy(out=ki[:, h, sl], in_=t2[:, sl]),
                  r=[("t2", hf)], w=[("ki", h, hf)])
                A("dve", lambda e, sl=SL[hf], h=h, hf=hf: e.tensor_tensor(
                    out=kst[:, sl].rearrange("p (n c) -> p n c", c=128),
                    in0=t2[:, sl].rearrange("p (n c) -> p n c", c=128),
                    in1=dec[:, h, hf * 8:(hf + 1) * 8].unsqueeze(2).to_broadcast([128, 8, 128]), op=ALU.mult),
                  r=[("t2", hf), ("dec", h, hf)], w=[("kst", hf)])
                A("dve", lambda e, sl=SL[hf], h=h: e.tensor_mul(out=qd[:, h, sl], in0=tq[:, sl], in1=eG[:, sl]),
                  r=[("tq", hf), ("eG", hf)], w=[("qd", h, hf)])
            for hf in HF:
                for cc in range(8):
                    n = hf * 8 + cc
                    A("pe", lambda e, n=n: e.transpose(out=pTk[:, n, :], in_=kst[:, n * 128:(n + 1) * 128],
                                                       identity=ident[:]),
                      r=[("kst", hf), "ident"], w=[("pTk", hf)])
                A("act", lambda e, h=h, hf=hf: e.copy(out=ks_tm[:, h, hf * 8:(hf + 1) * 8, :],
                                                      in_=pTk[:, hf * 8:(hf + 1) * 8, :]),
                  r=[("pTk", hf)], w=[("ks_tm", h, hf)])
        wb_o = load_wblock(1536)
        wb_v = load_wblock(1024)
        for h in range(4):
            for hf in range(2):
                sl = slice(hf * HS, (hf + 1) * HS)
                p, pk = next_pp()
                for q2 in range(2):
                    proj_fm(p[:, q2, :], pk, wb_o, h, hf * HS + q2 * 512, 512)
                A("act", lambda e, p=p, sl=sl, h=h: e.activation(out=sog[:, h, sl],
                                                                 in_=p[:].rearrange("p a b -> p (a b)"), func=AF.Silu),
                  r=[pk], w=[("sog", h, hf)])
        for i in range(0, NT, 2):
            p, pk = next_pp()
            for q2 in range(2):
                for c in range(8):
                    A("pe", lambda e, p=p, q2=q2, c=c, i=i: e.matmul(
                        p[:, q2, :], lhsT=hT[:, c, (i + q2) * 128:(i + q2 + 1) * 128], rhs=wbf[wb_v][:, c, :],
                        start=(c == 0), stop=(c == 7)),
                      r=[("wbf", wb_v), ("hT", (i + q2) // 4)], w=[pk])
            A("dve", lambda e, p=p, i=i: e.tensor_copy(out=v_tm[:, i:i + 2, :], in_=p[:]),
              r=[pk], w=[("v_tm", i), ("v_tm", i + 1)])
        sa.close()
        sb_ = Scope()
        maskU = sb_.sb("maskU", [128, 128])
        A("sp", lambda e: e.dma_start(out=maskU[:], in_=maskU_d[:, :]), w=["maskU"], dma=True)
        am = [[sb_.sb(f"am{h}_{i}", [128, 128], BF16) for i in range(2)] for h in range(4)]
        st = sb_.sb("st", [128, 4, 128])
        stb = sb_.sb("stb", [128, 4, 128], BF16)
        sq = [sb_.sb(f"sq{i}", [128, 512], BF16) for i in range(2)]
        sd = [sb_.sb(f"sd{i}", [128, 512]) for i in range(2)]
        yt = [sb_.sb(f"yt{i}", [128, 512]) for i in range(2)]
        pA = sb_.ps("pA", [128, 4, 128])
        pKV = sb_.ps("pKV", [128, 4, 128])
        pO = [sb_.ps(f"pO{h}", [128, 512]) for h in range(4)]
        pM = [sb_.ps(f"pM{i}", [128, 512]) for i in range(2)]
        nm = [0]
        for n in range(NT):
            cs = slice(n * 128, (n + 1) * 128)
            oc = slice((n % 4) * 128, (n % 4 + 1) * 128)
            for h in range(4):
                A("pe", lambda e, h=h, cs=cs: e.matmul(pA[:, h, :], lhsT=ki[:, h, cs], rhs=qd[:, h, cs],
                                                      start=True, stop=True),
                  r=[("ki", h, n // 8), ("qd", h, n // 8)], w=[("pA",)])
            for h in range(4):
                a = am[h][n % 2]
                ak = ("am", h, n % 2)
                A("dve", lambda e, h=h, a=a: e.tensor_tensor(out=a[:], in0=pA[:, h, :], in1=maskU[:], op=ALU.mult),
                  r=[("pA",), "maskU"], w=[ak])
            if n < NT - 1:
                for h in range(4):
                    A("pe", lambda e, h=h, n=n: e.matmul(pKV[:, h, :], lhsT=ks_tm[:, h, n, :],
                                                         rhs=v_tm[:, n, h * 128:(h + 1) * 128], start=True, stop=True),
                      r=[("ks_tm", h, n // 8), ("v_tm", n)], w=[("pKV",)])
            for h in range(4):
                a = am[h][n % 2]
                ak = ("am", h, n % 2)
                A("pe", lambda e, h=h, a=a, oc=oc, n=n: e.matmul(pO[h][:, oc], lhsT=v_tm[:, n, h * 128:(h + 1) * 128],
                                                               rhs=a[:], start=True, stop=(n == 0)),
                  r=[("v_tm", n), ak], w=[("pO", h)])
                if n > 0:
                    A("pe", lambda e, h=h, oc=oc, cs=cs: e.matmul(pO[h][:, oc], lhsT=stb[:, h, :], rhs=qd[:, h, cs],
                                                                 start=False, stop=True),
                      r=[("stb", h), ("qd", h, n // 8)], w=[("pO", h)])
            if n < NT - 1:
                for h in range(4):
                    if n == 0:
                        A("dve", lambda e, h=h: e.tensor_copy(out=st[:, h, :], in_=pKV[:, h, :]),
                          r=[("pKV",)], w=[("st", h)])
                    else:
                        A("dve", lambda e, h=h, n=n: e.scalar_tensor_tensor(
                            out=st[:, h, :], in0=st[:, h, :], scalar=dec[:, h, n:n + 1], in1=pKV[:, h, :],
                            op0=ALU.mult, op1=ALU.add),
                          r=[("pKV",), ("st", h), ("dec", h, n // 8)], w=[("st", h)])
                    A("act", lambda e, h=h: e.copy(out=stb[:, h, :], in_=st[:, h, :]),
                      r=[("st", h)], w=[("stb", h)])
            if n % 4 == 3:
                for h in range(4):
                    g = n // 4
                    i = nm[0] % 2
                    nm[0] += 1
                    gs = slice(g * 512, (g + 1) * 512)
                    A("act", lambda e, h=h, i=i: e.activation(out=sq[i][:], in_=pO[h][:], func=AF.Square),
                      r=[("pO", h)], w=[("sq", i)])
                    A("pe", lambda e, i=i: e.matmul(pM[i][:], lhsT=onesv[:], rhs=sq[i][:], start=True, stop=True),
                      r=[("sq", i), "onesv"], w=[("pM", i)])
                    A("act", lambda e, i=i: e.activation(out=sd[i][:], in_=pM[i][:], func=AF.Ln, bias=cst["epsc"][:, 0:1]),
                      r=[("pM", i), "epsc"], w=[("sd", i)])
                    A("act", lambda e, i=i: e.activation(out=sd[i][:], in_=sd[i][:], func=AF.Exp, scale=-0.5),
                      r=[("sd", i)], w=[("sd", i)])
                    A("dve", lambda e, h=h, i=i: e.scalar_tensor_tensor(
                        out=yt[i][:], in0=pO[h][:], scalar=hgg[:, 0:1], in1=sd[i][:], op0=ALU.mult, op1=ALU.mult),
                      r=[("pO", h), ("sd", i), "hgg"], w=[("yt", i)])
                    A("pool", lambda e, h=h, i=i, gs=gs: e.tensor_tensor(out=yaT[:, h, gs], in0=yt[i][:],
                                                                       in1=sog[:, h, gs], op=ALU.mult),
                      r=[("yt", i), ("sog", h, g // 2)], w=[("yaT", h, g)])
        sb_.close()
        so.close()

    ATT = [(128, 1), (512, 4), (2048, 16)]

    def stage_attn(s):
        wbf = ctx["wbf"]
        hT = ctx["hT"]
        ybT = ctx["ybT"]
        so = Scope()
        ND = so.sb("ND", [128, 4, S])
        def group(g, dl):
            nblk = (S // dl) // 128
            sg_ = Scope()
            QT = sg_.sb("QT", [64, 4, S], BF16)
            KT = sg_.sb("KT", [64, 4, S], BF16)
            Vg = sg_.sb("Vg", [128, 16, 4, 128], BF16)
            A("pool", lambda e: e.memset(Vg[:, :, :, 64:128], 1.0), w=[("Vg1",)])
            sa = Scope()
            sq = [sa.sb(f"asq{i}", [128, 512], BF16) for i in range(4)]
            sd = [sa.sb(f"asd{i}", [128, 512]) for i in range(4)]
            pp = [sa.ps(f"app{i}", [128, 512]) for i in range(4)]
            pM = [sa.ps(f"apM{i}", [128, 512]) for i in range(4)]
            cnt = [0]
            for which, (dst, col0, gain) in enumerate([(QT, 2048 + g * 256, "gq"), (KT, 2816 + g * 256, "gk")]):
                wb = load_wblock(col0, 256)
                for hh in range(4):
                    for tb in range(4):
                        i = cnt[0] % 4
                        p = pp[cnt[0] % 4]
                        pk = ("app", cnt[0] % 4)
                        cnt[0] += 1
                        ts_ = slice(tb * 512, (tb + 1) * 512)
                        for c in range(8):
                            A("pe", lambda e, c=c, p=p, wb=wb, hh=hh, ts_=ts_: e.matmul(
                                p[0:64, :], lhsT=wbf[wb][:, c, hh * 64:(hh + 1) * 64], rhs=hT[:, c, ts_],
                                start=(c == 0), stop=(c == 7)),
                              r=[("wbf", wb), ("hT", tb)], w=[pk])
                        A("act", lambda e, p=p, i=i: e.activation(out=sq[i][0:64, :], in_=p[0:64, :], func=AF.Square),
                          r=[pk], w=[("asq", i)])
                        A("pe", lambda e, i=i: e.matmul(pM[i][0:64, :], lhsT=cst["blk64"][0:64, 0:64], rhs=sq[i][0:64, :],
                                                        start=True, stop=True),
                          r=[("asq", i), "blk64"], w=[("apM", i)])
                        A("act", lambda e, i=i: e.activation(out=sd[i][0:64, :], in_=pM[i][0:64, :], func=AF.Ln, bias=cst["epsc"][0:64, 0:1]),
                          r=[("apM", i), "epsc"], w=[("asd", i)])
                        A("act", lambda e, i=i: e.activation(out=sd[i][0:64, :], in_=sd[i][0:64, :], func=AF.Exp, scale=-0.5),
                          r=[("asd", i)], w=[("asd", i)])
                        A("dve", lambda e, p=p, i=i, dst=dst, hh=hh, ts_=ts_, gain=gain: e.scalar_tensor_tensor(
                            out=dst[:, hh, ts_], in0=p[0:64, :], scalar=cst[gain][0:64, 0:1], in1=sd[i][0:64, :],
                            op0=ALU.mult, op1=ALU.mult),
                          r=[pk, ("asd", i), gain], w=[("QK", which, hh, tb)])
            wb = load_wblock(3584 + g * 256, 256)
            for r_ in range(dl):
                for n in range(nblk):
                    ti = r_ * nblk + n
                    t0 = r_ + dl * 128 * n
                    p = pp[cnt[0] % 4]
                    pk = ("app", cnt[0] % 4)
                    cnt[0] += 1
                    hs = slice(t0, t0 + 127 * dl + 1, dl)
                    for c in range(8):
                        A("pe", lambda e, p=p, c=c, hs=hs, wb=wb: e.matmul(
                            p[:, 0:256], lhsT=hT[:, c, hs], rhs=wbf[wb][:, c, 0:256],
                            start=(c == 0), stop=(c == 7)),
                          r=[("wbf", wb)] + [("hT", k) for k in range(4)], w=[pk])
                    A("dve", lambda e, p=p, ti=ti: e.tensor_copy(
                        out=Vg[:, ti, :, 0:64], in_=p[:, 0:256].rearrange("p (h e) -> p h e", e=64)),
                      r=[pk], w=[("Vg", ti)])
            sa.close()
            sb2 = Scope()
            ef = [sb2.sb(f"ef{i}", [128, 4, 128], BF16) for i in range(4)]
            em = [sb2.sb(f"em{i}", [128, 4, 128], BF16) for i in range(4)]
            pS = [sb2.ps(f"pS{i}", [128, 4, 128]) for i in range(4)]
            pO = [sb2.ps(f"pO{i}", [128, 4, 128]) for i in range(2)]
            kc = [0]
            allQK = [("QK", w_, jj, tb) for w_ in range(2) for jj in range(4) for tb in range(4)]

            def s_phase(r_, n):
                t0 = r_ + dl * 128 * n
                qs = slice(t0, t0 + 127 * dl + 1, dl)
                kbs = ([n - 1] if n > 0 else []) + [n]
                ems = []
                for kb in kbs:
                    i = kc[0] % 4
                    kc[0] += 1
                    k0 = r_ + dl * 128 * kb
                    ks = slice(k0, k0 + 127 * dl + 1, dl)
                    for hh in range(4):
                        A("pe", lambda e, i=i, hh=hh, ks=ks, qs=qs: e.matmul(
                            pS[i][:, hh, :], lhsT=KT[:, hh, ks], rhs=QT[:, hh, qs],
                            start=True, stop=True),
                          r=allQK, w=[("pS", i)])
                    A("act", lambda e, i=i: e.activation(out=ef[i][:], in_=pS[i][:], func=AF.Exp, scale=0.125),
                      r=[("pS", i)], w=[("ef", i)])
                    mname = "maskUb" if kb == n else "maskLb"
                    A("pool", lambda e, i=i, mname=mname: e.tensor_tensor(
                        out=em[i][:], in0=ef[i][:], in1=cst[mname][:].unsqueeze(1).to_broadcast([128, 4, 128]),
                        op=ALU.mult),
                      r=[("ef", i), mname], w=[("em", i)])
                    ems.append((i, r_ * nblk + kb))
                return qs, ems

            def pv_phase(it, qs, ems):
                o = it % 2
                for hh in range(4):
                    for idx, (i, ti) in enumerate(ems):
                        A("pe", lambda e, o=o, hh=hh, i=i, ti=ti, idx=idx, last=(idx == len(ems) - 1): e.matmul(
                            pO[o][:, hh, :], lhsT=Vg[:, ti, hh, :], rhs=em[i][:, hh, :],
                            start=(idx == 0), stop=last),
                          r=[("Vg", ti), ("Vg1",), ("em", i)], w=[("pO", o)])
                if g == 0:
                    A("act", lambda e, o=o, qs=qs: e.copy(out=ND[:, :, qs], in_=pO[o][:]),
                      r=[("pO", o)], w=["ND"])
                else:
                    A("dve", lambda e, o=o, qs=qs: e.tensor_tensor(out=ND[:, :, qs], in0=ND[:, :, qs],
                                                                  in1=pO[o][:], op=ALU.add),
                      r=[("pO", o), "ND"], w=["ND"])

            its = [(r_, n) for r_ in range(dl) for n in range(nblk)]
            prev_ = None
            for k_, (r_, n) in enumerate(its):
                cur_ = s_phase(r_, n)
                if prev_ is not None:
                    pv_phase(k_ - 1, *prev_)
                prev_ = cur_
            pv_phase(len(its) - 1, *prev_)
            sb2.close()
            sg_.close()

        for g, (win, dl) in enumerate(ATT):
            group(g, dl)
        sf = Scope()
        pB = [sf.ps(f"pB{i}", [128, 512]) for i in range(2)]
        shsel = sf.sb("shsel", [128, 64])
        A("sp", lambda e: e.dma_start(out=shsel[:], in_=shsel_d[:, :]), w=["shsel"], dma=True)
        k = 0
        for hh in range(4):
            A("act", lambda e, hh=hh: e.activation(out=ND[64:128, hh, :], in_=ND[64:128, hh, :], func=AF.Ln), r=["ND"], w=["ND"])
            A("act", lambda e, hh=hh: e.activation(out=ND[64:128, hh, :], in_=ND[64:128, hh, :], func=AF.Exp, scale=-1.0),
              r=["ND"], w=["ND"])
            for tb in range(4):
                i = k % 2
                k += 1
                ts_ = slice(tb * 512, (tb + 1) * 512)
                A("pe", lambda e, i=i, hh=hh, ts_=ts_: e.matmul(pB[i][0:64, :], lhsT=shsel[:], rhs=ND[:, hh, ts_],
                                                               start=True, stop=True),
                  r=["ND", "shsel"], w=[("pB", i)])
                A("dve", lambda e, i=i, hh=hh, ts_=ts_: e.tensor_tensor(out=ybT[:, hh, ts_], in0=ND[0:64, hh, ts_],
                                                                       in1=pB[i][0:64, :], op=ALU.mult),
                  r=["ND", ("pB", i)], w=[("ybT", hh, tb)])
        sf.close()
        so.close()

    wa_d = din("w_a", [512, D])
    wb_d = din("w_b", [256, D])
    wout_d = din("w_out", [D, D])

    def stage_merge(s, alloc_xacc):
        hT, yaT, ybT = ctx["hT"], ctx["yaT"], ctx["ybT"]
        sm = Scope()
        mT = sm.sb("mT", [128, 8, S], BF16)
        si = Scope()
        wa = si.sb("wa", [128, 4, D], BF16)
        wbb = si.sb("wbb", [64, 4, D], BF16)
        A("pool", lambda e: e.dma_start(out=wa[:], in_=wa_d.rearrange("(f p) n -> p f n", p=128)), w=["wa"], dma=True)
        A("pool", lambda e: e.dma_start(out=wbb[:], in_=wb_d.rearrange("(h e) n -> e h n", e=64)), w=["wbb"], dma=True)
        sg = [si.sb(f"sg{i}", [128, 512]) for i in range(4)]
        m12 = [si.sb(f"m12{i}", [128, 512]) for i in range(4)]
        pp = [si.ps(f"mpp{i}", [128, 512]) for i in range(4)]
        pab = [si.ps(f"mpab{i}", [128, 512]) for i in range(4)]
        k = 0
        for jb in range(2):
            wga = load_wblock(4352 + jb * 512)
            wgb = load_wblock(5376 + jb * 512)
            for jj in range(4):
                j = jb * 4 + jj
                js = slice(j * 128, (j + 1) * 128)
                for tb in range(4):
                    ts_ = slice(tb * 512, (tb + 1) * 512)
                    ia, ib = (2 * k) % 4, (2 * k + 1) % 4
                    k += 1
                    proj_fm(pp[ia][:], ("mpp", ia), wga, jj, tb * 512, 512)
                    A("act", lambda e, ia=ia: e.activation(out=sg[ia][:], in_=pp[ia][:], func=AF.Sigmoid),
                      r=[("mpp", ia)], w=[("sg", ia)])
                    for f in range(4):
                        A("pe", lambda e, ia=ia, f=f, js=js, ts_=ts_: e.matmul(
                            pab[ia][:], lhsT=wa[:, f, js], rhs=yaT[:, f, ts_], start=(f == 0), stop=(f == 3)),
                          r=["wa", "yaT"], w=[("mpab", ia)])
                    A("dve", lambda e, ia=ia: e.tensor_tensor(out=m12[ia][:], in0=pab[ia][:], in1=sg[ia][:], op=ALU.mult),
                      r=[("mpab", ia), ("sg", ia)], w=[("m12", ia)])
                    proj_fm(pp[ib][:], ("mpp", ib), wgb, jj, tb * 512, 512)
                    A("act", lambda e, ib=ib: e.activation(out=sg[ib][:], in_=pp[ib][:], func=AF.Sigmoid),
                      r=[("mpp", ib)], w=[("sg", ib)])
                    for hh in range(4):
                        A("pe", lambda e, ib=ib, hh=hh, js=js, ts_=ts_: e.matmul(
                            pab[ib][:], lhsT=wbb[:, hh, js], rhs=ybT[:, hh, ts_], start=(hh == 0), stop=(hh == 3)),
                          r=["wbb", "ybT"], w=[("mpab", ib)])
                    A("dve", lambda e, ib=ib: e.tensor_tensor(out=m12[ib][:], in0=pab[ib][:], in1=sg[ib][:], op=ALU.mult),
                      r=[("mpab", ib), ("sg", ib)], w=[("m12", ib)])
                    A("pool", lambda e, ia=ia, ib=ib, j=j, ts_=ts_: e.tensor_tensor(
                        out=mT[:, j, ts_], in0=m12[ia][:], in1=m12[ib][:], op=ALU.add),
                      r=[("m12", ia), ("m12", ib)], w=[("mT", j, tb)])
        si.close()
        alloc_xacc()
        xacc = ctx["xacc"]
        so = Scope()
        wbf = ctx["wbf"]
        wo = [load_wblock(mh * 512, 512, src=wout_d) for mh in range(2)]
        po = [so.ps(f"po{i}", [128, 512]) for i in range(4)]
        k = 0
        for i in range(NT):
            A("sp", lambda e, i=i: e.dma_start(out=xacc[:, i, :], in_=x_d[s, i * 128:(i + 1) * 128, :]),
              w=[("xacc", i)], dma=True)
        for i in range(NT):
            for mh in range(2):
                pi = k % 4
                k += 1
                ms_ = slice(mh * 512, (mh + 1) * 512)
                for n in range(8):
                    A("pe", lambda e, pi=pi, n=n, i=i, mh=mh: e.matmul(
                        po[pi][:], lhsT=mT[:, n, i * 128:(i + 1) * 128], rhs=wbf[wo[mh]][:, n, :],
                        start=(n == 0), stop=(n == 7)),
                      r=[("wbf", wo[mh])] + [("mT", n, i // 4)], w=[("po", pi)])
                A("dve", lambda e, pi=pi, i=i, ms_=ms_: e.tensor_tensor(out=xacc[:, i, ms_], in0=xacc[:, i, ms_],
                                                                       in1=po[pi][:], op=ALU.add),
                  r=[("po", pi), ("xacc", i)], w=[("xacc", i)])
        so.close()
        sm.close()

    wq_d = din("peer_wq", [D, D])
    KB_d = din("peer_kb", [128, 8, 256])
    KT12_d = din("peer_kt12", [128, 8, 128])
    uT_d = din("peer_uT", [D, 16384])
    v_d = din("peer_v", [16384, D])
    NEG_BIG = -1.0e30
    EGS = 512
    NEG = 16384 // EGS

    def stage_peer(s, dbg=None):
        h2T, xacc = ctx["hT"], ctx["xacc"]
        sp_ = Scope()
        qpT = sp_.sb("qpT", [128, 8, S], BF16)
        thr = sp_.sb("thr", [128, NT, 8])
        nb = sp_.sb("nb", [128, NT, 8])
        KT12 = sp_.sb("KT12", [128, 8, 128], BF16)
        A("pool", lambda e: e.dma_start(out=KT12[:], in_=KT12_d[:, :, :]), w=["KT12"], dma=True)
        sa = Scope()
        wq = sa.sb("wq", [128, 8, D], BF16)
        KB = sa.sb("KB", [128, 8, 256], BF16)
        A("pool", lambda e: e.dma_start(out=wq[:], in_=wq_d.rearrange("(c p) n -> p c n", p=128)), w=["wq"], dma=True)
        A("pool", lambda e: e.dma_start(out=KB[:], in_=KB_d[:, :, :]), w=["KB"], dma=True)
        s12 = [sa.sb(f"s12_{i}", [128, 8, 256]) for i in range(2)]
        wk = sa.sb("wk", [128, 8, 256])
        t16 = sa.sb("t16", [128, 8, 2, 16])
        cand = sa.sb("cand", [128, 8, 256])
        cand2 = sa.sb("cand2", [128, 8, 256])
        c16 = sa.sb("c16", [128, 8, 16])
        negm = sa.sb("negm", [128, 8])
        dlt = sa.sb("dlt", [128, 8, 16])
        zz = sa.sb("zz", [128, 8])
        pq = [sa.ps(f"pq{i}", [128, 512]) for i in range(2)]
        ps12 = [sa.ps(f"ps12{i}", [128, 2, 256]) for i in range(2)]
        k = 0
        for h in range(8):
            for tb in range(4):
                i = k % 2
                k += 1
                ts_ = slice(tb * 512, (tb + 1) * 512)
                for c in range(8):
                    A("pe", lambda e, i=i, c=c, h=h, ts_=ts_: e.matmul(
                        pq[i][:], lhsT=wq[:, c, h * 128:(h + 1) * 128], rhs=h2T[:, c, ts_],
                        start=(c == 0), stop=(c == 7)),
                      r=["wq", ("hT", tb)], w=[("pq", i)])
                if k % 2 == 0:
                    A("act", lambda e, i=i, h=h, ts_=ts_: e.copy(out=qpT[:, h, ts_], in_=pq[i][:]),
                      r=[("pq", i)], w=[("qpT", h, tb)])
                else:
                    A("dve", lambda e, i=i, h=h, ts_=ts_: e.tensor_copy(out=qpT[:, h, ts_], in_=pq[i][:]),
                      r=[("pq", i)], w=[("qpT", h, tb)])
        k = 0
        for i in range(NT):
            sb_ = s12[i % 2]
            sk = ("s12", i % 2)
            tk = slice(i * 128, (i + 1) * 128)
            for hp in range(4):
                pi = k % 2
                k += 1
                for hh in range(2):
                    h = hp * 2 + hh
                    A("pe", lambda e, pi=pi, hh=hh, h=h, tk=tk: e.matmul(
                        ps12[pi][:, hh, :], lhsT=qpT[:, h, tk], rhs=KB[:, h, :], start=True, stop=True),
                      r=[("qpT", h, i // 4), "KB"], w=[("ps12", pi)])
                A("act", lambda e, pi=pi, hp=hp, sb_=sb_: e.copy(out=sb_[:, hp * 2:hp * 2 + 2, :], in_=ps12[pi][:]),
                  r=[("ps12", pi)], w=[sk])
            hh_ = [(h, half, slice(half * 128, (half + 1) * 128)) for h in range(8) for half in range(2)]
            for h, half, hs_ in hh_:
                A("dve", lambda e, sb_=sb_, h=h, half=half, hs_=hs_: e.max(out=t16[:, h, half, 0:8], in_=sb_[:, h, hs_]),
                  r=[sk], w=[("t16a", h, half)])
            for h, half, hs_ in hh_:
                A("dve", lambda e, sb_=sb_, h=h, half=half, hs_=hs_: e.match_replace(
                    out=wk[:, h, hs_], in_to_replace=t16[:, h, half, 0:8], in_values=sb_[:, h, hs_],
                    imm_value=NEG_BIG),
                  r=[sk, ("t16a", h, half)], w=[("wk", h, half)])
            for h, half, hs_ in hh_:
                A("dve", lambda e, h=h, half=half, hs_=hs_: e.max(out=t16[:, h, half, 8:16], in_=wk[:, h, hs_]),
                  r=[("wk", h, half)], w=[("t16b", h, half)])
            for h in range(8):
                A("pool", lambda e, h=h: e.tensor_tensor(
                    out=cand[:, h, :].rearrange("p (a b) -> p a b", b=16),
                    in0=t16[:, h, 0, :].unsqueeze(2).to_broadcast([128, 16, 16]),
                    in1=t16[:, h, 1, :].unsqueeze(1).to_broadcast([128, 16, 16]), op=ALU.add),
                  r=[("t16a", h, 0), ("t16a", h, 1), ("t16b", h, 0), ("t16b", h, 1)], w=[("cand", h)])
            for h in range(8):
                A("dve", lambda e, h=h: e.max(out=c16[:, h, 0:8], in_=cand[:, h, :]), r=[("cand", h)], w=[("c16a", h)])
            for h in range(8):
                A("dve", lambda e, h=h: e.match_replace(out=cand2[:, h, :], in_to_replace=c16[:, h, 0:8],
                                                        in_values=cand[:, h, :], imm_value=NEG_BIG),
                  r=[("cand", h), ("c16a", h)], w=[("cand2", h)])
            for h in range(8):
                A("dve", lambda e, h=h: e.max(out=c16[:, h, 8:16], in_=cand2[:, h, :]), r=[("cand2", h)], w=[("c16b", h)])
            c16k = [("c16a", h) for h in range(8)] + [("c16b", h) for h in range(8)]
            A("dve", lambda e: e.tensor_scalar(out=negm[:], in0=c16[:, :, 0], scalar1=-1.0, scalar2=None, op0=ALU.mult),
              r=c16k, w=["negm"])
            A("dve", lambda e, i=i: e.tensor_scalar(out=thr[:, i, :], in0=c16[:, :, 15], scalar1=-1.0e-4, scalar2=None,
                                                    op0=ALU.add),
              r=c16k, w=[("thr", i)])
            A("dve", lambda e: e.tensor_tensor(out=dlt[:], in0=c16[:], in1=negm[:].unsqueeze(2).to_broadcast([128, 8, 16]),
                                               op=ALU.add),
              r=c16k + ["negm"], w=["dlt"])
            A("act", lambda e: e.activation(out=dlt[:], in_=dlt[:], func=AF.Exp), r=["dlt"], w=["dlt"])
            A("dve", lambda e: e.reduce_sum(out=zz[:], in_=dlt[:], axis=AX.X), r=["dlt"], w=["zz"])
            A("act", lambda e: e.activation(out=zz[:], in_=zz[:], func=AF.Ln), r=["zz"], w=["zz"])
            A("dve", lambda e, i=i: e.tensor_sub(out=nb[:, i, :], in0=negm[:], in1=zz[:]),
              r=["negm", "zz"], w=[("nb", i)])
        if dbg is not None:
            dthr, dnb, dq = dbg
            A("sp", lambda e: e.dma_start(out=dthr[:, :, :], in_=thr[:]), r=[("thr", i) for i in range(NT)], dma=True)
            A("sp", lambda e: e.dma_start(out=dnb[:, :, :], in_=nb[:]), r=[("nb", i) for i in range(NT)], dma=True)
            A("sp", lambda e: e.dma_start(out=dq[:, :, :], in_=qpT[:]),
              r=[("qpT", h, tb) for h in range(8) for tb in range(4)], dma=True)
        sa.close()
        sm = Scope()
        FKT = sm.sb("FKT", [128, 8, EGS], BF16)
        uT = [sm.sb(f"uT{i}", [128, 8, EGS], BF16) for i in range(2)]
        vv = [sm.sb(f"vv{i}", [128, 4, D], BF16) for i in range(2)]
        G = [sm.sb(f"G{i}", [128, 4, 512], BF16) for i in range(2)]
        E = [sm.sb(f"E{i}", [128, 512], BF16) for i in range(4)]
        EmE = [[sm.sb(f"EmE{j}_{k}", [128, 512], BF16) for k in range(4)] for j in range(2)]
        EmO = [sm.sb(f"EmO{k}", [128, 512], BF16) for k in range(4)]
        AT = [sm.sb(f"AT{i}", [128, 4, 128], BF16) for i in range(2)]
        pH = [sm.ps(f"pH{i}", [128, 512]) for i in range(1)]
        pS = [sm.ps(f"pS{i}", [128, 512]) for i in range(4)]
        pW = [sm.ps(f"pW{i}", [128, 4, 128]) for i in range(1)]
        pOut = [sm.ps(f"pOut{i}", [128, 512]) for i in range(2)]
        Gpre = sm.sb("Gpre", [128, 4, 512], BF16)
        A("pool", lambda e: e.tensor_copy(
            out=FKT[64:128, :, :].rearrange("p h (c i) -> p h c i", i=128),
            in_=KT12[64:128, :, :].unsqueeze(2).to_broadcast([64, 8, 4, 128])),
          r=["KT12"], w=[("FKTb",)])
        cn = {"H": 0, "S": 0, "E": 0, "O": 0}

        def load_group(eg):
            b = eg % 2
            A("pool", lambda e: e.dma_start(
                out=uT[b][:], in_=uT_d[:, eg * EGS:(eg + 1) * EGS].rearrange("(c p) n -> p c n", p=128)),
              w=[("uT", b)], dma=True)
            A("pool", lambda e: e.dma_start(
                out=vv[b][:], in_=v_d[eg * EGS:(eg + 1) * EGS, :].rearrange("(c p) n -> p c n", p=128)),
              w=[("vv", b)], dma=True)

        def fkt_group(eg):
            for hq in range(2):
                A("dve", lambda e, hq=hq: e.tensor_copy(
                    out=FKT[0:64, hq * 4:(hq + 1) * 4, :].rearrange("p h (c i) -> p h c i", i=128),
                    in_=KT12[0:64, hq * 4:(hq + 1) * 4, eg * 4:(eg + 1) * 4].unsqueeze(3).to_broadcast([64, 4, 4, 128])),
                  r=["KT12"], w=[("FKTt", h) for h in range(hq * 4, (hq + 1) * 4)])

        def h_mm(gs, cc, c):
            eg, sbk = gs // 4, gs % 4
            b = eg % 2
            A("pe", lambda e: e.matmul(
                pH[0][:], lhsT=uT[b][:, c, cc * 128:(cc + 1) * 128],
                rhs=h2T[:, c, sbk * 512:(sbk + 1) * 512], start=(c == 0), stop=(c == 7)),
              r=[("uT", b), ("hT", sbk)], w=[("pH", 0)])

        def h_copy(cc):
            A("act", lambda e: e.copy(out=Gpre[:, cc, :], in_=pH[0][:]), r=[("pH", 0)], w=[("Gpre", cc)])

        def gelu_all(gs):
            A("act", lambda e: e.activation(out=G[gs % 2][:], in_=Gpre[:], func=AF.Gelu),
              r=[("Gpre", cc) for cc in range(4)], w=[("G", gs % 2)])

        def s_head(u, h):
            eg, i = u // NT, u % NT
            tk = slice(i * 128, (i + 1) * 128)
            p_ = cn["S"] % 4
            cn["S"] += 1
            x_ = cn["E"] % 4
            cn["E"] += 1
            es_ = u % 2
            A("pe", lambda e: e.matmul(pS[p_][:], lhsT=qpT[:, h, tk], rhs=FKT[:, h, :], start=True, stop=True),
              r=[("qpT", h, i // 4), ("FKTb",), ("FKTt", h)], w=[("pS", p_)])
            A("act", lambda e: e.activation(out=E[x_][:], in_=pS[p_][:], func=AF.Exp, bias=nb[:, i, h:h + 1]),
              r=[("pS", p_), ("nb", i)], w=[("E", x_)])
            if h % 2 == 0:
                dst, dk = EmE[es_][h // 2], ("EmE", es_, h // 2)
            else:
                dst, dk = EmO[h // 2], ("EmO", h // 2)
            A("dve", lambda e: e.scalar_tensor_tensor(
                out=dst[:], in0=pS[p_][:], scalar=thr[:, i, h:h + 1], in1=E[x_][:],
                op0=ALU.is_ge, op1=ALU.mult),
              r=[("pS", p_), ("E", x_), ("thr", i)], w=[dk])
            if h % 2 == 1:
                pk_ = ("EmE", es_, h // 2)
                A("pool", lambda e: e.tensor_tensor(out=EmE[es_][h // 2][:], in0=EmE[es_][h // 2][:], in1=dst[:],
                                                    op=ALU.add),
                  r=[pk_, dk], w=[pk_])

        def t_piece(u, slot):
            es_ = u % 2
            cc = slot // 2
            for pr in range((slot % 2) * 2, (slot % 2) * 2 + 2):
                A("pe", lambda e, pr=pr: e.matmul(
                    pW[0][:, cc, :], lhsT=EmE[es_][pr][:, cc * 128:(cc + 1) * 128], rhs=ident[:],
                    start=(pr == 0), stop=(pr == 3)),
                  r=[("EmE", es_, pr), "ident"], w=[("pW", 0)])

        def at_piece(u):
            eg, i = u // NT, u % NT
            gs, tt = eg * 4 + i // 4, i % 4
            w_ = u % 2
            A("dve", lambda e: e.tensor_tensor(
                out=AT[w_][:], in0=pW[0][:], in1=G[gs % 2][:, :, tt * 128:(tt + 1) * 128], op=ALU.mult),
              r=[("pW", 0), ("G", gs % 2)], w=[("AT", w_)])

        def o_mm(u, k):
            eg, i = u // NT, u % NT
            b = eg % 2
            w_ = u % 2
            mh, cc = k // 4, k % 4
            ms_ = slice(mh * 512, (mh + 1) * 512)
            A("pe", lambda e: e.matmul(
                pOut[mh][:], lhsT=AT[w_][:, cc, :], rhs=vv[b][:, cc, ms_], start=(cc == 0), stop=(cc == 3)),
              r=[("AT", w_), ("vv", b)], w=[("pOut", mh)])

        tmpO = sm.sb("tmpO", [128, 512])

        def add_piece(u, mh):
            eg, i = u // NT, u % NT
            ms_ = slice(mh * 512, (mh + 1) * 512)
            if mh == 1:
                A("act", lambda e: e.copy(out=tmpO[:], in_=pOut[1][:]), r=[("pOut", 1)], w=["tmpO"])
                if eg < NEG - 1:
                    A("pool", lambda e: e.tensor_tensor(out=xacc[:, i, ms_], in0=xacc[:, i, ms_], in1=tmpO[:], op=ALU.add),
                      r=["tmpO", ("xacc", i)], w=[("xacc", i)])
                else:
                    A("pool", lambda e: e.tensor_tensor(out=ostage[:], in0=xacc[:, i, ms_], in1=tmpO[:], op=ALU.add),
                      r=["tmpO", ("xacc", i)], w=["ostage"])
                    A("sp", lambda e: e.dma_start(out=out_d[s, i * 128:(i + 1) * 128, ms_], in_=ostage[:]),
                      r=["ostage"], dma=True)
                return
            if eg < NEG - 1:
                A("dve", lambda e: e.tensor_tensor(out=xacc[:, i, ms_], in0=xacc[:, i, ms_], in1=pOut[mh][:], op=ALU.add),
                  r=[("pOut", mh), ("xacc", i)], w=[("xacc", i)])
            else:
                A("dve", lambda e: e.tensor_tensor(out=ostage[:], in0=xacc[:, i, ms_], in1=pOut[mh][:], op=ALU.add),
                  r=[("pOut", mh), ("xacc", i)], w=["ostage"])
                A("sp", lambda e: e.dma_start(out=out_d[s, i * 128:(i + 1) * 128, ms_], in_=ostage[:]),
                  r=["ostage"], dma=True)

        NU = NEG * NT
        load_group(0)
        load_group(1)
        for cc in range(4):
            for c in range(8):
                h_mm(0, cc, c)
            h_copy(cc)
        gelu_all(0)
        for v in range(NU + 3):
            cur = v < NU
            tv = v - 1 if 0 <= v - 1 < NU else None
            oa = v - 2 if 0 <= v - 2 < NU else None
            ob = v - 3 if 0 <= v - 3 < NU else None
            hc = None
            if cur:
                eg, i = v // NT, v % NT
                if i == 0:
                    fkt_group(eg)
                if i == 4 and 1 <= eg < NEG - 1:
                    load_group(eg + 1)
                gs, tt = eg * 4 + i // 4, i % 4
                if gs + 1 < NEG * 4:
                    hc = (gs + 1, tt)
            hk = 0
            for h in range(8):
                if cur:
                    s_head(v, h)
                if h < 4 and ob is not None:
                    o_mm(ob, 4 + h)
                if h >= 4 and oa is not None:
                    o_mm(oa, h - 4)
                if hc is not None and h >= 1:
                    h_mm(hc[0], hc[1], hk)
                    hk += 1
                    if h == 7:
                        h_mm(hc[0], hc[1], hk)
                if tv is not None:
                    t_piece(tv, h)
                if ob is not None and h == 4:
                    add_piece(ob, 1)
            if hc is not None:
                h_copy(hc[1])
                if hc[1] == 3:
                    gelu_all(hc[0])
            if tv is not None:
                at_piece(tv)
            if oa is not None:
                add_piece(oa, 0)
        sm.close()
        sp_.close()

    def run_sequence(s, upto="all"):
        sq_ = Scope()
        ctx["hT"] = sq_.sb("hT", [128, 8, S], BF16, side="right")
        sy = Scope()
        ctx["wbf"] = [sy.sb(f"wbf{i}", [128, 8, 512], BF16) for i in range(3)]
        stage_norm(s, "x")
        ctx["yaT"] = sy.sb("yaT", [128, 4, S], BF16)
        stage_hgrn(s)
        if upto == "hgrn":
            return
        ctx["ybT"] = sy.sb("ybT", [64, 4, S], BF16)
        stage_attn(s)
        if upto == "attn":
            return
        def alloc_xacc():
            ctx["xacc"] = sq_.sb("xacc", [128, NT, D], F32, side="right")

        stage_merge(s, alloc_xacc)
        sy.close()
        if upto == "merge":
            return
        stage_norm(s, "xacc")
        if upto == "norm2":
            return
        if upto == "peerprep":
            dthr = dbg_out("thr", [128, NT, 8])
            dnb = dbg_out("nb", [128, NT, 8])
            dq = dbg_out("qpT", [128, 8, S], BF16)
            stage_peer(s, dbg=(dthr, dnb, dq))
        else:
            stage_peer(s)
        sq_.close()

    if debug == "hgrn":
        run_sequence(0, "hgrn")
        dbg_ya = dbg_out("yaT", [128, 4, S], BF16)
        A("sp", lambda e: e.dma_start(out=dbg_ya[:, :, :], in_=ctx["yaT"][:]), dma=True)
    elif debug == "attn":
        run_sequence(0, "attn")
        dbg_ya = dbg_out("yaT", [128, 4, S], BF16)
        A("sp", lambda e: e.dma_start(out=dbg_ya[:, :, :], in_=ctx["yaT"][:]), dma=True)
        dbg_yb = dbg_out("ybT", [64, 4, S], BF16)
        A("sp", lambda e: e.dma_start(out=dbg_yb[:, :, :], in_=ctx["ybT"][:]), dma=True)
    elif debug in ("peerprep", "seq0"):
        run_sequence(0, debug)
    elif debug is None:
        for s_ in range(nseq):
            run_sequence(s_)
    elif debug == "norm2":
        run_sequence(0, "norm2")
        dbg_x1 = dbg_out("x1", [128, NT, D])
        A("sp", lambda e: e.dma_start(out=dbg_x1[:, :, :], in_=ctx["xacc"][:]), dma=True)
        dbg_h2 = dbg_out("h2T", [128, 8, S], BF16)
        A("sp", lambda e: e.dma_start(out=dbg_h2[:, :, :], in_=ctx["hT"][:]), dma=True)

    sc.barrier()

    with ExitStack() as es2:
        sems = {e: es2.enter_context(nc.semaphore("sem_" + e)) for e in Sched.ENGS}
        dma_sems = {e: [es2.enter_context(nc.semaphore(f"dsem_{e}{i}")) for i in range(8)] for e in ("sp", "pool", "act")}
        sc.finalize(sems, dma_sems)
        with nc.Block() as block:
            @block.sync
            def _(E):
                sc.emit_engine("sp", E)

            @block.scalar
            def _(E):
                sc.emit_engine("act", E)

            @block.vector
            def _(E):
                sc.emit_engine("dve", E)

            @block.gpsimd
            def _(E):
                sc.emit_engine("pool", E)

            @block.tensor
            def _(E):
                sc.emit_engine("pe", E)
    try:
        es.close()
    except AssertionError:
        if debug is None:
            raise
    return nc, dbg_outs


def host_consts():
    bf = ml_dtypes.bfloat16
    c = {}
    c["ident"] = np.eye(128, dtype=np.float32).astype(bf)
    i = np.arange(128)
    c["maskU"] = (i[:, None] <= i[None, :]).astype(np.float32)
    c["onesv"] = np.full((128, 128), 1.0 / 128, np.float32).astype(bf)
    sm = np.ones((128, 1024), np.float32)
    sm[:, ::128] = 0.0
    c["scanm"] = sm
    b64 = np.zeros((128, 128), np.float32)
    b64[:64, :64] = 1.0 / 64
    b64[64:, 64:] = 1.0 / 64
    c["blk64"] = b64.astype(bf)
    c["ones64"] = np.ones((128, 64), np.float32).astype(bf)
    c["epsc"] = np.full((128, 1), EPS, np.float32)
    sh = np.zeros((128, 64), np.float32)
    sh[64 + np.arange(64), np.arange(64)] = 1.0
    c["shsel"] = sh
    c["maskUb"] = (i[:, None] <= i[None, :]).astype(np.float32).astype(bf)
    c["maskLb"] = (i[:, None] >= i[None, :]).astype(np.float32).astype(bf)
    return c


def make_in_maps(inputs, nseq=NSEQ, ncores=NCORES):
    x = np.ascontiguousarray(inputs["x"], dtype=np.float32)
    consts = host_consts()
    w_in = np.ascontiguousarray(inputs["w_in"][0], dtype=np.float32)
    g1bc = np.ascontiguousarray(np.broadcast_to(inputs["norm1_g"][0][None, :], (128, D)), dtype=np.float32)
    g2bc = np.ascontiguousarray(np.broadcast_to(inputs["norm2_g"][0][None, :], (128, D)), dtype=np.float32)
    w_a = np.ascontiguousarray(inputs["w_branch_a"][0], dtype=np.float32)
    w_b = np.ascontiguousarray(inputs["w_branch_b"][0], dtype=np.float32)
    w_out = np.ascontiguousarray(inputs["w_out"][0], dtype=np.float32)
    lg = np.asarray(inputs["hg_lb_logits"], dtype=np.float32)
    lbl = np.ascontiguousarray(np.concatenate([lg[0].reshape(4, 128).T, lg[1].reshape(4, 128).T], axis=1))
    hgg = np.ascontiguousarray(inputs["hg_norm_g"][0].reshape(128, 1), dtype=np.float32)
    gq = np.ascontiguousarray(np.tile(inputs["q_norm_g"][0], 2).reshape(128, 1), dtype=np.float32)
    gk = np.ascontiguousarray(np.tile(inputs["k_norm_g"][0], 2).reshape(128, 1), dtype=np.float32)
    wq = np.ascontiguousarray(inputs["peer_wq"][0], dtype=np.float32)
    sk = np.asarray(inputs["peer_subkeys"][0], dtype=np.float32)
    kt12 = np.ascontiguousarray(np.concatenate([sk[:, 0].transpose(2, 0, 1), sk[:, 1].transpose(2, 0, 1)], axis=0))
    kb = np.zeros((128, 8, 256), np.float32)
    kb[0:64, :, 0:128] = kt12[0:64]
    kb[64:128, :, 128:256] = kt12[64:128]
    uT = np.ascontiguousarray(inputs["peer_u"][0].T, dtype=np.float32)
    pv = np.ascontiguousarray(inputs["peer_v"][0], dtype=np.float32)
    maps = []
    for c in range(ncores):
        m = {"x": x[c * nseq:(c + 1) * nseq], "w_in": w_in, "g1bc": g1bc, "g2bc": g2bc, "lbl": lbl, "hgg": hgg,
             "gq": gq, "gk": gk, "w_a": w_a, "w_b": w_b, "w_out": w_out,
             "peer_wq": wq, "peer_kb": kb, "peer_kt12": kt12, "peer_uT": uT, "peer_v": pv}
        m.update(consts)
        maps.append(m)
    return maps


def kernel(**inputs):
    nc, _ = build_program()
    in_maps = make_in_maps(inputs)
    res = run_bass_kernel_spmd(nc, in_maps, core_ids=list(range(NCORES)))
    return np.concatenate([r["out"] for r in res.results], axis=0)
```
